# Optimizing a Trainium2 kernel written in Bass

```python
import jax, jax.numpy as jnp
from jax import lax
import numpy as np

D_MODEL = 1024
BATCH = 4
SEQ = 8192
DEPTH = 2

CHUNK = 64
MEM_TOKENS = 256
RMS_EPS = 1e-6
XA_HEADS = 4
XA_HEAD_DIM = D_MODEL // XA_HEADS
HGRN_HEADS = 4
HGRN_DK = 128
HGRN_DV = 128
HGRN_WIDTH = HGRN_HEADS * HGRN_DV
GLA_HEADS = 4
GLA_DK = 64
GLA_DV = 128
GLA_RANK = 16
GLA_GATE_NORMALIZER = 16.0
GLA_WIDTH = GLA_HEADS * GLA_DV
MLSTM_HEADS = 4
MLSTM_DH = 128
MLSTM_WIDTH = MLSTM_HEADS * MLSTM_DH
MLSTM_CONV = 4
MLSTM_QKV_BLOCK = 4

MIX_WIDTH = HGRN_WIDTH + GLA_WIDTH + MLSTM_WIDTH
IN_SPLITS = [
    HGRN_HEADS * HGRN_DK,
    HGRN_HEADS * HGRN_DK,
    HGRN_WIDTH,
    HGRN_WIDTH,
    GLA_HEADS * GLA_DK,
    GLA_HEADS * GLA_DK,
    GLA_WIDTH,
    GLA_RANK,
    GLA_WIDTH,
    MLSTM_WIDTH,
    MLSTM_WIDTH,
    MLSTM_HEADS,
    MLSTM_HEADS,
]
IN_COLS = 4 * HGRN_WIDTH + 2 * GLA_HEADS * GLA_DK + 2 * GLA_WIDTH + GLA_RANK + 2 * MLSTM_WIDTH + 2 * MLSTM_HEADS

kernel_name = "hybrid_hgrn2_gla_mlstm_memxattn"


def rms_norm(x, g):
    xf = x.astype(jnp.float32)
    y = xf * lax.rsqrt(jnp.mean(xf * xf, axis=-1, keepdims=True) + RMS_EPS)
    return (y * g.astype(jnp.float32)).astype(x.dtype)


def head_rms_norm(o, g):
    y = o * lax.rsqrt(jnp.mean(o * o, axis=-1, keepdims=True) + RMS_EPS) * g.astype(jnp.float32)
    return y.reshape(o.shape[0], o.shape[1], -1)


def head_layer_norm(o, g):
    mu = jnp.mean(o, axis=-1, keepdims=True)
    oc = o - mu
    y = oc * lax.rsqrt(jnp.mean(oc * oc, axis=-1, keepdims=True) + RMS_EPS)
    return y.reshape(o.shape[0], o.shape[1], -1) * g.astype(jnp.float32)


def split_heads(t, n_heads):
    return t.reshape(t.shape[0], t.shape[1], n_heads, -1)


def to_chunks(t):
    B, S, H, d = t.shape
    return t.reshape(B, S // CHUNK, CHUNK, H, d).transpose(1, 0, 3, 2, 4)


def from_chunks(t):
    n, B, H, C, d = t.shape
    return t.transpose(1, 0, 3, 2, 4).reshape(B, n * C, H, d)


def chunked_decay_linear_attention(q, k, v, log_a):
    B, S, H, dk = q.shape
    dv = v.shape[-1]
    causal = jnp.tril(jnp.ones((CHUNK, CHUNK), dtype=bool))

    def step(state, inp):
        qi, ki, vi, gi = inp
        b = jnp.cumsum(gi, axis=2)
        o_inter = jnp.einsum('bhcd,bhde->bhce', qi * jnp.exp(b), state)
        diff = b[:, :, :, None, :] - b[:, :, None, :, :]
        decay = jnp.where(causal[:, :, None], jnp.exp(jnp.minimum(diff, 0.0)), 0.0)
        scores = jnp.einsum('bhid,bhjd,bhijd->bhij', qi, ki, decay)
        o_intra = jnp.einsum('bhij,bhje->bhie', scores, vi)
        b_last = b[:, :, -1:, :]
        k_dec = ki * jnp.exp(b_last - b)
        new_state = jnp.exp(b_last[:, :, 0, :])[..., None] * state + jnp.einsum('bhjd,bhje->bhde', k_dec, vi)
        return new_state, o_inter + o_intra

    state0 = jnp.zeros((B, H, dk, dv), jnp.float32)
    _, o = lax.scan(step, state0, (to_chunks(q), to_chunks(k), to_chunks(v), to_chunks(log_a)))
    return from_chunks(o)


def chunked_mlstm(q, k, v, i_pre, log_f):
    B, S, H, dk = q.shape
    dv = v.shape[-1]
    causal = jnp.tril(jnp.ones((CHUNK, CHUNK), dtype=bool))

    def gate_chunks(g):
        return g.reshape(B, S // CHUNK, CHUNK, H).transpose(1, 0, 3, 2)

    def step(carry, inp):
        c_state, n_state, m_state = carry
        qi, ki, vi, ii, fi = inp
        b = jnp.cumsum(fi, axis=-1)
        log_d = b[..., :, None] - b[..., None, :] + ii[..., None, :]
        log_d = jnp.where(causal, log_d, -jnp.inf)
        m_inter = b + m_state[..., None]
        m_i = jnp.maximum(m_inter, jnp.max(log_d, axis=-1))
        w_inter = jnp.exp(m_inter - m_i)
        s = jnp.einsum('bhid,bhjd->bhij', qi, ki) * jnp.exp(log_d - m_i[..., None])
        num = w_inter[..., None] * jnp.einsum('bhid,bhde->bhie', qi, c_state) + jnp.einsum('bhij,bhje->bhie', s, vi)
        den = w_inter * jnp.einsum('bhid,bhd->bhi', qi, n_state) + jnp.sum(s, axis=-1)
        h = num / jnp.maximum(jnp.abs(den), jnp.exp(-m_i))[..., None]
        b_last = b[..., -1]
        log_w = b_last[..., None] - b + ii
        m_new = jnp.maximum(b_last + m_state, jnp.max(log_w, axis=-1))
        w_prev = jnp.exp(b_last + m_state - m_new)
        wj = jnp.exp(log_w - m_new[..., None])
        c_new = w_prev[..., None, None] * c_state + jnp.einsum('bhj,bhjd,bhje->bhde', wj, ki, vi)
        n_new = w_prev[..., None] * n_state + jnp.einsum('bhj,bhjd->bhd', wj, ki)
        return (c_new, n_new, m_new), h

    carry0 = (jnp.zeros((B, H, dk, dv), jnp.float32), jnp.zeros((B, H, dk), jnp.float32), jnp.zeros((B, H), jnp.float32))
    _, h = lax.scan(step, carry0, (to_chunks(q), to_chunks(k), to_chunks(v), gate_chunks(i_pre), gate_chunks(log_f)))
    return from_chunks(h)


def causal_depthwise_conv(u, w, b):
    S = u.shape[1]
    up = jnp.pad(u, ((0, 0), (MLSTM_CONV - 1, 0), (0, 0)))
    out = b.astype(jnp.float32)
    for tap in range(MLSTM_CONV):
        out = out + up[:, tap:tap + S, :] * w[tap].astype(jnp.float32)
    return out


def block_diag_proj(t, w):
    B, S, C = t.shape
    tg = t.reshape(B, S, w.shape[0], w.shape[1])
    return jnp.einsum('bsgi,gio->bsgo', tg, w.astype(jnp.float32)).reshape(B, S, C)


def memory_cross_attention(h, m, wq, wk, wv, wo):
    B, S, D = h.shape
    M = m.shape[1]
    q = (h @ wq).reshape(B, S, XA_HEADS, XA_HEAD_DIM)
    k = (m @ wk).reshape(B, M, XA_HEADS, XA_HEAD_DIM)
    v = (m @ wv).reshape(B, M, XA_HEADS, XA_HEAD_DIM)
    s = jnp.einsum('bshd,bmhd->bhsm', q, k).astype(jnp.float32) * (XA_HEAD_DIM ** -0.5)
    p = jax.nn.softmax(s, axis=-1).astype(v.dtype)
    o = jnp.einsum('bhsm,bmhd->bshd', p, v).reshape(B, S, D)
    return o @ wo


def setup_inputs(seed: int = 0) -> dict:
    key = jax.random.key(seed)
    ks = jax.random.split(key, 26)

    def nrm(k, shape, scale):
        return jax.random.normal(k, shape, jnp.float32) * scale

    def gain(k, shape):
        return 1.0 + nrm(k, shape, 0.02)

    n_blk = MLSTM_WIDTH // MLSTM_QKV_BLOCK
    return {
        "x": nrm(ks[0], (BATCH, SEQ, D_MODEL), 1.0),
        "mem": nrm(ks[1], (BATCH, MEM_TOKENS, D_MODEL), 1.0),
        "norm_mix": gain(ks[2], (DEPTH, D_MODEL)),
        "w_in": nrm(ks[3], (DEPTH, D_MODEL, IN_COLS), D_MODEL ** -0.5),
        "hgrn_lb_logits": nrm(ks[4], (DEPTH, HGRN_HEADS * HGRN_DK), 0.5),
        "hgrn_norm": gain(ks[5], (DEPTH, HGRN_DV)),
        "gla_gate_up": nrm(ks[6], (DEPTH, GLA_RANK, GLA_HEADS * GLA_DK), GLA_RANK ** -0.5),
        "gla_gate_bias": nrm(ks[7], (DEPTH, GLA_HEADS * GLA_DK), 0.1),
        "gla_norm": gain(ks[8], (DEPTH, GLA_DV)),
        "mlstm_conv_w": nrm(ks[9], (DEPTH, MLSTM_CONV, MLSTM_WIDTH), MLSTM_CONV ** -0.5),
        "mlstm_conv_b": nrm(ks[10], (DEPTH, MLSTM_WIDTH), 0.02),
        "mlstm_wq": nrm(ks[11], (DEPTH, n_blk, MLSTM_QKV_BLOCK, MLSTM_QKV_BLOCK), MLSTM_QKV_BLOCK ** -0.5),
        "mlstm_wk": nrm(ks[12], (DEPTH, n_blk, MLSTM_QKV_BLOCK, MLSTM_QKV_BLOCK), MLSTM_QKV_BLOCK ** -0.5),
        "mlstm_wv": nrm(ks[13], (DEPTH, n_blk, MLSTM_QKV_BLOCK, MLSTM_QKV_BLOCK), MLSTM_QKV_BLOCK ** -0.5),
        "mlstm_igate_bias": nrm(ks[14], (DEPTH, MLSTM_HEADS), 0.1),
        "mlstm_fgate_bias": jnp.linspace(3.0, 6.0, MLSTM_HEADS, dtype=jnp.float32)[None, :] + nrm(ks[15], (DEPTH, MLSTM_HEADS), 0.1),
        "mlstm_skip": gain(ks[16], (DEPTH, MLSTM_WIDTH)),
        "mlstm_norm": gain(ks[17], (DEPTH, MLSTM_WIDTH)),
        "w_out": nrm(ks[18], (DEPTH, MIX_WIDTH, D_MODEL), MIX_WIDTH ** -0.5),
        "norm_xattn": gain(ks[19], (DEPTH, D_MODEL)),
        "norm_mem": gain(ks[20], (DEPTH, D_MODEL)),
        "xa_wq": nrm(ks[21], (DEPTH, D_MODEL, D_MODEL), D_MODEL ** -0.5),
        "xa_wk": nrm(ks[22], (DEPTH, D_MODEL, D_MODEL), D_MODEL ** -0.5),
        "xa_wv": nrm(ks[23], (DEPTH, D_MODEL, D_MODEL), D_MODEL ** -0.5),
        "xa_wo": nrm(ks[24], (DEPTH, D_MODEL, D_MODEL), D_MODEL ** -0.5),
        "norm_final": gain(ks[25], (D_MODEL,)),
    }


def reference(x, mem, norm_mix, w_in, hgrn_lb_logits, hgrn_norm, gla_gate_up, gla_gate_bias, gla_norm,
              mlstm_conv_w, mlstm_conv_b, mlstm_wq, mlstm_wk, mlstm_wv, mlstm_igate_bias, mlstm_fgate_bias,
              mlstm_skip, mlstm_norm, w_out, norm_xattn, norm_mem, xa_wq, xa_wk, xa_wv, xa_wo, norm_final):
    B, S, _ = x.shape
    split_idx = np.cumsum(IN_SPLITS)[:-1].tolist()
    lb_all = jnp.cumsum(jax.nn.softmax(hgrn_lb_logits.astype(jnp.float32), axis=0), axis=0)
    lb_all = lb_all - lb_all[0:1]

    for l in range(DEPTH):
        h = rms_norm(x, norm_mix[l])
        proj = jnp.einsum('bsd,dc->bsc', h, w_in[l]).astype(jnp.float32)
        (a_q, a_f, a_i, a_z, g_q, g_k, g_v, g_a, g_z, m_u, m_z, m_i, m_f) = jnp.split(proj, split_idx, axis=-1)

        lb = lb_all[l]
        log_f_a = jnp.logaddexp(jnp.log(lb), jnp.log1p(-lb) + jax.nn.log_sigmoid(a_f))
        k_a = (1.0 - lb) * jax.nn.sigmoid(-a_f)
        o_a = chunked_decay_linear_attention(split_heads(jax.nn.silu(a_q), HGRN_HEADS), split_heads(k_a, HGRN_HEADS),
                                             split_heads(a_i, HGRN_HEADS), split_heads(log_f_a, HGRN_HEADS))
        o_a = head_rms_norm(o_a, hgrn_norm[l]) * jax.nn.silu(a_z)

        log_alpha = jax.nn.log_sigmoid(g_a @ gla_gate_up[l].astype(jnp.float32) + gla_gate_bias[l].astype(jnp.float32)) / GLA_GATE_NORMALIZER
        o_b = chunked_decay_linear_attention(split_heads(g_q * (GLA_DK ** -0.5), GLA_HEADS), split_heads(g_k, GLA_HEADS),
                                             split_heads(g_v, GLA_HEADS), split_heads(log_alpha, GLA_HEADS))
        o_b = head_rms_norm(o_b, gla_norm[l]) * jax.nn.silu(g_z)

        conv = jax.nn.silu(causal_depthwise_conv(m_u, mlstm_conv_w[l], mlstm_conv_b[l]))
        q_c = block_diag_proj(conv, mlstm_wq[l])
        k_c = block_diag_proj(conv, mlstm_wk[l]) * (MLSTM_DH ** -0.5)
        v_c = block_diag_proj(m_u, mlstm_wv[l])
        i_pre = m_i + mlstm_igate_bias[l].astype(jnp.float32)
        log_f_c = jax.nn.log_sigmoid(m_f + mlstm_fgate_bias[l].astype(jnp.float32))
        o_c = chunked_mlstm(split_heads(q_c, MLSTM_HEADS), split_heads(k_c, MLSTM_HEADS), split_heads(v_c, MLSTM_HEADS), i_pre, log_f_c)
        o_c = (head_layer_norm(o_c, mlstm_norm[l]) + mlstm_skip[l].astype(jnp.float32) * conv) * jax.nn.silu(m_z)

        mix = jnp.concatenate([o_a, o_b, o_c], axis=-1).astype(x.dtype)
        x = x + jnp.einsum('bsc,cd->bsd', mix, w_out[l])

        x = x + memory_cross_attention(rms_norm(x, norm_xattn[l]), rms_norm(mem, norm_mem[l]),
                                       xa_wq[l], xa_wk[l], xa_wv[l], xa_wo[l])

    return rms_norm(x, norm_final)
```

```python
import contextlib
import numpy as np
import concourse.bass as bass
import concourse.mybir as mybir
from concourse.bass_utils import run_bass_kernel_spmd

F32 = mybir.dt.float32
BF16 = mybir.dt.bfloat16
AF = mybir.ActivationFunctionType
ALU = mybir.AluOpType
AX = mybir.AxisListType

D = 1024
DEPTH = 2
CH = 64
T = 512
NJ = T // 128
NCH = T // CH
EPS = 1e-6
MEM = 256
IN_SPLITS = [512, 512, 512, 512, 256, 256, 512, 16, 512, 512, 512, 4, 4]
OFF = np.concatenate([[0], np.cumsum(IN_SPLITS)]).tolist()
C_HQ, C_HF, C_GQ, C_GK, C_MU, C_GA, C_MI, C_MF = 0, 256, 512, 640, 768, 1024, 1040, 1042
C_TM = 1044
NCOL = C_TM + 1280


class V:
    __slots__ = ("buf", "ap")

    def __init__(self, buf, ap):
        self.buf = buf
        self.ap = ap

    def __getitem__(self, k):
        return V(self.buf, self.ap[k])

    def rr(self, s, **kw):
        return V(self.buf, self.ap.rearrange(s, **kw))

    def bc(self, shape):
        return V(self.buf, self.ap.to_broadcast(list(shape)))

    def unsq(self, axis):
        return V(self.buf, self.ap.unsqueeze(axis))

    def pbc(self, n):
        return V(self.buf, self.ap.partition_broadcast(n))

    @property
    def v(self):
        return self


class Buf:
    __slots__ = ("name", "t", "w", "r", "dsem", "dcnt", "excl")

    def __init__(self, name, t):
        self.name = name
        self.t = t
        self.excl = False
        self.w = {}
        self.r = {}
        self.dsem = None
        self.dcnt = 0

    def __getitem__(self, k):
        return V(self, self.t[k])

    @property
    def v(self):
        return V(self, self.t[:])


def _u(x):
    return x.ap if isinstance(x, V) else x


class Prog:
    def __init__(self, nc):
        self.nc = nc
        self.es = contextlib.ExitStack()
        self.eng = {"pe": nc.tensor, "act": nc.scalar, "dve": nc.vector, "pool": nc.gpsimd, "sp": nc.sync}
        self.sem = {}
        self.cnt = {}
        self.seen = {}
        for k in self.eng:
            self.sem[k] = self.es.enter_context(nc.semaphore("s_" + k))
            self.cnt[k] = 0
            self.seen[k] = {}
        self.nbuf = 0
        self.ninst = 0
        self.uid = 0
        self.stack = []
        self.root = self.es
        self.dcounts = {}
        self.nwait = {}

    def push(self):
        self.stack.append(self.es)
        self.es = contextlib.ExitStack()

    def pop(self):
        self.es.close()
        self.es = self.stack.pop()

    def sb(self, name, shape, dt=F32):
        self.uid += 1
        t = self.es.enter_context(self.nc.sbuf_tensor("sb%d_%s" % (self.uid, name), list(shape), dt))
        return Buf(name, t)

    def ps(self, name, shape, dt=F32):
        self.uid += 1
        t = self.es.enter_context(self.nc.psum_tensor("ps%d_%s" % (self.uid, name), list(shape), dt))
        b = Buf(name, t)
        b.excl = True
        return b

    def dram_in(self, name, shape, dt=F32):
        return Buf(name, self.nc.dram_tensor(name, list(shape), dt, kind="ExternalInput").ap())

    def dram_out(self, name, shape, dt=F32):
        return Buf(name, self.nc.dram_tensor(name, list(shape), dt, kind="ExternalOutput").ap())

    def dram_tmp(self, name, shape, dt=F32):
        return Buf(name, self.nc.dram_tensor(name, list(shape), dt, kind="Internal").ap())

    def alias(self, name, buf, ap):
        return V(buf, ap)

    def _wait(self, e, deps):
        for k, v in deps.items():
            if self.seen[e].get(k, 0) >= v:
                continue
            if k == e and e == "pe":
                continue
            self.eng[e].wait_ge(self.sem[k], v)
            self.nwait[e] = self.nwait.get(e, 0) + 1
            self.seen[e][k] = v

    @staticmethod
    def _deps(reads, writes, e=None):
        deps = {}
        for b in reads:
            for k, v in b.w.items():
                if deps.get(k, 0) < v:
                    deps[k] = v
            if b.excl:
                for k, v in b.r.items():
                    if k != e and deps.get(k, 0) < v:
                        deps[k] = v
        for b in writes:
            for d in (b.w, b.r):
                for k, v in d.items():
                    if deps.get(k, 0) < v:
                        deps[k] = v
        return deps

    def op(self, e, fn, ins, outs):
        reads = []
        for x in ins:
            if isinstance(x, V) and x.buf not in reads:
                reads.append(x.buf)
        writes = []
        for x in outs:
            if isinstance(x, V) and x.buf not in writes:
                writes.append(x.buf)
        self._wait(e, self._deps(reads, writes, e))
        ins_ = fn(self.eng[e])
        self.cnt[e] += 1
        c = self.cnt[e]
        ins_.then_inc(self.sem[e], 1)
        for b in writes:
            b.w = {e: c}
            b.r = {}
        for b in reads:
            if b not in writes:
                b.r[e] = c
        self.ninst += 1

    def dma(self, e, out, in_):
        reads = [in_.buf]
        writes = [out.buf]
        self._wait(e, self._deps(reads, writes))
        sbuf = out.buf
        if sbuf.dsem is None:
            key = "d%d" % self.nbuf
            self.nbuf += 1
            self.sem[key] = self.root.enter_context(self.nc.semaphore(key))
            sbuf.dsem = key
        self.eng[e].dma_start(out=out.ap, in_=in_.ap).then_inc(self.sem[sbuf.dsem], 16)
        sbuf.dcnt += 16
        self.dcounts[sbuf.dsem] = sbuf.dcnt
        out.buf.w = {sbuf.dsem: sbuf.dcnt}
        out.buf.r = {}
        in_.buf.r[sbuf.dsem] = sbuf.dcnt
        self.ninst += 1

    def barrier(self):
        deps = {k: v for k, v in self.cnt.items() if v > 0}
        deps.update(self.dcounts)
        for e in self.eng:
            self._wait(e, {k: v for k, v in deps.items() if k != e})

    def collective(self, kind, op, groups, in_buf, out_buf):
        e = "pool"
        self._wait(e, self._deps([in_buf], [out_buf], e))
        if out_buf.dsem is None:
            key = "c%d" % self.nbuf
            self.nbuf += 1
            self.sem[key] = self.root.enter_context(self.nc.semaphore(key))
            out_buf.dsem = key
        self.nc.gpsimd.collective_compute(kind, op, replica_groups=groups, ins=[in_buf.t.opt()],
                                          outs=[out_buf.t.opt()]).then_inc(self.sem[out_buf.dsem])
        out_buf.dcnt += 1
        self.dcounts[out_buf.dsem] = out_buf.dcnt
        out_buf.w = {out_buf.dsem: out_buf.dcnt}
        out_buf.r = {}
        in_buf.r[out_buf.dsem] = out_buf.dcnt
        self.ninst += 1

    def finish(self, e, bufs):
        self._wait(e, self._deps((), bufs))

    def act(self, out, in_, func, bias=None, scale=None, accum=None, e="act"):
        kw = {}
        if bias is not None:
            kw["bias"] = _u(bias)
        if scale is not None:
            kw["scale"] = _u(scale)
        if accum is not None:
            kw["accum_out"] = _u(accum)
        outs = [out] + ([accum] if accum is not None else [])
        self.op(e, lambda g: g.activation(out=out.ap, in_=in_.ap, func=func, **kw), [in_, bias, scale], outs)

    def tt(self, e, out, in0, in1, op):
        self.op(e, lambda g: g.tensor_tensor(out=out.ap, in0=in0.ap, in1=in1.ap, op=op), [in0, in1], [out])

    def ts(self, e, out, in0, s1, op0, s2=None, op1=None):
        if op1 is None:
            self.op(e, lambda g: g.tensor_scalar(out=out.ap, in0=in0.ap, scalar1=_u(s1), scalar2=None, op0=op0),
                    [in0, s1], [out])
        else:
            self.op(e, lambda g: g.tensor_scalar(out=out.ap, in0=in0.ap, scalar1=_u(s1), scalar2=_u(s2), op0=op0, op1=op1),
                    [in0, s1, s2], [out])

    def stt(self, e, out, in0, scalar, in1, op0, op1):
        self.op(e, lambda g: g.scalar_tensor_tensor(out=out.ap, in0=in0.ap, scalar=_u(scalar), in1=in1.ap, op0=op0, op1=op1),
                [in0, scalar, in1], [out])

    def copy(self, e, out, in_):
        if e == "act":
            self.act(out, in_, AF.Copy)
        else:
            self.op(e, lambda g: g.tensor_copy(out=out.ap, in_=in_.ap), [in_], [out])

    def memset(self, e, out, val):
        self.op(e, lambda g: g.memset(out.ap, val), [], [out])

    def scan(self, out, d0, d1, init, op0, op1):
        self.op("dve", lambda g: g.tensor_tensor_scan(out=out.ap, data0=d0.ap, data1=d1.ap, initial=_u(init), op0=op0, op1=op1),
                [d0, d1, init], [out])

    def reduce(self, e, out, in_, op, axis=AX.X):
        self.op(e, lambda g: g.tensor_reduce(out=out.ap, in_=in_.ap, axis=axis, op=op), [in_], [out])

    def recip(self, out, in_):
        self.op("dve", lambda g: g.reciprocal(out=out.ap, in_=in_.ap), [in_], [out])

    def mm(self, out, lhsT, rhs, start=True, stop=True):
        self.op("pe", lambda g: g.matmul(out.ap, lhsT=lhsT.ap, rhs=rhs.ap, start=start, stop=stop), [lhsT, rhs], [out])

    def tr(self, out, in_, ident):
        self.op("pe", lambda g: g.transpose(out=out.ap, in_=in_.ap, identity=ident.ap), [in_, ident], [out])

    def rstd(self, out, in_, scale, tmp):
        self.ts("dve", tmp, in_, scale, ALU.mult, EPS, ALU.add)
        self.act(tmp, tmp, AF.Ln)
        self.act(out, tmp, AF.Exp, scale=-0.5)


class Res:
    pass


def setup_common(P, consts_d):
    R = Res()
    R.ident = P.sb("ident", [128, 128])
    R.identb = P.sb("identb", [128, 128], BF16)
    R.maskT = P.sb("maskT", [128, 128])
    R.bdmask = P.sb("bdmask", [128, 32])
    R.sel = P.sb("sel", [2, 2, 128])
    P.dma("sp", R.ident.v, consts_d["ident"].v)
    P.dma("sp", R.maskT.v, consts_d["maskT"].v)
    P.dma("sp", R.bdmask.v, consts_d["bdmask"].v)
    P.dma("sp", R.sel.v, consts_d["sel"].v)
    P.copy("dve", R.identb.v, R.ident.v)
    R.pX = [P.ps("pX%d" % i, [128, 512]) for i in range(2)]
    bT = P.ps("bT", [128, 1024], BF16)
    R.pT = [P.alias("pT%d" % i, bT, bT.t[:, i * 512:(i + 1) * 512]) for i in range(2)]
    bO = [P.ps("bO%d" % i, [128, 512]) for i in range(2)]
    R.pO = [P.alias("pO%d" % i, bO[i % 2], bO[i % 2].t[:, (i // 2) * 160:(i // 2) * 160 + 132]) for i in range(6)]
    bS = P.ps("bS", [128, 512])
    R.pS = [P.alias("pS%d" % i, bS, bS.t[:, i * 128:(i + 1) * 128]) for i in range(4)]
    bKV = P.ps("bKV", [128, 512])
    R.pKV = [P.alias("pKV%d" % i, bKV, bKV.t[:, i * 160:i * 160 + 132]) for i in range(3)]
    bSm = P.ps("bSm", [128, 512])
    R.pSm = P.alias("pSm", bSm, bSm.t[:, 0:64])
    R.px_i = 0
    R.ps_i = 0
    R.pkv_i = 0
    R.pt_i = 0
    return R


def nxt(R, what):
    if what == "pX":
        R.px_i += 1
        return R.pX[R.px_i % len(R.pX)]
    if what == "pS":
        R.ps_i += 1
        return R.pS[R.ps_i % len(R.pS)]
    if what == "pKV":
        R.pkv_i += 1
        return R.pKV[R.pkv_i % len(R.pKV)]
    if what == "pT":
        R.pt_i += 1
        return R.pT[R.pt_i % len(R.pT)]
    raise KeyError(what)


def rms_to_hT(P, R, xt, hT, gcol, ntok_j, scr):
    nj = ntok_j
    ss, tmp, rs, junk, xn = scr
    P.memset("pool", ss.v, 0.0)
    for j in range(nj):
        P.act(junk.v, xt[:, j, :], AF.Square, accum=ss[:, j:j + 1])
    P.rstd(rs[:, 0:nj], ss[:, 0:nj], 1.0 / D, tmp[:, 0:nj])
    for j in range(nj):
        if j % 2 == 0:
            P.act(xn[:, j, :], xt[:, j, :], AF.Copy, scale=rs[:, j:j + 1])
        else:
            P.ts("dve", xn[:, j, :], xt[:, j, :], rs[:, j:j + 1], ALU.mult)
    for k in range(8):
        pt = nxt(R, "pT")
        for j in range(nj):
            P.tr(pt[:, j * 128:(j + 1) * 128], xn[:, j, k * 128:(k + 1) * 128], R.identb.v)
        P.ts("dve", hT[:, k, 0:nj * 128], pt[:, 0:nj * 128], gcol[:, k:k + 1], ALU.mult)


def load_cast_weight(P, w_sb, w_d, nk, ncols, stage, eng_cycle):
    for k in range(nk):
        st = stage[k % len(stage)]
        P.dma("sp", st[:, 0:ncols], w_d[k * 128:(k + 1) * 128, :])
        P.copy(eng_cycle[k % len(eng_cycle)], w_sb[:, k, :], st[:, 0:ncols])


STOP = 99


def emit_mixer(P, R, S, x_d, y_d, wd, layer):
    NT = S // T
    stage = [P.sb("wstage%d" % i, [128, NCOL]) for i in range(1)]
    W = P.sb("W", [128, 8, NCOL], BF16)
    load_cast_weight(P, W, wd["wc"], 8, NCOL, stage, ["pool", "dve"])
    Wo = P.sb("Wo", [128, 6, D], BF16)
    load_cast_weight(P, Wo, wd["wo"], 6, D, stage, ["pool", "dve"])
    gmix = P.sb("gmix", [128, 8]); P.dma("sp", gmix.v, wd["gmix"].v)
    lbl = P.sb("lbl", [128, 2, DEPTH]); P.dma("sp", lbl.v, wd["lblog"].v)
    lbe = P.sb("lbe", [128, 2, DEPTH]); P.act(lbe.v, lbl.v, AF.Exp)
    lbtot = P.sb("lbtot", [128, 2]); P.reduce("dve", lbtot.v, lbe.v, ALU.add)
    lbr = P.sb("lbr", [128, 2]); P.recip(lbr.v, lbtot.v)
    lb = P.sb("lb", [128, 2]); P.memset("pool", lb.v, 0.0)
    for l2 in range(1, layer + 1):
        P.tt("dve", lb.v, lb.v, lbe[:, :, l2], ALU.add)
    P.tt("dve", lb.v, lb.v, lbr.v, ALU.mult)
    oml = P.sb("oml", [128, 2]); P.ts("dve", oml.v, lb.v, -1.0, ALU.mult, 1.0, ALU.add)
    lbm1 = P.sb("lbm1", [128, 2]); P.ts("dve", lbm1.v, lb.v, -1.0, ALU.add)
    grow = P.sb("grow", [128, 768])
    for i, nm in enumerate(["hnorm", "hnorm", "gnorm", "gnorm"]):
        P.dma("sp", grow[:, i * 128:(i + 1) * 128], wd[nm].v.pbc(128))
    P.dma("sp", grow[:, 512:768], wd["mnorm"].v.pbc(128))
    gup = P.sb("gup", [16, 128]); P.dma("sp", gup.v, wd["gup"].v)
    gbias = P.sb("gbias", [128, 1]); P.dma("sp", gbias.v, wd["gbias"].v)
    convw = P.sb("convw", [128, 2, 4]); P.dma("sp", convw.v, wd["convw"].v)
    convb = P.sb("convb", [128, 2]); P.dma("sp", convb.v, wd["convb"].v)
    skip = P.sb("skip", [128, 2]); P.dma("sp", skip.v, wd["skip"].v)
    bi = P.sb("bi", [2, 1]); P.dma("sp", bi.v, wd["bi"].v)
    bf = P.sb("bf", [2, 1]); P.dma("sp", bf.v, wd["bf"].v)
    BDq, BDks, BDv = [], [], []
    wsm = {}
    for nm in ("wq", "wk", "wv"):
        wsm[nm] = P.sb("wsm_" + nm, [128, 2, 4]); P.dma("sp", wsm[nm].v, wd[nm].v)
    for h in range(2):
        bq = P.sb("BDq%d" % h, [128, 128], BF16)
        bks = P.sb("BDks%d" % h, [128, 256], BF16)
        bv = P.sb("BDv%d" % h, [128, 128], BF16)
        for dst, nm in ((bq.v, "wq"), (bks[:, 0:128], "wk"), (bv.v, "wv")):
            P.tt("pool", dst.rr("p (g o) -> p g o", o=4),
                 wsm[nm][:, h, :].unsq(1).bc([128, 32, 4]),
                 R.bdmask.v.unsq(2).bc([128, 32, 4]), ALU.mult)
        P.ts("pool", bks[:, 128:256], R.ident.v, skip[:, h:h + 1], ALU.mult)
        BDq.append(bq); BDks.append(bks); BDv.append(bv)

    st_f = {}
    st_b = {}
    for nm, ncol in (("h0", 128), ("h1", 128), ("g", 128), ("m0", 129), ("m1", 129)):
        st_f[nm] = P.sb("stf_" + nm, [128, ncol])
        P.memset("pool", st_f[nm].v, 0.0)
    for nm, ncol in (("h0", 128), ("h1", 128), ("g0", 128), ("g1", 128), ("m0", 129), ("m1", 129)):
        st_b[nm] = [P.sb("stb_%s_%d" % (nm, i), [128, ncol], BF16) for i in range(2)]
        P.memset("pool", st_b[nm][0].v, 0.0)
    mstP = P.sb("mstP", [128, 129])
    mstP2 = P.sb("mstP2", [128, 129])
    uext = [P.sb("uext%d" % h, [128, 3 + T]) for h in range(2)]
    for h in range(2):
        P.memset("pool", uext[h][:, 0:3], 0.0)
    m0 = P.sb("m0", [2, 1]); P.memset("pool", m0.v, 0.0)

    xt = P.sb("xt", [128, NJ, D])
    scr = (P.sb("ss", [128, NJ]), P.sb("sstmp", [128, NJ]), P.sb("rs", [128, NJ]),
           P.sb("junk", [128, D], BF16), P.sb("xn", [128, NJ, D], BF16))
    hT = P.sb("hT", [128, 8, T], BF16)
    vtm = P.sb("vtm", [128, NJ, 512], BF16)
    ztm = P.sb("ztm", [128, NJ, 768])
    osb = xt
    ones = P.sb("ones", [128, T]); P.memset("pool", ones.v, 1.0)
    tmpA = P.sb("tmpA", [128, 8, T])
    tq, tsg, tg, tkk, tB, tDm, teD, teDn = [tmpA[:, i, :] for i in range(8)]
    blk = []
    for i in range(3):
        b = Res()
        b.q, b.sg, b.g, b.kk, b.B, b.Dm, b.eD, b.eDn = tq, tsg, tg, tkk, tB, tDm, teD, teDn
        b.qt = P.sb("qt%d" % i, [128, T], BF16)
        b.kt = P.sb("kt%d" % i, [128, T], BF16)
        b.qe = P.sb("qe%d" % i, [128, T], BF16)
        b.ktm = P.sb("ktm%d" % i, [128, NJ, 128], BF16)
        b.prev = P.sb("prev%d" % i, [128, NCH])
        b.d3 = P.sb("d3%d" % i, [128, 3, NCH])
        b.e3 = P.sb("e3%d" % i, [128, 3, NCH])
        blk.append(b)
    ga_sb = P.sb("ga_sb", [16, T])
    tmpkv = [P.sb("tmpkv%d" % i, [128, 128]) for i in range(3)]
    scT = [P.sb("scT%d" % i, [128, 128], BF16) for i in range(6)]
    ML = []
    for h in range(2):
        m = Res()
        m.acc = P.sb("cacc%d" % h, [128, T])
        m.conv = P.sb("conv%d" % h, [128, T], BF16)
        m.ub = P.sb("ub%d" % h, [128, T], BF16)
        m.qT = P.sb("mqT%d" % h, [128, T], BF16)
        m.kT = P.sb("mkT%d" % h, [128, T], BF16)
        m.khat = P.sb("khat%d" % h, [128, NJ, 128], BF16)
        m.vaug = P.sb("vaug%d" % h, [128, NJ, 132], BF16)
        P.memset("pool", m.vaug.v, 1.0)
        m.wp = P.sb("wp%d" % h, [128, NCH])
        ML.append(m)
    sctm = P.sb("sctm", [128, NJ, 256])
    g_sf = P.sb("g_sf", [2, T]); g_lf = P.sb("g_lf", [2, T]); g_B = P.sb("g_B", [2, T])
    g_a = P.sb("g_a", [2, T]); g_wj = P.sb("g_wj", [2, T]); g_thr = P.sb("g_thr", [2, T])
    g_am = P.sb("g_am", [2, NCH]); g_R = P.sb("g_R", [2, NCH]); g_mp = P.sb("g_mp", [2, NCH]); g_wprev = P.sb("g_wprev", [2, NCH])
    gtm = P.sb("gtm", [128, NJ, 2, 2])
    den = P.sb("den", [128, 4])
    st6 = P.sb("st6", [128, NJ, 6]); st6b = P.sb("st6b", [128, NJ, 6]); st6c = P.sb("st6c", [128, NJ, 6])
    msum = P.sb("msum", [128, NJ, 2]); mmean = P.sb("mmean", [128, NJ, 2]); mvar = P.sb("mvar", [128, NJ, 2])
    osq = tmpA.v.rr("p a t -> p (a t)")[:, 0:NJ * 768].rr("p (j c) -> p j c", c=768)
    mixb = scr[4]
    mixT = hT
    ysb = [P.sb("ysb%d" % i, [128, D]) for i in range(2)]
    def c3(v):
        return v.rr("p (c t) -> p c t", t=CH)

    if STOP <= 1:
        return
    for it in range(NT):
        t0 = it * T
        P.dma("sp", xt.v, x_d(t0).rr("(j p) d -> p j d", p=128))
        rms_to_hT(P, R, xt, hT, gmix, NJ, scr)
        if STOP <= 2:
            continue

        def proj_fm(c0, M):
            px = nxt(R, "pX")
            for k in range(8):
                P.mm(px[0:M, :], W[:, k, c0:c0 + M], hT[:, k, :], start=(k == 0), stop=(k == 7))
            return px

        def decay_block(b, B_scale, is_gla):
            P.scan(b.B, ones.v, b.g, 0.0, ALU.mult, ALU.add)
            B3 = c3(b.B)
            P.tt("dve", c3(b.Dm), B3, B3[:, :, 32:33].bc([128, NCH, CH]), ALU.subtract)
            P.act(b.eD, b.Dm, AF.Exp, scale=B_scale)
            P.act(b.eDn, b.Dm, AF.Exp, scale=-B_scale)
            P.memset("pool", b.prev[:, 0:1], 0.0)
            P.copy("pool", b.prev[:, 1:NCH], B3[:, 0:NCH - 1, 63])
            P.tt("pool", b.d3[:, 0, :], B3[:, :, 32], b.prev.v, ALU.subtract)
            P.tt("pool", b.d3[:, 1, :], B3[:, :, 63], b.prev.v, ALU.subtract)
            P.tt("pool", b.d3[:, 2, :], B3[:, :, 63], B3[:, :, 32], ALU.subtract)
            P.act(b.e3.v, b.d3.v, AF.Exp, scale=B_scale)

        for h in range(2):
            b = blk[h]
            px = proj_fm(C_HQ + h * 128, 128)
            P.act(b.q, px.v, AF.Silu)
            px = proj_fm(C_HF + h * 128, 128)
            P.act(b.sg, px.v, AF.Sigmoid)
            P.act(b.g, b.sg, AF.Ln, bias=lb[:, h:h + 1], scale=oml[:, h:h + 1])
            P.ts("dve", b.kk, b.sg, lbm1[:, h:h + 1], ALU.mult, oml[:, h:h + 1], ALU.add)
            decay_block(b, 1.0, False)
            P.tt("pool", b.qt.v, b.q, b.eD, ALU.mult)
            P.tt("pool", b.kt.v, b.kk, b.eDn, ALU.mult)
            P.tt("pool", c3(b.qe.v), c3(b.qt.v), b.e3[:, 0, :].unsq(2).bc([128, NCH, CH]), ALU.mult)
        if STOP <= 2.1:
            continue
        b = blk[2]
        px = proj_fm(C_GA, 16)
        P.copy("act", ga_sb.v, px[0:16, :])
        px = nxt(R, "pX")
        P.mm(px.v, gup.v, ga_sb.v)
        P.act(b.sg, px.v, AF.Sigmoid, bias=gbias[:, 0:1])
        P.act(b.g, b.sg, AF.Ln)
        decay_block(b, 1.0 / 16.0, True)
        px = proj_fm(C_GQ, 128)
        P.stt("dve", b.qt.v, px.v, 0.125, b.eD, ALU.mult, ALU.mult)
        px = proj_fm(C_GK, 128)
        P.tt("dve", b.kt.v, px.v, b.eDn, ALU.mult)
        P.tt("pool", c3(b.qe.v), c3(b.qt.v), b.e3[:, 0, :].unsq(2).bc([128, NCH, CH]), ALU.mult)
        if STOP <= 2.2:
            continue
        for i in range(3):
            b = blk[i]
            pt = nxt(R, "pT")
            for j in range(NJ):
                P.tr(pt[:, j * 128:(j + 1) * 128], b.kt[:, j * 128:(j + 1) * 128], R.identb.v)
            P.copy("act", b.ktm.v.rr("p j d -> p (j d)"), pt.v)
        if STOP <= 2.3:
            continue
        for h in range(2):
            m = ML[h]
            px = proj_fm(C_MU + h * 128, 128)
            P.copy("act", uext[h][:, 3:3 + T], px.v)
            P.ts("dve", m.acc.v, uext[h][:, 0:T], convw[:, h, 0:1], ALU.mult, convb[:, h:h + 1], ALU.add)
            for tap in range(1, 4):
                P.stt("dve", m.acc.v, uext[h][:, tap:tap + T], convw[:, h, tap:tap + 1], m.acc.v, ALU.mult, ALU.add)
            P.act(m.conv.v, m.acc.v, AF.Silu)
            P.copy("pool", m.ub.v, uext[h][:, 3:3 + T])
            P.copy("pool", uext[h][:, 0:3], uext[h][:, T:T + 3])
            px = nxt(R, "pX"); P.mm(px.v, BDq[h].v, m.conv.v)
            P.copy("act", m.qT.v, px.v)
            px = nxt(R, "pX"); P.mm(px.v, BDks[h][:, 0:128], m.conv.v)
            P.copy("act", m.kT.v, px.v)
        if STOP <= 2.4:
            continue
        px = proj_fm(C_MF, 2)
        P.act(g_sf.v, px[0:2, :], AF.Sigmoid, bias=bf[:, 0:1])
        P.act(g_lf.v, g_sf.v, AF.Ln)
        P.scan(g_B.v, ones[0:2, :], g_lf.v, 0.0, ALU.mult, ALU.add)
        px = proj_fm(C_MI, 2)
        P.stt("dve", g_a.v, px[0:2, :], bi[:, 0:1], g_B.v, ALU.add, ALU.subtract)
        P.reduce("dve", g_am.v, c3(g_a.v), ALU.max)
        P.scan(g_R.v, g_am.v, g_am.v, m0[:, 0:1], ALU.max, ALU.max)
        P.copy("dve", g_mp[:, 0:1], m0.v)
        P.copy("dve", g_mp[:, 1:NCH], g_R[:, 0:NCH - 1])
        P.tt("dve", g_wprev.v, g_mp.v, g_R.v, ALU.subtract)
        P.act(g_wprev.v, g_wprev.v, AF.Exp)
        P.tt("dve", c3(g_wj.v), c3(g_a.v), g_R.v.unsq(2).bc([2, NCH, CH]), ALU.subtract)
        P.act(g_wj.v, g_wj.v, AF.Exp)
        P.tt("dve", c3(g_thr.v), c3(g_B.v), g_R.v.unsq(2).bc([2, NCH, CH]), ALU.add)
        P.act(g_thr.v, g_thr.v, AF.Exp, scale=-1.0)
        P.tt("dve", m0.v, g_R[:, NCH - 1:NCH], g_B[:, T - 1:T], ALU.add)
        if STOP <= 2.5:
            continue
        for j in range(NJ):
            P.tr(R.pSm[:, j * 4:j * 4 + 2], g_wj[:, j * 128:(j + 1) * 128], R.ident[0:2, 0:2])
            P.tr(R.pSm[:, j * 4 + 2:j * 4 + 4], g_thr[:, j * 128:(j + 1) * 128], R.ident[0:2, 0:2])
        for h in range(2):
            P.mm(R.pSm[:, 16 + h * NCH:16 + (h + 1) * NCH], R.sel[:, h, :], g_wprev.v)
        P.copy("dve", gtm.v.rr("p j a h -> p (j a h)"), R.pSm[:, 0:16])
        P.ts("dve", gtm[:, :, 0, :], gtm[:, :, 0, :], float(128 ** -0.5), ALU.mult)
        for h in range(2):
            P.copy("dve", ML[h].wp.v, R.pSm[:, 16 + h * NCH:16 + (h + 1) * NCH])
        if STOP <= 2.6:
            continue
        for h in range(2):
            m = ML[h]
            for j2 in range(NJ // 2):
                px = nxt(R, "pX")
                for jj in range(2):
                    j = j2 * 2 + jj
                    P.mm(px[:, jj * 256:(jj + 1) * 256], m.conv[:, j * 128:(j + 1) * 128], BDks[h].v)
                for jj in range(2):
                    j = j2 * 2 + jj
                    P.act(m.khat[:, j, :], px[:, jj * 256:jj * 256 + 128], AF.Copy, scale=gtm[:, j, 0, h:h + 1])
                    P.copy("dve", sctm[:, j, h * 128:(h + 1) * 128], px[:, jj * 256 + 128:(jj + 1) * 256])
            if STOP <= 2.65:
                continue
            px = nxt(R, "pX")
            for j in range(NJ):
                P.mm(px[:, j * 128:(j + 1) * 128], m.ub[:, j * 128:(j + 1) * 128], BDv[h].v)
            P.copy("act", m.vaug[:, :, 0:128], px.v.rr("p (j e) -> p j e", e=128))
        if STOP <= 2.7:
            continue
        for j in range(NJ):
            for gi, (c0, n) in enumerate(((C_TM, 512), (C_TM + 512, 512), (C_TM + 1024, 256))):
                px = nxt(R, "pX")
                for k in range(8):
                    P.mm(px[:, 0:n], hT[:, k, j * 128:(j + 1) * 128], W[:, k, c0:c0 + n], start=(k == 0), stop=(k == 7))
                if gi == 0:
                    P.copy("dve", vtm[:, j, :], px.v)
                elif gi == 1:
                    P.act(ztm[:, j, 0:512], px.v, AF.Silu)
                else:
                    P.act(ztm[:, j, 512:768], px[:, 0:256], AF.Silu)

        if STOP <= 3:
            continue
        dl = [(blk[0], 0, 128, st_f["h0"], st_b["h0"], 0, 0),
              (blk[1], 0, 128, st_f["h1"], st_b["h1"], 128, 128),
              (blk[2], 0, 64, st_f["g"], st_b["g0"], 256, 256),
              (blk[2], 64, 64, st_f["g"], st_b["g1"], 384, 384)]
        for pr in range(NJ):
            tok = slice(pr * 128, (pr + 1) * 128)
            for hi, (b, pb, K, sf, sbb, vc, oc) in enumerate(dl):
                pS = nxt(R, "pS")
                P.mm(pS.v, b.kt[pb:pb + K, tok], b.qt[pb:pb + K, tok])
                P.tt("dve", scT[hi].v, pS.v, R.maskT.v, ALU.mult)
            for h in range(2):
                m = ML[h]
                pS = nxt(R, "pS")
                P.mm(pS.v, m.kT[:, tok], m.qT[:, tok])
                P.stt("dve", scT[4 + h].v, pS.v, gtm[:, pr, 0, h:h + 1], R.maskT.v, ALU.mult, ALU.mult)
            c0, c1 = 2 * pr, 2 * pr + 1
            gc0 = it * NCH + c0
            r0, r1 = slice(0, 64), slice(64, 128)
            t0c, t1c = slice(c0 * CH, (c0 + 1) * CH), slice(c1 * CH, (c1 + 1) * CH)

            def dl_update(hi, c, rows, dst_par):
                b, pb, K, sf, sbb, vc, oc = dl[hi]
                pk = slice(pb, pb + K)
                pKV = nxt(R, "pKV")
                P.mm(pKV[pk, 0:128], b.ktm[rows, pr, pk], vtm[rows, pr, vc:vc + 128])
                tk = tmpkv[hi % 3]
                P.act(tk[pk, :], pKV[pk, 0:128], AF.Copy, scale=b.e3[pk, 2, c:c + 1])
                P.stt("dve", sf[pk, :], sf[pk, :], b.e3[pk, 1, c:c + 1], tk[pk, :], ALU.mult, ALU.add)
                P.copy("pool", sbb[dst_par][pk, :], sf[pk, :])

            for hi in range(4):
                dl_update(hi, c0, r0, 1)
            for h in range(2):
                m = ML[h]
                sf = st_f["m%d" % h]; sbb = st_b["m%d" % h]; sP = mstP if h == 0 else mstP2
                P.ts("dve", sP.v, sf.v, m.wp[:, c0:c0 + 1], ALU.mult)
                P.act(sbb[0].v, sf.v, AF.Copy, scale=m.wp[:, c0:c0 + 1])
                pKV = nxt(R, "pKV")
                P.mm(pKV[:, 0:129], m.khat[r0, pr, :], m.vaug[r0, pr, 0:129])
                P.tt("dve", sf.v, sP.v, pKV[:, 0:129], ALU.add)
                P.ts("dve", sP.v, sf.v, m.wp[:, c1:c1 + 1], ALU.mult)
                P.act(sbb[1].v, sf.v, AF.Copy, scale=m.wp[:, c1:c1 + 1])
            for hi, (b, pb, K, sf, sbb, vc, oc) in enumerate(dl):
                pk = slice(pb, pb + K)
                pO = R.pO[hi]
                P.mm(pO[r0, 0:128], b.qe[pk, t0c], sbb[0][pk, :], start=True, stop=False)
                P.mm(pO[r1, 0:128], b.qe[pk, t1c], sbb[1][pk, :], start=True, stop=False)
                P.mm(pO[:, 0:128], scT[hi].v, vtm[:, pr, vc:vc + 128], start=False, stop=True)
                P.copy("act", osb[:, pr, oc:oc + 128], pO[:, 0:128])
            for h in range(2):
                m = ML[h]
                sbb = st_b["m%d" % h]
                pO = R.pO[4 + h]
                P.mm(pO[r0, 0:129], m.qT[:, t0c], sbb[0].v, start=True, stop=False)
                P.mm(pO[r1, 0:129], m.qT[:, t1c], sbb[1].v, start=True, stop=False)
                P.mm(pO[:, 0:129], scT[4 + h].v, m.vaug[:, pr, 0:129], start=False, stop=True)
                P.act(den[:, 0:1], pO[:, 128:129], AF.Abs)
                P.tt("dve", den[:, 1:2], den[:, 0:1], gtm[:, pr, 1, h:h + 1], ALU.max)
                P.recip(den[:, 2:3], den[:, 1:2])
                P.act(osb[:, pr, 512 + h * 128:512 + (h + 1) * 128], pO[:, 0:128], AF.Copy, scale=den[:, 2:3])
            for hi in range(4):
                dl_update(hi, c1, r1, 0)
            for h in range(2):
                m = ML[h]
                sf = st_f["m%d" % h]; sP = mstP if h == 0 else mstP2
                pKV = nxt(R, "pKV")
                P.mm(pKV[:, 0:129], m.khat[r1, pr, :], m.vaug[r1, pr, 0:129])
                P.tt("dve", sf.v, sP.v, pKV[:, 0:129], ALU.add)

        if STOP <= 4:
            continue
        P.tt("pool", osq, osb[:, :, 0:768], osb[:, :, 0:768], ALU.mult)
        P.reduce("dve", st6.v, osq.rr("p j (h e) -> p j h e", e=128), ALU.add)
        P.reduce("dve", msum.v, osb[:, :, 512:768].rr("p j (h e) -> p j h e", e=128), ALU.add)
        P.ts("dve", mmean.v, msum.v, 1.0 / 128, ALU.mult)
        P.tt("dve", mvar.v, mmean.v, mmean.v, ALU.mult)
        P.ts("dve", st6b.v, st6.v, 1.0 / 128, ALU.mult)
        P.tt("dve", st6b[:, :, 4:6], st6b[:, :, 4:6], mvar.v, ALU.subtract)
        P.ts("dve", st6b.v, st6b.v, EPS, ALU.add)
        P.act(st6c.v, st6b.v, AF.Ln)
        P.act(st6c.v, st6c.v, AF.Exp, scale=-0.5)
        o4 = osb[:, :, 512:768].rr("p j (h e) -> p j h e", e=128)
        P.tt("pool", o4, o4, mmean.v.unsq(3).bc([128, NJ, 2, 128]), ALU.subtract)
        oall = osb[:, :, 0:768].rr("p j (h e) -> p j h e", e=128)
        P.tt("dve", oall, oall, st6c.v.unsq(3).bc([128, NJ, 6, 128]), ALU.mult)
        P.tt("pool", osb[:, :, 0:768], osb[:, :, 0:768], grow.v.unsq(1).bc([128, NJ, 768]), ALU.mult)
        P.tt("dve", osb[:, :, 512:768], osb[:, :, 512:768], sctm.v, ALU.add)
        P.tt("pool", mixb[:, :, 0:768], osb[:, :, 0:768], ztm.v, ALU.mult)
        for cc in range(6):
            pt = nxt(R, "pT")
            for j in range(NJ):
                P.tr(pt[:, j * 128:(j + 1) * 128], mixb[:, j, cc * 128:(cc + 1) * 128], R.identb.v)
            P.copy("act" if cc % 2 == 0 else "dve", mixT[:, cc, :], pt.v)
        for j in range(NJ):
            yb = ysb[j % 2]
            for nh in range(2):
                px = nxt(R, "pX")
                for cc in range(6):
                    P.mm(px.v, mixT[:, cc, j * 128:(j + 1) * 128], Wo[:, cc, nh * 512:(nh + 1) * 512], start=(cc == 0), stop=(cc == 5))
                P.copy("act" if nh == 0 else "dve", yb[:, nh * 512:(nh + 1) * 512], px.v)
            P.dma("sp", y_d[t0 + j * 128:t0 + (j + 1) * 128, :], yb.v)


def consts_np():
    ident = np.eye(128, dtype=np.float32)
    maskT = np.zeros((128, 128), np.float32)
    for a in range(2):
        blk = np.triu(np.ones((CH, CH), np.float32))
        maskT[a * CH:(a + 1) * CH, a * CH:(a + 1) * CH] = blk
    bdmask = np.kron(np.eye(32, dtype=np.float32), np.ones((4, 1), np.float32))
    sel = np.zeros((2, 2, 128), np.float32)
    sel[0, 0, :] = 1.0
    sel[1, 1, :] = 1.0
    return {"ident": ident, "maskT": maskT, "bdmask": bdmask, "sel": sel}


CONST_SHAPES = {"ident": [128, 128], "maskT": [128, 128], "bdmask": [128, 32], "sel": [2, 2, 128]}

MIX_SHAPES = {
    "wc": [D, NCOL], "wo": [768, D], "gmix": [128, 8], "lblog": [128, 2, DEPTH], "hnorm": [128], "gnorm": [128],
    "mnorm": [256], "gup": [16, 128], "gbias": [128, 1], "convw": [128, 2, 4], "convb": [128, 2], "skip": [128, 2],
    "bi": [2, 1], "bf": [2, 1], "wq": [128, 2, 4], "wk": [128, 2, 4], "wv": [128, 2, 4],
}


def mixer_weights_np(inp, l, hh):
    w_in = inp["w_in"][l]
    h2 = slice(hh * 256, (hh + 1) * 256)
    h1 = slice(hh * 128, (hh + 1) * 128)

    def cols(i, sl):
        return w_in[:, OFF[i] + sl.start:OFF[i] + sl.stop]

    wc = np.concatenate([
        cols(0, h2), cols(1, h2), cols(4, h1), cols(5, h1), cols(9, h2),
        w_in[:, OFF[7]:OFF[8]], cols(11, slice(hh * 2, hh * 2 + 2)), cols(12, slice(hh * 2, hh * 2 + 2)),
        cols(2, h2), cols(6, h2), cols(3, h2), cols(8, h2), cols(10, h2)], axis=1)
    assert wc.shape[1] == NCOL
    w_out = inp["w_out"][l]
    wo = np.concatenate([w_out[hh * 256:(hh + 1) * 256], w_out[512 + hh * 256:512 + (hh + 1) * 256],
                         w_out[1024 + hh * 256:1024 + (hh + 1) * 256]], axis=0)
    d = {
        "wc": wc, "wo": wo,
        "gmix": inp["norm_mix"][l].reshape(8, 128).T,
        "lblog": inp["hgrn_lb_logits"][:, h2].reshape(DEPTH, 2, 128).transpose(2, 1, 0),
        "hnorm": inp["hgrn_norm"][l], "gnorm": inp["gla_norm"][l],
        "mnorm": inp["mlstm_norm"][l][h2],
        "gup": inp["gla_gate_up"][l][:, h1],
        "gbias": inp["gla_gate_bias"][l][h1].reshape(128, 1),
        "convw": inp["mlstm_conv_w"][l][:, h2].reshape(4, 2, 128).transpose(2, 1, 0),
        "convb": inp["mlstm_conv_b"][l][h2].reshape(2, 128).T,
        "skip": inp["mlstm_skip"][l][h2].reshape(2, 128).T,
        "bi": inp["mlstm_igate_bias"][l][hh * 2:hh * 2 + 2].reshape(2, 1),
        "bf": inp["mlstm_fgate_bias"][l][hh * 2:hh * 2 + 2].reshape(2, 1),
    }
    for nm in ("wq", "wk", "wv"):
        w = inp["mlstm_" + nm][l]
        d[nm] = w[hh * 64:(hh + 1) * 64].reshape(2, 128, 4).transpose(1, 0, 2)
    return {k: np.ascontiguousarray(v, dtype=np.float32) for k, v in d.items()}


def build_mixer_prog(S, layer):
    nc = bass.Bass("TRN2", target_bir_lowering=False)
    P = Prog(nc)
    cd = {k: P.dram_in("c_" + k, shp) for k, shp in CONST_SHAPES.items()}
    wd = {k: P.dram_in("w_" + k, shp) for k, shp in MIX_SHAPES.items()}
    x_d = P.dram_in("x", [S, D])
    y_d = P.dram_out("y", [S, D])
    R = setup_common(P, cd)
    emit_mixer(P, R, S, lambda t0: x_d[t0:t0 + T, :], y_d, wd, layer)
    P.finish("sp", [y_d])
    P.es.close()
    return nc, P


XA_SHAPES = {"wq": [D, D], "wk": [D, D], "wv": [D, D], "wo": [D, D], "gx": [128, 8], "gm": [128, 8], "gfin": [D]}


def emit_xattn(P, R, S2, x_d, ys_d, mem_d, out_d, wd, final):
    NT = S2 // T
    stage = P.sb("xstage", [128, D])
    Wq = P.sb("Wq", [128, 8, D], BF16)
    Wo = P.sb("Wo2", [128, 8, D], BF16)
    kT = P.sb("kT", [128, 8, MEM], BF16)
    vtm = P.sb("xvtm", [128, 2, D], BF16)
    gx = P.sb("gx", [128, 8]); P.dma("sp", gx.v, wd["gx"].v)
    gm = P.sb("gm", [128, 8]); P.dma("sp", gm.v, wd["gm"].v)
    onesb = P.sb("onesb", [128, 128], BF16); P.memset("pool", onesb.v, 1.0)
    gfin = None
    if final:
        gfin = P.sb("gfin", [128, D]); P.dma("sp", gfin.v, wd["gfin"].v.pbc(128))
    xt = P.sb("x_xt", [128, NJ, D])
    yt = P.sb("x_yt", [128, NJ, D])
    scr = (P.sb("x_ss", [128, NJ]), P.sb("x_sstmp", [128, NJ]), P.sb("x_rs", [128, NJ]),
           P.sb("x_junk", [128, D], BF16), P.sb("x_xn", [128, NJ, D], BF16))
    hT = P.sb("x_hT", [128, 8, T], BF16)
    P.push()
    Wk = P.sb("Wk", [128, 8, D], BF16)
    Wv = P.sb("Wv", [128, 8, D], BF16)
    load_cast_weight(P, Wk, wd["wk"], 8, D, [stage], ["pool", "dve"])
    load_cast_weight(P, Wv, wd["wv"], 8, D, [stage], ["pool", "dve"])
    P.dma("sp", xt[:, 0:2, :], mem_d.v.rr("(j p) d -> p j d", p=128))
    rms_to_hT(P, R, xt, hT, gm, 2, scr)
    for cb in range(8):
        px = nxt(R, "pX")
        for k in range(8):
            P.mm(px[:, 0:MEM], Wk[:, k, cb * 128:(cb + 1) * 128], hT[:, k, 0:MEM], start=(k == 0), stop=(k == 7))
        P.copy("act" if cb % 2 == 0 else "dve", kT[:, cb, :], px[:, 0:MEM])
    for mj in range(2):
        for nh in range(2):
            px = nxt(R, "pX")
            for k in range(8):
                P.mm(px.v, hT[:, k, mj * 128:(mj + 1) * 128], Wv[:, k, nh * 512:(nh + 1) * 512], start=(k == 0), stop=(k == 7))
            P.copy("act" if nh == 0 else "dve", vtm[:, mj, nh * 512:(nh + 1) * 512], px.v)
    P.pop()
    load_cast_weight(P, Wq, wd["wq"], 8, D, [stage], ["pool", "dve"])
    load_cast_weight(P, Wo, wd["wo"], 8, D, [stage], ["pool", "dve"])
    qT = P.sb("x_qT", [128, 8, T], BF16)
    pT = [P.sb("x_pT%d" % i, [128, T], BF16) for i in range(2)]
    rinv = P.sb("x_rinv", [128, T])
    oT = P.sb("x_oT", [128, 8, T], BF16)
    for it in range(NT):
        t0 = it * T
        P.dma("sp", xt.v, x_d(t0).rr("(j p) d -> p j d", p=128))
        for yi, y_d in enumerate(ys_d):
            P.dma("act", yt.v, y_d[t0:t0 + T, :].rr("(j p) d -> p j d", p=128))
            P.tt("dve" if yi == 0 else "pool", xt.v, xt.v, yt.v, ALU.add)
        rms_to_hT(P, R, xt, hT, gx, NJ, scr)
        for cb in range(8):
            px = nxt(R, "pX")
            for k in range(8):
                P.mm(px.v, Wq[:, k, cb * 128:(cb + 1) * 128], hT[:, k, :], start=(k == 0), stop=(k == 7))
            P.copy("act" if cb % 2 == 0 else "dve", qT[:, cb, :], px.v)
        for hd in range(4):
            for mj in range(2):
                px = nxt(R, "pX")
                for i2 in range(2):
                    cb = hd * 2 + i2
                    P.mm(px.v, kT[:, cb, mj * 128:(mj + 1) * 128], qT[:, cb, :], start=(i2 == 0), stop=(i2 == 1))
                P.act(pT[mj].v, px.v, AF.Exp, scale=1.0 / 16.0)
            px = nxt(R, "pX")
            for mj in range(2):
                P.mm(px.v, onesb.v, pT[mj].v, start=(mj == 0), stop=(mj == 1))
            P.recip(rinv.v, px.v)
            for e2 in range(2):
                px = nxt(R, "pX")
                c0 = hd * 256 + e2 * 128
                for mj in range(2):
                    P.mm(px.v, vtm[:, mj, c0:c0 + 128], pT[mj].v, start=(mj == 0), stop=(mj == 1))
                P.tt("dve", oT[:, hd * 2 + e2, :], px.v, rinv.v, ALU.mult)
        for j in range(NJ):
            for nh in range(2):
                px = nxt(R, "pX")
                for cb in range(8):
                    P.mm(px.v, oT[:, cb, j * 128:(j + 1) * 128], Wo[:, cb, nh * 512:(nh + 1) * 512], start=(cb == 0), stop=(cb == 7))
                P.tt("dve", xt[:, j, nh * 512:(nh + 1) * 512], px.v, xt[:, j, nh * 512:(nh + 1) * 512], ALU.add)
        if final:
            ss, tmp, rs, junk, xn = scr
            P.memset("pool", ss.v, 0.0)
            for j in range(NJ):
                P.act(junk.v, xt[:, j, :], AF.Square, accum=ss[:, j:j + 1])
            P.rstd(rs.v, ss.v, 1.0 / D, tmp.v)
            for j in range(NJ):
                P.act(xt[:, j, :], xt[:, j, :], AF.Copy, scale=rs[:, j:j + 1])
                P.tt("pool", xt[:, j, :], xt[:, j, :], gfin.v, ALU.mult)
        P.dma("sp", out_d(t0).rr("(j p) d -> p j d", p=128), xt.v)


def xattn_weights_np(inp, l):
    d = {"wq": inp["xa_wq"][l], "wk": inp["xa_wk"][l], "wv": inp["xa_wv"][l], "wo": inp["xa_wo"][l],
         "gx": inp["norm_xattn"][l].reshape(8, 128).T, "gm": inp["norm_mem"][l].reshape(8, 128).T,
         "gfin": inp["norm_final"]}
    return {k: np.ascontiguousarray(v, dtype=np.float32) for k, v in d.items()}


def build_xattn_prog(S2, final, ny=2):
    nc = bass.Bass("TRN2", target_bir_lowering=False)
    P = Prog(nc)
    cd = {k: P.dram_in("c_" + k, shp) for k, shp in CONST_SHAPES.items()}
    wd = {k: P.dram_in("w_" + k, shp) for k, shp in XA_SHAPES.items()}
    x_d = P.dram_in("x", [S2, D])
    ys_d = [P.dram_in("y%d" % i, [S2, D]) for i in range(ny)]
    mem_d = P.dram_in("mem", [MEM, D])
    out_d = P.dram_out("out", [S2, D])
    R = setup_common(P, cd)
    emit_xattn(P, R, S2, lambda t0: x_d[t0:t0 + T, :], ys_d, mem_d, lambda t0: out_d[t0:t0 + T, :], wd, final)
    P.finish("sp", [out_d])
    P.es.close()
    return nc, P


N_CORES = 8
BATCH = 4
SEQ = 8192
PAIRS = [[0, 1], [2, 3], [4, 5], [6, 7]]


def build_fused(S):
    S2 = S // 2
    nc = bass.Bass("TRN2", target_bir_lowering=False)
    P = Prog(nc)
    cd = {k: P.dram_in("c_" + k, shp) for k, shp in CONST_SHAPES.items()}
    mwd = [{k: P.dram_in("m%d_%s" % (l, k), shp) for k, shp in MIX_SHAPES.items()} for l in range(DEPTH)]
    xwd = [{k: P.dram_in("a%d_%s" % (l, k), shp) for k, shp in XA_SHAPES.items()} for l in range(DEPTH)]
    x_full = P.dram_in("x_full", [S, D])
    x_half = P.dram_in("x_half", [S2, D])
    mem_d = P.dram_in("mem", [MEM, D])
    out_d = P.dram_out("out", [S2, D])
    ypart = P.dram_tmp("ypart", [S, D])
    ysum = P.dram_tmp("ysum", [S2, D])
    NC2 = S2 // T
    xh = [P.dram_tmp("xh%d" % c, [T, D]) for c in range(NC2)]
    xf = [P.dram_tmp("xf%d" % c, [2 * T, D]) for c in range(NC2)]
    R = setup_common(P, cd)

    def full_from_xf(t0):
        s_, u = t0 // S2, t0 % S2
        return xf[u // T][s_ * T:(s_ + 1) * T, :]

    full_src = lambda t0: x_full[t0:t0 + T, :]
    half_src = lambda t0: x_half[t0:t0 + T, :]
    for l in range(DEPTH):
        final = (l == DEPTH - 1)
        P.push()
        emit_mixer(P, R, S, full_src, ypart, mwd[l], l)
        P.barrier()
        P.pop()
        P.collective("ReduceScatter", ALU.add, PAIRS, ypart, ysum)
        P.push()
        dst = (lambda t0: out_d[t0:t0 + T, :]) if final else (lambda t0: xh[t0 // T].v)
        emit_xattn(P, R, S2, half_src, [ysum], mem_d, dst, xwd[l], final)
        P.barrier()
        P.pop()
        if not final:
            for c in range(NC2):
                P.collective("AllGather", ALU.bypass, PAIRS, xh[c], xf[c])
            full_src = full_from_xf
            half_src = lambda t0: xh[t0 // T].v
    for i in range(PADPE):
        P.mm(R.pSm[0:1, 0:1], R.ident[0:1, 0:1], R.ident[0:1, 0:1])
    for i in range(PADV):
        P.memset("dve" if i % 2 == 0 else "act", R.bdmask[0:1, 0:1], 1.0) if i % 2 == 0 else P.act(R.sel[0:1, 0, 0:1], R.sel[0:1, 0, 0:1], AF.Copy)
    if PAD:
        padt = P.sb("padt", [128, 4096])
        P.memset("pool", padt.v, 0.5)
        for i in range(PAD):
            P.act(padt.v, padt.v, AF.Copy)
    P.finish("sp", [out_d])
    P.es.close()
    return nc, P


PAD = 0
PADPE = 0
PADV = 0


def fused_inputs(inp, S):
    S2 = S // 2
    cn = {"c_" + k: v for k, v in consts_np().items()}
    mw = [[mixer_weights_np(inp, l, hh) for hh in range(2)] for l in range(DEPTH)]
    xw = [xattn_weights_np(inp, l) for l in range(DEPTH)]
    maps = []
    for i in range(N_CORES):
        b, hh = i // 2, i % 2
        xb = np.ascontiguousarray(inp["x"][b, :S], dtype=np.float32)
        m = {"x_full": xb, "x_half": np.ascontiguousarray(xb[hh * S2:(hh + 1) * S2]),
             "mem": np.ascontiguousarray(inp["mem"][b], dtype=np.float32)}
        m.update(cn)
        for l in range(DEPTH):
            m.update({"m%d_%s" % (l, k): v for k, v in mw[l][hh].items()})
            m.update({"a%d_%s" % (l, k): v for k, v in xw[l].items()})
        maps.append(m)
    return maps


def kernel(**inp):
    inp = {k: np.asarray(v) for k, v in inp.items()}
    S = inp["x"].shape[1]
    nc, _ = build_fused(S)
    maps = fused_inputs(inp, S)
    res = run_bass_kernel_spmd(nc, maps, core_ids=list(range(N_CORES)))
    outs = [r["out"] for r in res.results]
    full = [np.concatenate([outs[2 * b], outs[2 * b + 1]], axis=0) for b in range(BATCH)]
    return np.stack(full, axis=0).astype(np.float32)
```

```python
import contextlib
import numpy as np
import concourse.bass as bass
import concourse.mybir as mybir
from concourse.bass_utils import run_bass_kernel_spmd

F32 = mybir.dt.float32
BF16 = mybir.dt.bfloat16
AF = mybir.ActivationFunctionType
ALU = mybir.AluOpType
AX = mybir.AxisListType

D = 1024
DEPTH = 2
CH = 64
T = 512
NJ = T // 128
NCH = T // CH
EPS = 1e-6
MEM = 256
IN_SPLITS = [512, 512, 512, 512, 256, 256, 512, 16, 512, 512, 512, 4, 4]
OFF = np.concatenate([[0], np.cumsum(IN_SPLITS)]).tolist()
C_HQ, C_HF, C_GQ, C_GK, C_MU, C_GA, C_MI, C_MF = 0, 256, 512, 640, 768, 1024, 1040, 1042
C_TM = 1044
NCOL = C_TM + 1280


class V:
    __slots__ = ("buf", "ap")

    def __init__(self, buf, ap):
        self.buf = buf
        self.ap = ap

    def __getitem__(self, k):
        return V(self.buf, self.ap[k])

    def rr(self, s, **kw):
        return V(self.buf, self.ap.rearrange(s, **kw))

    def bc(self, shape):
        return V(self.buf, self.ap.to_broadcast(list(shape)))

    def unsq(self, axis):
        return V(self.buf, self.ap.unsqueeze(axis))

    def pbc(self, n):
        return V(self.buf, self.ap.partition_broadcast(n))

    @property
    def v(self):
        return self


class Buf:
    __slots__ = ("name", "t", "w", "r", "dsem", "dcnt", "excl")

    def __init__(self, name, t):
        self.name = name
        self.t = t
        self.excl = False
        self.w = {}
        self.r = {}
        self.dsem = None
        self.dcnt = 0

    def __getitem__(self, k):
        return V(self, self.t[k])

    @property
    def v(self):
        return V(self, self.t[:])


def _u(x):
    return x.ap if isinstance(x, V) else x


class Prog:
    def __init__(self, nc):
        self.nc = nc
        self.es = contextlib.ExitStack()
        self.eng = {"pe": nc.tensor, "act": nc.scalar, "dve": nc.vector, "pool": nc.gpsimd, "sp": nc.sync}
        self.sem = {}
        self.cnt = {}
        self.seen = {}
        for k in self.eng:
            self.sem[k] = self.es.enter_context(nc.semaphore("s_" + k))
            self.cnt[k] = 0
            self.seen[k] = {}
        self.nbuf = 0
        self.ninst = 0
        self.uid = 0
        self.stack = []
        self.root = self.es
        self.dcounts = {}
        self.nwait = {}

    def push(self):
        self.stack.append(self.es)
        self.es = contextlib.ExitStack()

    def pop(self):
        self.es.close()
        self.es = self.stack.pop()

    def sb(self, name, shape, dt=F32):
        self.uid += 1
        t = self.es.enter_context(self.nc.sbuf_tensor("sb%d_%s" % (self.uid, name), list(shape), dt))
        return Buf(name, t)

    def ps(self, name, shape, dt=F32):
        self.uid += 1
        t = self.es.enter_context(self.nc.psum_tensor("ps%d_%s" % (self.uid, name), list(shape), dt))
        b = Buf(name, t)
        b.excl = True
        return b

    def dram_in(self, name, shape, dt=F32):
        return Buf(name, self.nc.dram_tensor(name, list(shape), dt, kind="ExternalInput").ap())

    def dram_out(self, name, shape, dt=F32):
        return Buf(name, self.nc.dram_tensor(name, list(shape), dt, kind="ExternalOutput").ap())

    def dram_tmp(self, name, shape, dt=F32):
        return Buf(name, self.nc.dram_tensor(name, list(shape), dt, kind="Internal").ap())

    def alias(self, name, buf, ap):
        return V(buf, ap)

    def _wait(self, e, deps):
        for k, v in deps.items():
            if self.seen[e].get(k, 0) >= v:
                continue
            if k == e and e == "pe":
                continue
            self.eng[e].wait_ge(self.sem[k], v)
            self.nwait[e] = self.nwait.get(e, 0) + 1
            self.seen[e][k] = v

    @staticmethod
    def _deps(reads, writes, e=None):
        deps = {}
        for b in reads:
            for k, v in b.w.items():
                if deps.get(k, 0) < v:
                    deps[k] = v
            if b.excl:
                for k, v in b.r.items():
                    if k != e and deps.get(k, 0) < v:
                        deps[k] = v
        for b in writes:
            for d in (b.w, b.r):
                for k, v in d.items():
                    if deps.get(k, 0) < v:
                        deps[k] = v
        return deps

    def op(self, e, fn, ins, outs):
        reads = []
        for x in ins:
            if isinstance(x, V) and x.buf not in reads:
                reads.append(x.buf)
        writes = []
        for x in outs:
            if isinstance(x, V) and x.buf not in writes:
                writes.append(x.buf)
        self._wait(e, self._deps(reads, writes, e))
        ins_ = fn(self.eng[e])
        self.cnt[e] += 1
        c = self.cnt[e]
        ins_.then_inc(self.sem[e], 1)
        for b in writes:
            b.w = {e: c}
            b.r = {}
        for b in reads:
            if b not in writes:
                b.r[e] = c
        self.ninst += 1

    def dma(self, e, out, in_):
        reads = [in_.buf]
        writes = [out.buf]
        self._wait(e, self._deps(reads, writes))
        sbuf = out.buf
        if sbuf.dsem is None:
            key = "d%d" % self.nbuf
            self.nbuf += 1
            self.sem[key] = self.root.enter_context(self.nc.semaphore(key))
            sbuf.dsem = key
        self.eng[e].dma_start(out=out.ap, in_=in_.ap).then_inc(self.sem[sbuf.dsem], 16)
        sbuf.dcnt += 16
        self.dcounts[sbuf.dsem] = sbuf.dcnt
        out.buf.w = {sbuf.dsem: sbuf.dcnt}
        out.buf.r = {}
        in_.buf.r[sbuf.dsem] = sbuf.dcnt
        self.ninst += 1

    def barrier(self):
        deps = {k: v for k, v in self.cnt.items() if v > 0}
        deps.update(self.dcounts)
        for e in self.eng:
            self._wait(e, {k: v for k, v in deps.items() if k != e})

    def collective(self, kind, op, groups, in_buf, out_buf):
        e = "pool"
        self._wait(e, self._deps([in_buf], [out_buf], e))
        if out_buf.dsem is None:
            key = "c%d" % self.nbuf
            self.nbuf += 1
            self.sem[key] = self.root.enter_context(self.nc.semaphore(key))
            out_buf.dsem = key
        self.nc.gpsimd.collective_compute(kind, op, replica_groups=groups, ins=[in_buf.t.opt()],
                                          outs=[out_buf.t.opt()]).then_inc(self.sem[out_buf.dsem])
        out_buf.dcnt += 1
        self.dcounts[out_buf.dsem] = out_buf.dcnt
        out_buf.w = {out_buf.dsem: out_buf.dcnt}
        out_buf.r = {}
        in_buf.r[out_buf.dsem] = out_buf.dcnt
        self.ninst += 1

    def finish(self, e, bufs):
        self._wait(e, self._deps((), bufs))

    def act(self, out, in_, func, bias=None, scale=None, accum=None, e="act"):
        kw = {}
        if bias is not None:
            kw["bias"] = _u(bias)
        if scale is not None:
            kw["scale"] = _u(scale)
        if accum is not None:
            kw["accum_out"] = _u(accum)
        outs = [out] + ([accum] if accum is not None else [])
        self.op(e, lambda g: g.activation(out=out.ap, in_=in_.ap, func=func, **kw), [in_, bias, scale], outs)

    def tt(self, e, out, in0, in1, op):
        self.op(e, lambda g: g.tensor_tensor(out=out.ap, in0=in0.ap, in1=in1.ap, op=op), [in0, in1], [out])

    def ts(self, e, out, in0, s1, op0, s2=None, op1=None):
        if op1 is None:
            self.op(e, lambda g: g.tensor_scalar(out=out.ap, in0=in0.ap, scalar1=_u(s1), scalar2=None, op0=op0),
                    [in0, s1], [out])
        else:
            self.op(e, lambda g: g.tensor_scalar(out=out.ap, in0=in0.ap, scalar1=_u(s1), scalar2=_u(s2), op0=op0, op1=op1),
                    [in0, s1, s2], [out])

    def stt(self, e, out, in0, scalar, in1, op0, op1):
        self.op(e, lambda g: g.scalar_tensor_tensor(out=out.ap, in0=in0.ap, scalar=_u(scalar), in1=in1.ap, op0=op0, op1=op1),
                [in0, scalar, in1], [out])

    def copy(self, e, out, in_):
        if e == "act":
            self.act(out, in_, AF.Copy)
        else:
            self.op(e, lambda g: g.tensor_copy(out=out.ap, in_=in_.ap), [in_], [out])

    def memset(self, e, out, val):
        self.op(e, lambda g: g.memset(out.ap, val), [], [out])

    def scan(self, out, d0, d1, init, op0, op1):
        self.op("dve", lambda g: g.tensor_tensor_scan(out=out.ap, data0=d0.ap, data1=d1.ap, initial=_u(init), op0=op0, op1=op1),
                [d0, d1, init], [out])

    def reduce(self, e, out, in_, op, axis=AX.X):
        self.op(e, lambda g: g.tensor_reduce(out=out.ap, in_=in_.ap, axis=axis, op=op), [in_], [out])

    def recip(self, out, in_):
        self.op("dve", lambda g: g.reciprocal(out=out.ap, in_=in_.ap), [in_], [out])

    def mm(self, out, lhsT, rhs, start=True, stop=True):
        self.op("pe", lambda g: g.matmul(out.ap, lhsT=lhsT.ap, rhs=rhs.ap, start=start, stop=stop), [lhsT, rhs], [out])

    def tr(self, out, in_, ident):
        self.op("pe", lambda g: g.transpose(out=out.ap, in_=in_.ap, identity=ident.ap), [in_, ident], [out])

    def rstd(self, out, in_, scale, tmp):
        self.ts("dve", tmp, in_, scale, ALU.mult, EPS, ALU.add)
        self.act(tmp, tmp, AF.Ln)
        self.act(out, tmp, AF.Exp, scale=-0.5)


class Res:
    pass


def setup_common(P, consts_d):
    R = Res()
    R.ident = P.sb("ident", [128, 128])
    R.identb = P.sb("identb", [128, 128], BF16)
    R.maskT = P.sb("maskT", [128, 128])
    R.bdmask = P.sb("bdmask", [128, 32])
    R.sel = P.sb("sel", [2, 2, 128])
    P.dma("sp", R.ident.v, consts_d["ident"].v)
    P.dma("sp", R.maskT.v, consts_d["maskT"].v)
    P.dma("sp", R.bdmask.v, consts_d["bdmask"].v)
    P.dma("sp", R.sel.v, consts_d["sel"].v)
    P.copy("dve", R.identb.v, R.ident.v)
    R.pX = [P.ps("pX%d" % i, [128, 512]) for i in range(2)]
    bT = P.ps("bT", [128, 1024], BF16)
    R.pT = [P.alias("pT%d" % i, bT, bT.t[:, i * 512:(i + 1) * 512]) for i in range(2)]
    bO = [P.ps("bO%d" % i, [128, 512]) for i in range(2)]
    R.pO = [P.alias("pO%d" % i, bO[i % 2], bO[i % 2].t[:, (i // 2) * 160:(i // 2) * 160 + 132]) for i in range(6)]
    bS = P.ps("bS", [128, 512])
    R.pS = [P.alias("pS%d" % i, bS, bS.t[:, i * 128:(i + 1) * 128]) for i in range(4)]
    bKV = P.ps("bKV", [128, 512])
    R.pKV = [P.alias("pKV%d" % i, bKV, bKV.t[:, i * 160:i * 160 + 132]) for i in range(3)]
    bSm = P.ps("bSm", [128, 512])
    R.pSm = P.alias("pSm", bSm, bSm.t[:, 0:64])
    R.pX = R.pX + [bO[0], bO[1], bS, bKV]
    R.px_i = 0
    R.ps_i = 0
    R.pkv_i = 0
    R.pt_i = 0
    return R


def nxt(R, what):
    if what == "pX":
        R.px_i += 1
        return R.pX[R.px_i % len(R.pX)]
    if what == "pS":
        R.ps_i += 1
        return R.pS[R.ps_i % len(R.pS)]
    if what == "pKV":
        R.pkv_i += 1
        return R.pKV[R.pkv_i % len(R.pKV)]
    if what == "pT":
        R.pt_i += 1
        return R.pT[R.pt_i % len(R.pT)]
    raise KeyError(what)


def rms_to_hT(P, R, xt, hT, gcol, ntok_j, scr):
    nj = ntok_j
    ss, tmp, rs, junk, xn = scr
    P.memset("pool", ss.v, 0.0)
    for j in range(nj):
        P.act(junk.v, xt[:, j, :], AF.Square, accum=ss[:, j:j + 1])
    P.rstd(rs[:, 0:nj], ss[:, 0:nj], 1.0 / D, tmp[:, 0:nj])
    for j in range(nj):
        if j % 2 == 0:
            P.act(xn[:, j, :], xt[:, j, :], AF.Copy, scale=rs[:, j:j + 1])
        else:
            P.ts("dve", xn[:, j, :], xt[:, j, :], rs[:, j:j + 1], ALU.mult)
    for k in range(8):
        pt = nxt(R, "pT")
        for j in range(nj):
            P.tr(pt[:, j * 128:(j + 1) * 128], xn[:, j, k * 128:(k + 1) * 128], R.identb.v)
        P.ts("dve", hT[:, k, 0:nj * 128], pt[:, 0:nj * 128], gcol[:, k:k + 1], ALU.mult)


def load_cast_weight(P, w_sb, w_d, nk, ncols, stage, eng_cycle):
    for k in range(nk):
        st = stage[k % len(stage)]
        P.dma("sp", st[:, 0:ncols], w_d[k * 128:(k + 1) * 128, :])
        P.copy(eng_cycle[k % len(eng_cycle)], w_sb[:, k, :], st[:, 0:ncols])


STOP = 99


def emit_mixer(P, R, S, x_d, y_d, wd, layer, after_tile=None):
    NT = S // T
    stage = [P.sb("wstage%d" % i, [128, NCOL]) for i in range(1)]
    W = P.sb("W", [128, 8, NCOL], BF16)
    load_cast_weight(P, W, wd["wc"], 8, NCOL, stage, ["pool", "dve"])
    Wo = P.sb("Wo", [128, 6, D], BF16)
    load_cast_weight(P, Wo, wd["wo"], 6, D, stage, ["pool", "dve"])
    gmix = P.sb("gmix", [128, 8]); P.dma("sp", gmix.v, wd["gmix"].v)
    lbl = P.sb("lbl", [128, 2, DEPTH]); P.dma("sp", lbl.v, wd["lblog"].v)
    lbe = P.sb("lbe", [128, 2, DEPTH]); P.act(lbe.v, lbl.v, AF.Exp)
    lbtot = P.sb("lbtot", [128, 2]); P.reduce("dve", lbtot.v, lbe.v, ALU.add)
    lbr = P.sb("lbr", [128, 2]); P.recip(lbr.v, lbtot.v)
    lb = P.sb("lb", [128, 2]); P.memset("pool", lb.v, 0.0)
    for l2 in range(1, layer + 1):
        P.tt("dve", lb.v, lb.v, lbe[:, :, l2], ALU.add)
    P.tt("dve", lb.v, lb.v, lbr.v, ALU.mult)
    oml = P.sb("oml", [128, 2]); P.ts("dve", oml.v, lb.v, -1.0, ALU.mult, 1.0, ALU.add)
    lbm1 = P.sb("lbm1", [128, 2]); P.ts("dve", lbm1.v, lb.v, -1.0, ALU.add)
    grow = P.sb("grow", [128, 768])
    for i, nm in enumerate(["hnorm", "hnorm", "gnorm", "gnorm"]):
        P.dma("sp", grow[:, i * 128:(i + 1) * 128], wd[nm].v.pbc(128))
    P.dma("sp", grow[:, 512:768], wd["mnorm"].v.pbc(128))
    gup = P.sb("gup", [16, 128]); P.dma("sp", gup.v, wd["gup"].v)
    gbias = P.sb("gbias", [128, 1]); P.dma("sp", gbias.v, wd["gbias"].v)
    convw = P.sb("convw", [128, 2, 4]); P.dma("sp", convw.v, wd["convw"].v)
    convb = P.sb("convb", [128, 2]); P.dma("sp", convb.v, wd["convb"].v)
    skip = P.sb("skip", [128, 2]); P.dma("sp", skip.v, wd["skip"].v)
    bi = P.sb("bi", [2, 1]); P.dma("sp", bi.v, wd["bi"].v)
    bf = P.sb("bf", [2, 1]); P.dma("sp", bf.v, wd["bf"].v)
    BDq, BDks, BDv = [], [], []
    wsm = {}
    for nm in ("wq", "wk", "wv"):
        wsm[nm] = P.sb("wsm_" + nm, [128, 2, 4]); P.dma("sp", wsm[nm].v, wd[nm].v)
    for h in range(2):
        bq = P.sb("BDq%d" % h, [128, 128], BF16)
        bks = P.sb("BDks%d" % h, [128, 256], BF16)
        bv = P.sb("BDv%d" % h, [128, 128], BF16)
        for dst, nm in ((bq.v, "wq"), (bks[:, 0:128], "wk"), (bv.v, "wv")):
            P.tt("pool", dst.rr("p (g o) -> p g o", o=4),
                 wsm[nm][:, h, :].unsq(1).bc([128, 32, 4]),
                 R.bdmask.v.unsq(2).bc([128, 32, 4]), ALU.mult)
        P.ts("pool", bks[:, 128:256], R.ident.v, skip[:, h:h + 1], ALU.mult)
        BDq.append(bq); BDks.append(bks); BDv.append(bv)

    st_f = {}
    st_b = {}
    for nm, ncol in (("h0", 128), ("h1", 128), ("g", 128), ("m0", 129), ("m1", 129)):
        st_f[nm] = P.sb("stf_" + nm, [128, ncol])
        P.memset("pool", st_f[nm].v, 0.0)
    for nm, ncol in (("h0", 128), ("h1", 128), ("g0", 128), ("g1", 128), ("m0", 129), ("m1", 129)):
        st_b[nm] = [P.sb("stb_%s_%d" % (nm, i), [128, ncol], BF16) for i in range(2)]
        P.memset("pool", st_b[nm][0].v, 0.0)
    mstP = P.sb("mstP", [128, 129])
    mstP2 = P.sb("mstP2", [128, 129])
    uext = [P.sb("uext%d" % h, [128, 3 + T]) for h in range(2)]
    for h in range(2):
        P.memset("pool", uext[h][:, 0:3], 0.0)
    m0 = P.sb("m0", [2, 1]); P.memset("pool", m0.v, 0.0)

    xt = P.sb("xt", [128, NJ, D])
    scr = (P.sb("ss", [128, NJ]), P.sb("sstmp", [128, NJ]), P.sb("rs", [128, NJ]),
           P.sb("junk", [128, D], BF16), P.sb("xn", [128, NJ, D], BF16))
    hT = P.sb("hT", [128, 8, T], BF16)
    vtm = P.sb("vtm", [128, NJ, 512], BF16)
    ztm = P.sb("ztm", [128, NJ, 768])
    osb = xt
    ones = P.sb("ones", [128, T]); P.memset("pool", ones.v, 1.0)
    tmpA = P.sb("tmpA", [128, 8, T])
    tq, tsg, tg, tkk, tB, tDm, teD, teDn = [tmpA[:, i, :] for i in range(8)]
    blk = []
    for i in range(3):
        b = Res()
        b.q, b.sg, b.g, b.kk, b.B, b.Dm, b.eD, b.eDn = tq, tsg, tg, tkk, tB, tDm, teD, teDn
        b.qt = P.sb("qt%d" % i, [128, T], BF16)
        b.kt = P.sb("kt%d" % i, [128, T], BF16)
        b.qe = P.sb("qe%d" % i, [128, T], BF16)
        b.ktm = P.sb("ktm%d" % i, [128, NJ, 128], BF16)
        b.prev = P.sb("prev%d" % i, [128, NCH])
        b.d3 = P.sb("d3%d" % i, [128, 3, NCH])
        b.e3 = P.sb("e3%d" % i, [128, 3, NCH])
        blk.append(b)
    ga_sb = P.sb("ga_sb", [16, T])
    tmpkv = [P.sb("tmpkv%d" % i, [128, 128]) for i in range(3)]
    scT = [P.sb("scT%d" % i, [128, 128], BF16) for i in range(6)]
    ML = []
    for h in range(2):
        m = Res()
        m.acc = P.sb("cacc%d" % h, [128, T])
        m.conv = P.sb("conv%d" % h, [128, T], BF16)
        m.ub = P.sb("ub%d" % h, [128, T], BF16)
        m.qT = P.sb("mqT%d" % h, [128, T], BF16)
        m.kT = P.sb("mkT%d" % h, [128, T], BF16)
        m.khat = P.sb("khat%d" % h, [128, NJ, 128], BF16)
        m.vaug = P.sb("vaug%d" % h, [128, NJ, 132], BF16)
        P.memset("pool", m.vaug.v, 1.0)
        m.wp = P.sb("wp%d" % h, [128, NCH])
        ML.append(m)
    sctm = P.sb("sctm", [128, NJ, 256])
    g_sf = P.sb("g_sf", [2, T]); g_lf = P.sb("g_lf", [2, T]); g_B = P.sb("g_B", [2, T])
    g_a = P.sb("g_a", [2, T]); g_wj = P.sb("g_wj", [2, T]); g_thr = P.sb("g_thr", [2, T])
    g_am = P.sb("g_am", [2, NCH]); g_R = P.sb("g_R", [2, NCH]); g_mp = P.sb("g_mp", [2, NCH]); g_wprev = P.sb("g_wprev", [2, NCH])
    gtm = P.sb("gtm", [128, NJ, 2, 2])
    den = P.sb("den", [128, 4])
    st6 = P.sb("st6", [128, NJ, 6]); st6b = P.sb("st6b", [128, NJ, 6]); st6c = P.sb("st6c", [128, NJ, 6])
    msum = P.sb("msum", [128, NJ, 2]); mmean = P.sb("mmean", [128, NJ, 2]); mvar = P.sb("mvar", [128, NJ, 2])
    osq = tmpA.v.rr("p a t -> p (a t)")[:, 0:NJ * 768].rr("p (j c) -> p j c", c=768)
    mixb = scr[4]
    mixT = hT
    ysb = [P.sb("ysb%d" % i, [128, D]) for i in range(2)]
    def c3(v):
        return v.rr("p (c t) -> p c t", t=CH)

    if STOP <= 1:
        return
    for it in range(NT):
        t0 = it * T
        P.dma("sp", xt.v, x_d(t0).rr("(j p) d -> p j d", p=128))
        rms_to_hT(P, R, xt, hT, gmix, NJ, scr)
        if STOP <= 2:
            continue

        def proj_fm(c0, M):
            px = nxt(R, "pX")
            for k in range(8):
                P.mm(px[0:M, :], W[:, k, c0:c0 + M], hT[:, k, :], start=(k == 0), stop=(k == 7))
            return px

        def decay_block(b, B_scale, is_gla):
            P.scan(b.B, ones.v, b.g, 0.0, ALU.mult, ALU.add)
            B3 = c3(b.B)
            P.tt("dve", c3(b.Dm), B3, B3[:, :, 32:33].bc([128, NCH, CH]), ALU.subtract)
            P.act(b.eD, b.Dm, AF.Exp, scale=B_scale)
            P.act(b.eDn, b.Dm, AF.Exp, scale=-B_scale)
            P.memset("pool", b.prev[:, 0:1], 0.0)
            P.copy("pool", b.prev[:, 1:NCH], B3[:, 0:NCH - 1, 63])
            P.tt("pool", b.d3[:, 0, :], B3[:, :, 32], b.prev.v, ALU.subtract)
            P.tt("pool", b.d3[:, 1, :], B3[:, :, 63], b.prev.v, ALU.subtract)
            P.tt("pool", b.d3[:, 2, :], B3[:, :, 63], B3[:, :, 32], ALU.subtract)
            P.act(b.e3.v, b.d3.v, AF.Exp, scale=B_scale)

        for h in range(2):
            b = blk[h]
            px = proj_fm(C_HQ + h * 128, 128)
            P.act(b.q, px.v, AF.Silu)
            px = proj_fm(C_HF + h * 128, 128)
            P.act(b.sg, px.v, AF.Sigmoid)
            P.act(b.g, b.sg, AF.Ln, bias=lb[:, h:h + 1], scale=oml[:, h:h + 1])
            P.ts("dve", b.kk, b.sg, lbm1[:, h:h + 1], ALU.mult, oml[:, h:h + 1], ALU.add)
            decay_block(b, 1.0, False)
            P.tt("pool", b.qt.v, b.q, b.eD, ALU.mult)
            P.tt("pool", b.kt.v, b.kk, b.eDn, ALU.mult)
            P.tt("pool", c3(b.qe.v), c3(b.qt.v), b.e3[:, 0, :].unsq(2).bc([128, NCH, CH]), ALU.mult)
        if STOP <= 2.1:
            continue
        b = blk[2]
        px = proj_fm(C_GA, 16)
        P.copy("act", ga_sb.v, px[0:16, :])
        px = nxt(R, "pX")
        P.mm(px.v, gup.v, ga_sb.v)
        P.act(b.sg, px.v, AF.Sigmoid, bias=gbias[:, 0:1])
        P.act(b.g, b.sg, AF.Ln)
        decay_block(b, 1.0 / 16.0, True)
        px = proj_fm(C_GQ, 128)
        P.stt("dve", b.qt.v, px.v, 0.125, b.eD, ALU.mult, ALU.mult)
        px = proj_fm(C_GK, 128)
        P.tt("dve", b.kt.v, px.v, b.eDn, ALU.mult)
        P.tt("pool", c3(b.qe.v), c3(b.qt.v), b.e3[:, 0, :].unsq(2).bc([128, NCH, CH]), ALU.mult)
        if STOP <= 2.2:
            continue
        for i in range(3):
            b = blk[i]
            pt = nxt(R, "pT")
            for j in range(NJ):
                P.tr(pt[:, j * 128:(j + 1) * 128], b.kt[:, j * 128:(j + 1) * 128], R.identb.v)
            P.copy("act", b.ktm.v.rr("p j d -> p (j d)"), pt.v)
        if STOP <= 2.3:
            continue
        for h in range(2):
            m = ML[h]
            px = proj_fm(C_MU + h * 128, 128)
            P.copy("act", uext[h][:, 3:3 + T], px.v)
            P.ts("dve", m.acc.v, uext[h][:, 0:T], convw[:, h, 0:1], ALU.mult, convb[:, h:h + 1], ALU.add)
            for tap in range(1, 4):
                P.stt("dve", m.acc.v, uext[h][:, tap:tap + T], convw[:, h, tap:tap + 1], m.acc.v, ALU.mult, ALU.add)
            P.act(m.conv.v, m.acc.v, AF.Silu)
            P.copy("pool", m.ub.v, uext[h][:, 3:3 + T])
            P.copy("pool", uext[h][:, 0:3], uext[h][:, T:T + 3])
            px = nxt(R, "pX"); P.mm(px.v, BDq[h].v, m.conv.v)
            P.copy("act", m.qT.v, px.v)
            px = nxt(R, "pX"); P.mm(px.v, BDks[h][:, 0:128], m.conv.v)
            P.copy("act", m.kT.v, px.v)
        if STOP <= 2.4:
            continue
        px = proj_fm(C_MF, 2)
        P.act(g_sf.v, px[0:2, :], AF.Sigmoid, bias=bf[:, 0:1])
        P.act(g_lf.v, g_sf.v, AF.Ln)
        P.scan(g_B.v, ones[0:2, :], g_lf.v, 0.0, ALU.mult, ALU.add)
        px = proj_fm(C_MI, 2)
        P.stt("dve", g_a.v, px[0:2, :], bi[:, 0:1], g_B.v, ALU.add, ALU.subtract)
        P.reduce("dve", g_am.v, c3(g_a.v), ALU.max)
        P.scan(g_R.v, g_am.v, g_am.v, m0[:, 0:1], ALU.max, ALU.max)
        P.copy("dve", g_mp[:, 0:1], m0.v)
        P.copy("dve", g_mp[:, 1:NCH], g_R[:, 0:NCH - 1])
        P.tt("dve", g_wprev.v, g_mp.v, g_R.v, ALU.subtract)
        P.act(g_wprev.v, g_wprev.v, AF.Exp)
        P.tt("dve", c3(g_wj.v), c3(g_a.v), g_R.v.unsq(2).bc([2, NCH, CH]), ALU.subtract)
        P.act(g_wj.v, g_wj.v, AF.Exp)
        P.tt("dve", c3(g_thr.v), c3(g_B.v), g_R.v.unsq(2).bc([2, NCH, CH]), ALU.add)
        P.act(g_thr.v, g_thr.v, AF.Exp, scale=-1.0)
        P.tt("dve", m0.v, g_R[:, NCH - 1:NCH], g_B[:, T - 1:T], ALU.add)
        if STOP <= 2.5:
            continue
        for j in range(NJ):
            P.tr(R.pSm[:, j * 4:j * 4 + 2], g_wj[:, j * 128:(j + 1) * 128], R.ident[0:2, 0:2])
            P.tr(R.pSm[:, j * 4 + 2:j * 4 + 4], g_thr[:, j * 128:(j + 1) * 128], R.ident[0:2, 0:2])
        for h in range(2):
            P.mm(R.pSm[:, 16 + h * NCH:16 + (h + 1) * NCH], R.sel[:, h, :], g_wprev.v)
        P.copy("dve", gtm.v.rr("p j a h -> p (j a h)"), R.pSm[:, 0:16])
        P.ts("dve", gtm[:, :, 0, :], gtm[:, :, 0, :], float(128 ** -0.5), ALU.mult)
        for h in range(2):
            P.copy("dve", ML[h].wp.v, R.pSm[:, 16 + h * NCH:16 + (h + 1) * NCH])
        if STOP <= 2.6:
            continue
        for h in range(2):
            m = ML[h]
            for j2 in range(NJ // 2):
                px = nxt(R, "pX")
                for jj in range(2):
                    j = j2 * 2 + jj
                    P.mm(px[:, jj * 256:(jj + 1) * 256], m.conv[:, j * 128:(j + 1) * 128], BDks[h].v)
                for jj in range(2):
                    j = j2 * 2 + jj
                    P.act(m.khat[:, j, :], px[:, jj * 256:jj * 256 + 128], AF.Copy, scale=gtm[:, j, 0, h:h + 1])
                    P.copy("dve", sctm[:, j, h * 128:(h + 1) * 128], px[:, jj * 256 + 128:(jj + 1) * 256])
            if STOP <= 2.65:
                continue
            px = nxt(R, "pX")
            for j in range(NJ):
                P.mm(px[:, j * 128:(j + 1) * 128], m.ub[:, j * 128:(j + 1) * 128], BDv[h].v)
            P.copy("act", m.vaug[:, :, 0:128], px.v.rr("p (j e) -> p j e", e=128))
        if STOP <= 2.7:
            continue
        for j in range(NJ):
            for gi, (c0, n) in enumerate(((C_TM, 512), (C_TM + 512, 512), (C_TM + 1024, 256))):
                px = nxt(R, "pX")
                for k in range(8):
                    P.mm(px[:, 0:n], hT[:, k, j * 128:(j + 1) * 128], W[:, k, c0:c0 + n], start=(k == 0), stop=(k == 7))
                if gi == 0:
                    P.copy("dve", vtm[:, j, :], px.v)
                elif gi == 1:
                    P.act(ztm[:, j, 0:512], px.v, AF.Silu)
                else:
                    P.act(ztm[:, j, 512:768], px[:, 0:256], AF.Silu)

        if STOP <= 3:
            continue
        dl = [(blk[0], 0, 128, st_f["h0"], st_b["h0"], 0, 0),
              (blk[1], 0, 128, st_f["h1"], st_b["h1"], 128, 128),
              (blk[2], 0, 64, st_f["g"], st_b["g0"], 256, 256),
              (blk[2], 64, 64, st_f["g"], st_b["g1"], 384, 384)]
        for pr in range(NJ):
            tok = slice(pr * 128, (pr + 1) * 128)
            for hi, (b, pb, K, sf, sbb, vc, oc) in enumerate(dl):
                pS = nxt(R, "pS")
                P.mm(pS.v, b.kt[pb:pb + K, tok], b.qt[pb:pb + K, tok])
                P.tt("dve", scT[hi].v, pS.v, R.maskT.v, ALU.mult)
            for h in range(2):
                m = ML[h]
                pS = nxt(R, "pS")
                P.mm(pS.v, m.kT[:, tok], m.qT[:, tok])
                P.stt("dve", scT[4 + h].v, pS.v, gtm[:, pr, 0, h:h + 1], R.maskT.v, ALU.mult, ALU.mult)
            c0, c1 = 2 * pr, 2 * pr + 1
            gc0 = it * NCH + c0
            r0, r1 = slice(0, 64), slice(64, 128)
            t0c, t1c = slice(c0 * CH, (c0 + 1) * CH), slice(c1 * CH, (c1 + 1) * CH)

            def dl_update(hi, c, rows, dst_par):
                b, pb, K, sf, sbb, vc, oc = dl[hi]
                pk = slice(pb, pb + K)
                pKV = nxt(R, "pKV")
                P.mm(pKV[pk, 0:128], b.ktm[rows, pr, pk], vtm[rows, pr, vc:vc + 128])
                tk = tmpkv[hi % 3]
                P.act(tk[pk, :], pKV[pk, 0:128], AF.Copy, scale=b.e3[pk, 2, c:c + 1])
                P.stt("dve", sf[pk, :], sf[pk, :], b.e3[pk, 1, c:c + 1], tk[pk, :], ALU.mult, ALU.add)
                P.copy("pool", sbb[dst_par][pk, :], sf[pk, :])

            for hi in range(4):
                dl_update(hi, c0, r0, 1)
            for h in range(2):
                m = ML[h]
                sf = st_f["m%d" % h]; sbb = st_b["m%d" % h]; sP = mstP if h == 0 else mstP2
                P.ts("dve", sP.v, sf.v, m.wp[:, c0:c0 + 1], ALU.mult)
                P.act(sbb[0].v, sf.v, AF.Copy, scale=m.wp[:, c0:c0 + 1])
                pKV = nxt(R, "pKV")
                P.mm(pKV[:, 0:129], m.khat[r0, pr, :], m.vaug[r0, pr, 0:129])
                P.tt("dve", sf.v, sP.v, pKV[:, 0:129], ALU.add)
                P.ts("dve", sP.v, sf.v, m.wp[:, c1:c1 + 1], ALU.mult)
                P.act(sbb[1].v, sf.v, AF.Copy, scale=m.wp[:, c1:c1 + 1])
            for hi, (b, pb, K, sf, sbb, vc, oc) in enumerate(dl):
                pk = slice(pb, pb + K)
                pO = R.pO[hi]
                P.mm(pO[r0, 0:128], b.qe[pk, t0c], sbb[0][pk, :], start=True, stop=False)
                P.mm(pO[r1, 0:128], b.qe[pk, t1c], sbb[1][pk, :], start=True, stop=False)
                P.mm(pO[:, 0:128], scT[hi].v, vtm[:, pr, vc:vc + 128], start=False, stop=True)
                P.copy("act", osb[:, pr, oc:oc + 128], pO[:, 0:128])
            for h in range(2):
                m = ML[h]
                sbb = st_b["m%d" % h]
                pO = R.pO[4 + h]
                P.mm(pO[r0, 0:129], m.qT[:, t0c], sbb[0].v, start=True, stop=False)
                P.mm(pO[r1, 0:129], m.qT[:, t1c], sbb[1].v, start=True, stop=False)
                P.mm(pO[:, 0:129], scT[4 + h].v, m.vaug[:, pr, 0:129], start=False, stop=True)
                P.act(den[:, 0:1], pO[:, 128:129], AF.Abs)
                P.tt("dve", den[:, 1:2], den[:, 0:1], gtm[:, pr, 1, h:h + 1], ALU.max)
                P.recip(den[:, 2:3], den[:, 1:2])
                P.act(osb[:, pr, 512 + h * 128:512 + (h + 1) * 128], pO[:, 0:128], AF.Copy, scale=den[:, 2:3])
            for hi in range(4):
                dl_update(hi, c1, r1, 0)
            for h in range(2):
                m = ML[h]
                sf = st_f["m%d" % h]; sP = mstP if h == 0 else mstP2
                pKV = nxt(R, "pKV")
                P.mm(pKV[:, 0:129], m.khat[r1, pr, :], m.vaug[r1, pr, 0:129])
                P.tt("dve", sf.v, sP.v, pKV[:, 0:129], ALU.add)

        if STOP <= 4:
            continue
        P.tt("pool", osq, osb[:, :, 0:768], osb[:, :, 0:768], ALU.mult)
        P.reduce("dve", st6.v, osq.rr("p j (h e) -> p j h e", e=128), ALU.add)
        P.reduce("dve", msum.v, osb[:, :, 512:768].rr("p j (h e) -> p j h e", e=128), ALU.add)
        P.ts("dve", mmean.v, msum.v, 1.0 / 128, ALU.mult)
        P.tt("dve", mvar.v, mmean.v, mmean.v, ALU.mult)
        P.ts("dve", st6b.v, st6.v, 1.0 / 128, ALU.mult)
        P.tt("dve", st6b[:, :, 4:6], st6b[:, :, 4:6], mvar.v, ALU.subtract)
        P.ts("dve", st6b.v, st6b.v, EPS, ALU.add)
        P.act(st6c.v, st6b.v, AF.Ln)
        P.act(st6c.v, st6c.v, AF.Exp, scale=-0.5)
        o4 = osb[:, :, 512:768].rr("p j (h e) -> p j h e", e=128)
        P.tt("pool", o4, o4, mmean.v.unsq(3).bc([128, NJ, 2, 128]), ALU.subtract)
        oall = osb[:, :, 0:768].rr("p j (h e) -> p j h e", e=128)
        P.tt("dve", oall, oall, st6c.v.unsq(3).bc([128, NJ, 6, 128]), ALU.mult)
        P.tt("pool", osb[:, :, 0:768], osb[:, :, 0:768], grow.v.unsq(1).bc([128, NJ, 768]), ALU.mult)
        P.tt("dve", osb[:, :, 512:768], osb[:, :, 512:768], sctm.v, ALU.add)
        P.tt("pool", mixb[:, :, 0:768], osb[:, :, 0:768], ztm.v, ALU.mult)
        for cc in range(6):
            pt = nxt(R, "pT")
            for j in range(NJ):
                P.tr(pt[:, j * 128:(j + 1) * 128], mixb[:, j, cc * 128:(cc + 1) * 128], R.identb.v)
            P.copy("act" if cc % 2 == 0 else "dve", mixT[:, cc, :], pt.v)
        for j in range(NJ):
            yb = ysb[j % 2]
            for nh in range(2):
                px = nxt(R, "pX")
                for cc in range(6):
                    P.mm(px.v, mixT[:, cc, j * 128:(j + 1) * 128], Wo[:, cc, nh * 512:(nh + 1) * 512], start=(cc == 0), stop=(cc == 5))
                P.copy("act" if nh == 0 else "dve", yb[:, nh * 512:(nh + 1) * 512], px.v)
            P.dma("sp", y_d(t0, j), yb.v)
        if after_tile is not None:
            after_tile(it)


def consts_np():
    ident = np.eye(128, dtype=np.float32)
    maskT = np.zeros((128, 128), np.float32)
    for a in range(2):
        blk = np.triu(np.ones((CH, CH), np.float32))
        maskT[a * CH:(a + 1) * CH, a * CH:(a + 1) * CH] = blk
    bdmask = np.kron(np.eye(32, dtype=np.float32), np.ones((4, 1), np.float32))
    sel = np.zeros((2, 2, 128), np.float32)
    sel[0, 0, :] = 1.0
    sel[1, 1, :] = 1.0
    return {"ident": ident, "maskT": maskT, "bdmask": bdmask, "sel": sel}


CONST_SHAPES = {"ident": [128, 128], "maskT": [128, 128], "bdmask": [128, 32], "sel": [2, 2, 128]}

MIX_SHAPES = {
    "wc": [D, NCOL], "wo": [768, D], "gmix": [128, 8], "lblog": [128, 2, DEPTH], "hnorm": [128], "gnorm": [128],
    "mnorm": [256], "gup": [16, 128], "gbias": [128, 1], "convw": [128, 2, 4], "convb": [128, 2], "skip": [128, 2],
    "bi": [2, 1], "bf": [2, 1], "wq": [128, 2, 4], "wk": [128, 2, 4], "wv": [128, 2, 4],
}


def mixer_weights_np(inp, l, hh):
    w_in = inp["w_in"][l]
    h2 = slice(hh * 256, (hh + 1) * 256)
    h1 = slice(hh * 128, (hh + 1) * 128)

    def cols(i, sl):
        return w_in[:, OFF[i] + sl.start:OFF[i] + sl.stop]

    wc = np.concatenate([
        cols(0, h2), cols(1, h2), cols(4, h1), cols(5, h1), cols(9, h2),
        w_in[:, OFF[7]:OFF[8]], cols(11, slice(hh * 2, hh * 2 + 2)), cols(12, slice(hh * 2, hh * 2 + 2)),
        cols(2, h2), cols(6, h2), cols(3, h2), cols(8, h2), cols(10, h2)], axis=1)
    assert wc.shape[1] == NCOL
    w_out = inp["w_out"][l]
    wo = np.concatenate([w_out[hh * 256:(hh + 1) * 256], w_out[512 + hh * 256:512 + (hh + 1) * 256],
                         w_out[1024 + hh * 256:1024 + (hh + 1) * 256]], axis=0)
    d = {
        "wc": wc, "wo": wo,
        "gmix": inp["norm_mix"][l].reshape(8, 128).T,
        "lblog": inp["hgrn_lb_logits"][:, h2].reshape(DEPTH, 2, 128).transpose(2, 1, 0),
        "hnorm": inp["hgrn_norm"][l], "gnorm": inp["gla_norm"][l],
        "mnorm": inp["mlstm_norm"][l][h2],
        "gup": inp["gla_gate_up"][l][:, h1],
        "gbias": inp["gla_gate_bias"][l][h1].reshape(128, 1),
        "convw": inp["mlstm_conv_w"][l][:, h2].reshape(4, 2, 128).transpose(2, 1, 0),
        "convb": inp["mlstm_conv_b"][l][h2].reshape(2, 128).T,
        "skip": inp["mlstm_skip"][l][h2].reshape(2, 128).T,
        "bi": inp["mlstm_igate_bias"][l][hh * 2:hh * 2 + 2].reshape(2, 1),
        "bf": inp["mlstm_fgate_bias"][l][hh * 2:hh * 2 + 2].reshape(2, 1),
    }
    for nm in ("wq", "wk", "wv"):
        w = inp["mlstm_" + nm][l]
        d[nm] = w[hh * 64:(hh + 1) * 64].reshape(2, 128, 4).transpose(1, 0, 2)
    return {k: np.ascontiguousarray(v, dtype=np.float32) for k, v in d.items()}


def build_mixer_prog(S, layer):
    nc = bass.Bass("TRN2", target_bir_lowering=False)
    P = Prog(nc)
    cd = {k: P.dram_in("c_" + k, shp) for k, shp in CONST_SHAPES.items()}
    wd = {k: P.dram_in("w_" + k, shp) for k, shp in MIX_SHAPES.items()}
    x_d = P.dram_in("x", [S, D])
    y_d = P.dram_out("y", [S, D])
    R = setup_common(P, cd)
    emit_mixer(P, R, S, lambda t0: x_d[t0:t0 + T, :], lambda t0, j: y_d[t0 + j * 128:t0 + (j + 1) * 128, :], wd, layer)
    P.finish("sp", [y_d])
    P.es.close()
    return nc, P


XA_SHAPES = {"wq": [D, D], "wk": [D, D], "wv": [D, D], "wo": [D, D], "gx": [128, 8], "gm": [128, 8], "gfin": [D]}


def emit_xattn(P, R, S2, x_d, ys_d, mem_d, out_d, wd, final, after_tile=None):
    NT = S2 // T
    stage = P.sb("xstage", [128, D])
    Wq = P.sb("Wq", [128, 8, D], BF16)
    Wo = P.sb("Wo2", [128, 8, D], BF16)
    kT = P.sb("kT", [128, 8, MEM], BF16)
    vtm = P.sb("xvtm", [128, 2, D], BF16)
    gx = P.sb("gx", [128, 8]); P.dma("sp", gx.v, wd["gx"].v)
    gm = P.sb("gm", [128, 8]); P.dma("sp", gm.v, wd["gm"].v)
    onesb = P.sb("onesb", [128, 128], BF16); P.memset("pool", onesb.v, 1.0)
    gfin = None
    if final:
        gfin = P.sb("gfin", [128, D]); P.dma("sp", gfin.v, wd["gfin"].v.pbc(128))
    xt = P.sb("x_xt", [128, NJ, D])
    yt = P.sb("x_yt", [128, NJ, D])
    scr = (P.sb("x_ss", [128, NJ]), P.sb("x_sstmp", [128, NJ]), P.sb("x_rs", [128, NJ]),
           P.sb("x_junk", [128, D], BF16), P.sb("x_xn", [128, NJ, D], BF16))
    hT = P.sb("x_hT", [128, 8, T], BF16)
    P.push()
    Wk = P.sb("Wk", [128, 8, D], BF16)
    Wv = P.sb("Wv", [128, 8, D], BF16)
    load_cast_weight(P, Wk, wd["wk"], 8, D, [stage], ["pool", "dve"])
    load_cast_weight(P, Wv, wd["wv"], 8, D, [stage], ["pool", "dve"])
    P.dma("sp", xt[:, 0:2, :], mem_d.v.rr("(j p) d -> p j d", p=128))
    rms_to_hT(P, R, xt, hT, gm, 2, scr)
    for cb in range(8):
        px = nxt(R, "pX")
        for k in range(8):
            P.mm(px[:, 0:MEM], Wk[:, k, cb * 128:(cb + 1) * 128], hT[:, k, 0:MEM], start=(k == 0), stop=(k == 7))
        P.copy("act" if cb % 2 == 0 else "dve", kT[:, cb, :], px[:, 0:MEM])
    for mj in range(2):
        for nh in range(2):
            px = nxt(R, "pX")
            for k in range(8):
                P.mm(px.v, hT[:, k, mj * 128:(mj + 1) * 128], Wv[:, k, nh * 512:(nh + 1) * 512], start=(k == 0), stop=(k == 7))
            P.copy("act" if nh == 0 else "dve", vtm[:, mj, nh * 512:(nh + 1) * 512], px.v)
    P.pop()
    load_cast_weight(P, Wq, wd["wq"], 8, D, [stage], ["pool", "dve"])
    load_cast_weight(P, Wo, wd["wo"], 8, D, [stage], ["pool", "dve"])
    qT = P.sb("x_qT", [128, 8, T], BF16)
    pT = [P.sb("x_pT%d" % i, [128, T], BF16) for i in range(2)]
    rinv = P.sb("x_rinv", [128, T])
    oT = P.sb("x_oT", [128, 8, T], BF16)
    for it in range(NT):
        t0 = it * T
        P.dma("sp", xt.v, x_d(t0).rr("(j p) d -> p j d", p=128))
        for yi, y_d in enumerate(ys_d):
            P.dma("act", yt.v, y_d(t0).rr("(j p) d -> p j d", p=128))
            P.tt("dve" if yi == 0 else "pool", xt.v, xt.v, yt.v, ALU.add)
        rms_to_hT(P, R, xt, hT, gx, NJ, scr)
        for cb in range(8):
            px = nxt(R, "pX")
            for k in range(8):
                P.mm(px.v, Wq[:, k, cb * 128:(cb + 1) * 128], hT[:, k, :], start=(k == 0), stop=(k == 7))
            P.copy("act" if cb % 2 == 0 else "dve", qT[:, cb, :], px.v)
        for hd in range(4):
            for mj in range(2):
                px = nxt(R, "pX")
                for i2 in range(2):
                    cb = hd * 2 + i2
                    P.mm(px.v, kT[:, cb, mj * 128:(mj + 1) * 128], qT[:, cb, :], start=(i2 == 0), stop=(i2 == 1))
                P.act(pT[mj].v, px.v, AF.Exp, scale=1.0 / 16.0)
            px = nxt(R, "pX")
            for mj in range(2):
                P.mm(px.v, onesb.v, pT[mj].v, start=(mj == 0), stop=(mj == 1))
            P.recip(rinv.v, px.v)
            for e2 in range(2):
                px = nxt(R, "pX")
                c0 = hd * 256 + e2 * 128
                for mj in range(2):
                    P.mm(px.v, vtm[:, mj, c0:c0 + 128], pT[mj].v, start=(mj == 0), stop=(mj == 1))
                P.tt("dve", oT[:, hd * 2 + e2, :], px.v, rinv.v, ALU.mult)
        for j in range(NJ):
            for nh in range(2):
                px = nxt(R, "pX")
                for cb in range(8):
                    P.mm(px.v, oT[:, cb, j * 128:(j + 1) * 128], Wo[:, cb, nh * 512:(nh + 1) * 512], start=(cb == 0), stop=(cb == 7))
                P.tt("dve", xt[:, j, nh * 512:(nh + 1) * 512], px.v, xt[:, j, nh * 512:(nh + 1) * 512], ALU.add)
        if final:
            ss, tmp, rs, junk, xn = scr
            P.memset("pool", ss.v, 0.0)
            for j in range(NJ):
                P.act(junk.v, xt[:, j, :], AF.Square, accum=ss[:, j:j + 1])
            P.rstd(rs.v, ss.v, 1.0 / D, tmp.v)
            for j in range(NJ):
                P.act(xt[:, j, :], xt[:, j, :], AF.Copy, scale=rs[:, j:j + 1])
                P.tt("pool", xt[:, j, :], xt[:, j, :], gfin.v, ALU.mult)
        P.dma("sp", out_d(t0).rr("(j p) d -> p j d", p=128), xt.v)
        if after_tile is not None:
            after_tile(it)


def xattn_weights_np(inp, l):
    d = {"wq": inp["xa_wq"][l], "wk": inp["xa_wk"][l], "wv": inp["xa_wv"][l], "wo": inp["xa_wo"][l],
         "gx": inp["norm_xattn"][l].reshape(8, 128).T, "gm": inp["norm_mem"][l].reshape(8, 128).T,
         "gfin": inp["norm_final"]}
    return {k: np.ascontiguousarray(v, dtype=np.float32) for k, v in d.items()}


def build_xattn_prog(S2, final, ny=2):
    nc = bass.Bass("TRN2", target_bir_lowering=False)
    P = Prog(nc)
    cd = {k: P.dram_in("c_" + k, shp) for k, shp in CONST_SHAPES.items()}
    wd = {k: P.dram_in("w_" + k, shp) for k, shp in XA_SHAPES.items()}
    x_d = P.dram_in("x", [S2, D])
    ys_d = [P.dram_in("y%d" % i, [S2, D]) for i in range(ny)]
    mem_d = P.dram_in("mem", [MEM, D])
    out_d = P.dram_out("out", [S2, D])
    R = setup_common(P, cd)
    emit_xattn(P, R, S2, lambda t0: x_d[t0:t0 + T, :], [(lambda t0, y=y: y[t0:t0 + T, :]) for y in ys_d], mem_d, lambda t0: out_d[t0:t0 + T, :], wd, final)
    P.finish("sp", [out_d])
    P.es.close()
    return nc, P


N_CORES = 8
BATCH = 4
SEQ = 8192
PAIRS = [[0, 1], [2, 3], [4, 5], [6, 7]]


def build_fused(S):
    S2 = S // 2
    nc = bass.Bass("TRN2", target_bir_lowering=False)
    P = Prog(nc)
    cd = {k: P.dram_in("c_" + k, shp) for k, shp in CONST_SHAPES.items()}
    mwd = [{k: P.dram_in("m%d_%s" % (l, k), shp) for k, shp in MIX_SHAPES.items()} for l in range(DEPTH)]
    xwd = [{k: P.dram_in("a%d_%s" % (l, k), shp) for k, shp in XA_SHAPES.items()} for l in range(DEPTH)]
    x_full = P.dram_in("x_full", [S, D])
    x_half = P.dram_in("x_half", [S2, D])
    mem_d = P.dram_in("mem", [MEM, D])
    out_d = P.dram_out("out", [S2, D])
    NC2 = S2 // T
    yp = [P.dram_tmp("yp%d" % c, [2 * T, D]) for c in range(NC2)]
    ysum = [P.dram_tmp("ysum%d" % c, [T, D]) for c in range(NC2)]
    xh = [P.dram_tmp("xh%d" % c, [T, D]) for c in range(NC2)]
    xf = [P.dram_tmp("xf%d" % c, [2 * T, D]) for c in range(NC2)]
    R = setup_common(P, cd)

    def full_from_xf(t0):
        s_, u = t0 // S2, t0 % S2
        return xf[u // T][s_ * T:(s_ + 1) * T, :]

    def y_dst(t0, j):
        s_, u = t0 // S2, t0 % S2
        return yp[u // T][s_ * T + j * 128:s_ * T + (j + 1) * 128, :]

    def rs_after(it):
        NT = S // T
        if it - 1 >= NC2:
            c = it - 1 - NC2
            P.collective("ReduceScatter", ALU.add, PAIRS, yp[c], ysum[c])
        if it == NT - 1:
            P.collective("ReduceScatter", ALU.add, PAIRS, yp[NC2 - 1], ysum[NC2 - 1])

    def ag_after(it):
        if it >= 1:
            P.collective("AllGather", ALU.bypass, PAIRS, xh[it - 1], xf[it - 1])
        if it == NC2 - 1:
            P.collective("AllGather", ALU.bypass, PAIRS, xh[it], xf[it])

    full_src = lambda t0: x_full[t0:t0 + T, :]
    half_src = lambda t0: x_half[t0:t0 + T, :]
    for l in range(DEPTH):
        final = (l == DEPTH - 1)
        P.push()
        emit_mixer(P, R, S, full_src, y_dst, mwd[l], l, after_tile=rs_after)
        P.barrier()
        P.pop()
        P.push()
        dst = (lambda t0: out_d[t0:t0 + T, :]) if final else (lambda t0: xh[t0 // T].v)
        emit_xattn(P, R, S2, half_src, [lambda t0: ysum[t0 // T].v], mem_d, dst, xwd[l], final,
                   after_tile=None if final else ag_after)
        P.barrier()
        P.pop()
        if not final:
            full_src = full_from_xf
            half_src = lambda t0: xh[t0 // T].v
    for i in range(PADPE):
        P.mm(R.pSm[0:1, 0:1], R.ident[0:1, 0:1], R.ident[0:1, 0:1])
    for i in range(PADV):
        P.memset("dve" if i % 2 == 0 else "act", R.bdmask[0:1, 0:1], 1.0) if i % 2 == 0 else P.act(R.sel[0:1, 0, 0:1], R.sel[0:1, 0, 0:1], AF.Copy)
    if PAD:
        padt = P.sb("padt", [128, 4096])
        P.memset("pool", padt.v, 0.5)
        for i in range(PAD):
            P.act(padt.v, padt.v, AF.Copy)
    P.finish("sp", [out_d])
    P.es.close()
    return nc, P


PAD = 0
PADPE = 0
PADV = 0


def fused_inputs(inp, S):
    S2 = S // 2
    cn = {"c_" + k: v for k, v in consts_np().items()}
    mw = [[mixer_weights_np(inp, l, hh) for hh in range(2)] for l in range(DEPTH)]
    xw = [xattn_weights_np(inp, l) for l in range(DEPTH)]
    maps = []
    for i in range(N_CORES):
        b, hh = i // 2, i % 2
        xb = np.ascontiguousarray(inp["x"][b, :S], dtype=np.float32)
        m = {"x_full": xb, "x_half": np.ascontiguousarray(xb[hh * S2:(hh + 1) * S2]),
             "mem": np.ascontiguousarray(inp["mem"][b], dtype=np.float32)}
        m.update(cn)
        for l in range(DEPTH):
            m.update({"m%d_%s" % (l, k): v for k, v in mw[l][hh].items()})
            m.update({"a%d_%s" % (l, k): v for k, v in xw[l].items()})
        maps.append(m)
    return maps


def kernel(**inp):
    inp = {k: np.asarray(v) for k, v in inp.items()}
    S = inp["x"].shape[1]
    nc, _ = build_fused(S)
    maps = fused_inputs(inp, S)
    res = run_bass_kernel_spmd(nc, maps, core_ids=list(range(N_CORES)))
    outs = [r["out"] for r in res.results]
    full = [np.concatenate([outs[2 * b], outs[2 * b + 1]], axis=0) for b in range(BATCH)]
    return np.stack(full, axis=0).astype(np.float32)
```

```python
import contextlib
import numpy as np
import concourse.bass as bass
import concourse.mybir as mybir
from concourse.bass_utils import run_bass_kernel_spmd

F32 = mybir.dt.float32
BF16 = mybir.dt.bfloat16
AF = mybir.ActivationFunctionType
ALU = mybir.AluOpType
AX = mybir.AxisListType

D = 1024
DEPTH = 2
CH = 64
T = 512
NJ = T // 128
NCH = T // CH
EPS = 1e-6
MEM = 256
IN_SPLITS = [512, 512, 512, 512, 256, 256, 512, 16, 512, 512, 512, 4, 4]
OFF = np.concatenate([[0], np.cumsum(IN_SPLITS)]).tolist()
C_HQ, C_HF, C_GQ, C_GK, C_MU, C_GA, C_MI, C_MF = 0, 256, 512, 640, 768, 1024, 1040, 1042
C_TM = 1044
NCOL = C_TM + 1280


class V:
    __slots__ = ("buf", "ap")

    def __init__(self, buf, ap):
        self.buf = buf
        self.ap = ap

    def __getitem__(self, k):
        return V(self.buf, self.ap[k])

    def rr(self, s, **kw):
        return V(self.buf, self.ap.rearrange(s, **kw))

    def bc(self, shape):
        return V(self.buf, self.ap.to_broadcast(list(shape)))

    def unsq(self, axis):
        return V(self.buf, self.ap.unsqueeze(axis))

    def pbc(self, n):
        return V(self.buf, self.ap.partition_broadcast(n))

    @property
    def v(self):
        return self


class Buf:
    __slots__ = ("name", "t", "w", "r", "dsem", "dcnt", "excl")

    def __init__(self, name, t):
        self.name = name
        self.t = t
        self.excl = False
        self.w = {}
        self.r = {}
        self.dsem = None
        self.dcnt = 0

    def __getitem__(self, k):
        return V(self, self.t[k])

    @property
    def v(self):
        return V(self, self.t[:])


def _u(x):
    return x.ap if isinstance(x, V) else x


class Prog:
    def __init__(self, nc):
        self.nc = nc
        self.es = contextlib.ExitStack()
        self.eng = {"pe": nc.tensor, "act": nc.scalar, "dve": nc.vector, "pool": nc.gpsimd, "sp": nc.sync}
        self.sem = {}
        self.cnt = {}
        self.seen = {}
        for k in self.eng:
            self.sem[k] = self.es.enter_context(nc.semaphore("s_" + k))
            self.cnt[k] = 0
            self.seen[k] = {}
        self.nbuf = 0
        self.ninst = 0
        self.uid = 0
        self.stack = []
        self.root = self.es
        self.dcounts = {}
        self.nwait = {}

    def push(self):
        self.stack.append(self.es)
        self.es = contextlib.ExitStack()

    def pop(self):
        self.es.close()
        self.es = self.stack.pop()

    def sb(self, name, shape, dt=F32):
        self.uid += 1
        t = self.es.enter_context(self.nc.sbuf_tensor("sb%d_%s" % (self.uid, name), list(shape), dt))
        return Buf(name, t)

    def ps(self, name, shape, dt=F32):
        self.uid += 1
        t = self.es.enter_context(self.nc.psum_tensor("ps%d_%s" % (self.uid, name), list(shape), dt))
        b = Buf(name, t)
        b.excl = True
        return b

    def dram_in(self, name, shape, dt=F32):
        return Buf(name, self.nc.dram_tensor(name, list(shape), dt, kind="ExternalInput").ap())

    def dram_out(self, name, shape, dt=F32):
        return Buf(name, self.nc.dram_tensor(name, list(shape), dt, kind="ExternalOutput").ap())

    def dram_tmp(self, name, shape, dt=F32):
        return Buf(name, self.nc.dram_tensor(name, list(shape), dt, kind="Internal").ap())

    def alias(self, name, buf, ap):
        return V(buf, ap)

    def _wait(self, e, deps):
        for k, v in deps.items():
            if self.seen[e].get(k, 0) >= v:
                continue
            if k == e and e == "pe":
                continue
            self.eng[e].wait_ge(self.sem[k], v)
            self.nwait[e] = self.nwait.get(e, 0) + 1
            self.seen[e][k] = v

    @staticmethod
    def _deps(reads, writes, e=None):
        deps = {}
        for b in reads:
            for k, v in b.w.items():
                if deps.get(k, 0) < v:
                    deps[k] = v
            if b.excl:
                for k, v in b.r.items():
                    if k != e and deps.get(k, 0) < v:
                        deps[k] = v
        for b in writes:
            for d in (b.w, b.r):
                for k, v in d.items():
                    if deps.get(k, 0) < v:
                        deps[k] = v
        return deps

    def op(self, e, fn, ins, outs):
        reads = []
        for x in ins:
            if isinstance(x, V) and x.buf not in reads:
                reads.append(x.buf)
        writes = []
        for x in outs:
            if isinstance(x, V) and x.buf not in writes:
                writes.append(x.buf)
        self._wait(e, self._deps(reads, writes, e))
        ins_ = fn(self.eng[e])
        self.cnt[e] += 1
        c = self.cnt[e]
        ins_.then_inc(self.sem[e], 1)
        for b in writes:
            b.w = {e: c}
            b.r = {}
        for b in reads:
            if b not in writes:
                b.r[e] = c
        self.ninst += 1

    def dma(self, e, out, in_):
        reads = [in_.buf]
        writes = [out.buf]
        self._wait(e, self._deps(reads, writes))
        sbuf = out.buf
        if sbuf.dsem is None:
            key = "d%d" % self.nbuf
            self.nbuf += 1
            self.sem[key] = self.root.enter_context(self.nc.semaphore(key))
            sbuf.dsem = key
        self.eng[e].dma_start(out=out.ap, in_=in_.ap).then_inc(self.sem[sbuf.dsem], 16)
        sbuf.dcnt += 16
        self.dcounts[sbuf.dsem] = sbuf.dcnt
        out.buf.w = {sbuf.dsem: sbuf.dcnt}
        out.buf.r = {}
        in_.buf.r[sbuf.dsem] = sbuf.dcnt
        self.ninst += 1

    def barrier(self):
        deps = {k: v for k, v in self.cnt.items() if v > 0}
        deps.update(self.dcounts)
        for e in self.eng:
            self._wait(e, {k: v for k, v in deps.items() if k != e})

    def collective(self, kind, op, groups, in_buf, out_buf):
        e = "pool"
        self._wait(e, self._deps([in_buf], [out_buf], e))
        if out_buf.dsem is None:
            key = "c%d" % self.nbuf
            self.nbuf += 1
            self.sem[key] = self.root.enter_context(self.nc.semaphore(key))
            out_buf.dsem = key
        self.nc.gpsimd.collective_compute(kind, op, replica_groups=groups, ins=[in_buf.t.opt()],
                                          outs=[out_buf.t.opt()]).then_inc(self.sem[out_buf.dsem])
        out_buf.dcnt += 1
        self.dcounts[out_buf.dsem] = out_buf.dcnt
        out_buf.w = {out_buf.dsem: out_buf.dcnt}
        out_buf.r = {}
        in_buf.r[out_buf.dsem] = out_buf.dcnt
        self.ninst += 1

    def finish(self, e, bufs):
        self._wait(e, self._deps((), bufs))

    def act(self, out, in_, func, bias=None, scale=None, accum=None, e="act"):
        kw = {}
        if bias is not None:
            kw["bias"] = _u(bias)
        if scale is not None:
            kw["scale"] = _u(scale)
        if accum is not None:
            kw["accum_out"] = _u(accum)
        outs = [out] + ([accum] if accum is not None else [])
        self.op(e, lambda g: g.activation(out=out.ap, in_=in_.ap, func=func, **kw), [in_, bias, scale], outs)

    def tt(self, e, out, in0, in1, op):
        self.op(e, lambda g: g.tensor_tensor(out=out.ap, in0=in0.ap, in1=in1.ap, op=op), [in0, in1], [out])

    def ts(self, e, out, in0, s1, op0, s2=None, op1=None):
        if op1 is None:
            self.op(e, lambda g: g.tensor_scalar(out=out.ap, in0=in0.ap, scalar1=_u(s1), scalar2=None, op0=op0),
                    [in0, s1], [out])
        else:
            self.op(e, lambda g: g.tensor_scalar(out=out.ap, in0=in0.ap, scalar1=_u(s1), scalar2=_u(s2), op0=op0, op1=op1),
                    [in0, s1, s2], [out])

    def stt(self, e, out, in0, scalar, in1, op0, op1):
        self.op(e, lambda g: g.scalar_tensor_tensor(out=out.ap, in0=in0.ap, scalar=_u(scalar), in1=in1.ap, op0=op0, op1=op1),
                [in0, scalar, in1], [out])

    def copy(self, e, out, in_):
        if e == "act":
            self.act(out, in_, AF.Copy)
        else:
            self.op(e, lambda g: g.tensor_copy(out=out.ap, in_=in_.ap), [in_], [out])

    def memset(self, e, out, val):
        self.op(e, lambda g: g.memset(out.ap, val), [], [out])

    def scan(self, out, d0, d1, init, op0, op1):
        self.op("dve", lambda g: g.tensor_tensor_scan(out=out.ap, data0=d0.ap, data1=d1.ap, initial=_u(init), op0=op0, op1=op1),
                [d0, d1, init], [out])

    def reduce(self, e, out, in_, op, axis=AX.X):
        self.op(e, lambda g: g.tensor_reduce(out=out.ap, in_=in_.ap, axis=axis, op=op), [in_], [out])

    def recip(self, out, in_):
        self.op("dve", lambda g: g.reciprocal(out=out.ap, in_=in_.ap), [in_], [out])

    def mm(self, out, lhsT, rhs, start=True, stop=True):
        self.op("pe", lambda g: g.matmul(out.ap, lhsT=lhsT.ap, rhs=rhs.ap, start=start, stop=stop), [lhsT, rhs], [out])

    def tr(self, out, in_, ident):
        self.op("pe", lambda g: g.transpose(out=out.ap, in_=in_.ap, identity=ident.ap), [in_, ident], [out])

    def rstd(self, out, in_, scale, tmp):
        self.ts("dve", tmp, in_, scale, ALU.mult, EPS, ALU.add)
        self.act(tmp, tmp, AF.Ln)
        self.act(out, tmp, AF.Exp, scale=-0.5)


class Res:
    pass


def setup_common(P, consts_d):
    R = Res()
    R.ident = P.sb("ident", [128, 128])
    R.identb = P.sb("identb", [128, 128], BF16)
    R.maskT = P.sb("maskT", [128, 128])
    R.bdmask = P.sb("bdmask", [128, 32])
    R.sel = P.sb("sel", [2, 2, 128])
    P.dma("sp", R.ident.v, consts_d["ident"].v)
    P.dma("sp", R.maskT.v, consts_d["maskT"].v)
    P.dma("sp", R.bdmask.v, consts_d["bdmask"].v)
    P.dma("sp", R.sel.v, consts_d["sel"].v)
    P.copy("dve", R.identb.v, R.ident.v)
    R.pX = [P.ps("pX%d" % i, [128, 512]) for i in range(2)]
    bT = P.ps("bT", [128, 1024], BF16)
    R.pT = [P.alias("pT%d" % i, bT, bT.t[:, i * 512:(i + 1) * 512]) for i in range(2)]
    bO = [P.ps("bO%d" % i, [128, 512]) for i in range(2)]
    R.pO = [P.alias("pO%d" % i, bO[i % 2], bO[i % 2].t[:, (i // 2) * 160:(i // 2) * 160 + 132]) for i in range(6)]
    bS = P.ps("bS", [128, 512])
    R.pS = [P.alias("pS%d" % i, bS, bS.t[:, i * 128:(i + 1) * 128]) for i in range(4)]
    bKV = P.ps("bKV", [128, 512])
    R.pKV = [P.alias("pKV%d" % i, bKV, bKV.t[:, i * 160:i * 160 + 132]) for i in range(3)]
    bSm = P.ps("bSm", [128, 512])
    R.pSm = P.alias("pSm", bSm, bSm.t[:, 0:64])
    R.pX = R.pX + [bO[0], bO[1], bS, bKV]
    R.px_i = 0
    R.ps_i = 0
    R.pkv_i = 0
    R.pt_i = 0
    return R


def nxt(R, what):
    if what == "pX":
        R.px_i += 1
        return R.pX[R.px_i % len(R.pX)]
    if what == "pS":
        R.ps_i += 1
        return R.pS[R.ps_i % len(R.pS)]
    if what == "pKV":
        R.pkv_i += 1
        return R.pKV[R.pkv_i % len(R.pKV)]
    if what == "pT":
        R.pt_i += 1
        return R.pT[R.pt_i % len(R.pT)]
    raise KeyError(what)


def rms_to_hT(P, R, xt, hT, gcol, ntok_j, scr):
    nj = ntok_j
    ss, tmp, rs, junk, xn = scr
    P.memset("pool", ss.v, 0.0)
    for j in range(nj):
        P.act(junk.v, xt[:, j, :], AF.Square, accum=ss[:, j:j + 1])
    P.rstd(rs[:, 0:nj], ss[:, 0:nj], 1.0 / D, tmp[:, 0:nj])
    for j in range(nj):
        if j % 2 == 0:
            P.act(xn[:, j, :], xt[:, j, :], AF.Copy, scale=rs[:, j:j + 1])
        else:
            P.ts("dve", xn[:, j, :], xt[:, j, :], rs[:, j:j + 1], ALU.mult)
    for k in range(8):
        pt = nxt(R, "pT")
        for j in range(nj):
            P.tr(pt[:, j * 128:(j + 1) * 128], xn[:, j, k * 128:(k + 1) * 128], R.identb.v)
        P.ts("dve", hT[:, k, 0:nj * 128], pt[:, 0:nj * 128], gcol[:, k:k + 1], ALU.mult)


def load_cast_weight(P, w_sb, w_d, nk, ncols, stage, eng_cycle):
    for k in range(nk):
        st = stage[k % len(stage)]
        P.dma("sp", st[:, 0:ncols], w_d[k * 128:(k + 1) * 128, :])
        P.copy(eng_cycle[k % len(eng_cycle)], w_sb[:, k, :], st[:, 0:ncols])


STOP = 99


def emit_mixer(P, R, S, x_d, y_d, wd, layer):
    NT = S // T
    stage = [P.sb("wstage%d" % i, [128, NCOL]) for i in range(1)]
    W = P.sb("W", [128, 8, NCOL], BF16)
    load_cast_weight(P, W, wd["wc"], 8, NCOL, stage, ["pool", "dve"])
    Wo = P.sb("Wo", [128, 6, D], BF16)
    load_cast_weight(P, Wo, wd["wo"], 6, D, stage, ["pool", "dve"])
    gmix = P.sb("gmix", [128, 8]); P.dma("sp", gmix.v, wd["gmix"].v)
    lbl = P.sb("lbl", [128, 2, DEPTH]); P.dma("sp", lbl.v, wd["lblog"].v)
    lbe = P.sb("lbe", [128, 2, DEPTH]); P.act(lbe.v, lbl.v, AF.Exp)
    lbtot = P.sb("lbtot", [128, 2]); P.reduce("dve", lbtot.v, lbe.v, ALU.add)
    lbr = P.sb("lbr", [128, 2]); P.recip(lbr.v, lbtot.v)
    lb = P.sb("lb", [128, 2]); P.memset("pool", lb.v, 0.0)
    for l2 in range(1, layer + 1):
        P.tt("dve", lb.v, lb.v, lbe[:, :, l2], ALU.add)
    P.tt("dve", lb.v, lb.v, lbr.v, ALU.mult)
    oml = P.sb("oml", [128, 2]); P.ts("dve", oml.v, lb.v, -1.0, ALU.mult, 1.0, ALU.add)
    lbm1 = P.sb("lbm1", [128, 2]); P.ts("dve", lbm1.v, lb.v, -1.0, ALU.add)
    grow = P.sb("grow", [128, 768])
    for i, nm in enumerate(["hnorm", "hnorm", "gnorm", "gnorm"]):
        P.dma("sp", grow[:, i * 128:(i + 1) * 128], wd[nm].v.pbc(128))
    P.dma("sp", grow[:, 512:768], wd["mnorm"].v.pbc(128))
    gup = P.sb("gup", [16, 128]); P.dma("sp", gup.v, wd["gup"].v)
    gbias = P.sb("gbias", [128, 1]); P.dma("sp", gbias.v, wd["gbias"].v)
    convw = P.sb("convw", [128, 2, 4]); P.dma("sp", convw.v, wd["convw"].v)
    convb = P.sb("convb", [128, 2]); P.dma("sp", convb.v, wd["convb"].v)
    skip = P.sb("skip", [128, 2]); P.dma("sp", skip.v, wd["skip"].v)
    bi = P.sb("bi", [2, 1]); P.dma("sp", bi.v, wd["bi"].v)
    bf = P.sb("bf", [2, 1]); P.dma("sp", bf.v, wd["bf"].v)
    BDq, BDks, BDv = [], [], []
    wsm = {}
    for nm in ("wq", "wk", "wv"):
        wsm[nm] = P.sb("wsm_" + nm, [128, 2, 4]); P.dma("sp", wsm[nm].v, wd[nm].v)
    for h in range(2):
        bq = P.sb("BDq%d" % h, [128, 128], BF16)
        bks = P.sb("BDks%d" % h, [128, 256], BF16)
        bv = P.sb("BDv%d" % h, [128, 128], BF16)
        for dst, nm in ((bq.v, "wq"), (bks[:, 0:128], "wk"), (bv.v, "wv")):
            P.tt("pool", dst.rr("p (g o) -> p g o", o=4),
                 wsm[nm][:, h, :].unsq(1).bc([128, 32, 4]),
                 R.bdmask.v.unsq(2).bc([128, 32, 4]), ALU.mult)
        P.ts("pool", bks[:, 128:256], R.ident.v, skip[:, h:h + 1], ALU.mult)
        BDq.append(bq); BDks.append(bks); BDv.append(bv)

    st_f = {}
    st_b = {}
    for nm, ncol in (("h0", 128), ("h1", 128), ("g", 128), ("m0", 129), ("m1", 129)):
        st_f[nm] = P.sb("stf_" + nm, [128, ncol])
        P.memset("pool", st_f[nm].v, 0.0)
    for nm, ncol in (("h0", 128), ("h1", 128), ("g0", 128), ("g1", 128), ("m0", 129), ("m1", 129)):
        st_b[nm] = [P.sb("stb_%s_%d" % (nm, i), [128, ncol], BF16) for i in range(2)]
        P.memset("pool", st_b[nm][0].v, 0.0)
    mstP = P.sb("mstP", [128, 129])
    mstP2 = P.sb("mstP2", [128, 129])
    uext = [P.sb("uext%d" % h, [128, 3 + T]) for h in range(2)]
    for h in range(2):
        P.memset("pool", uext[h][:, 0:3], 0.0)
    m0 = P.sb("m0", [2, 1]); P.memset("pool", m0.v, 0.0)

    xt = P.sb("xt", [128, NJ, D])
    scr = (P.sb("ss", [128, NJ]), P.sb("sstmp", [128, NJ]), P.sb("rs", [128, NJ]),
           P.sb("junk", [128, D], BF16), P.sb("xn", [128, NJ, D], BF16))
    hT = P.sb("hT", [128, 8, T], BF16)
    vtm = P.sb("vtm", [128, NJ, 512], BF16)
    ztm = P.sb("ztm", [128, NJ, 768])
    osb = xt
    ones = P.sb("ones", [128, T]); P.memset("pool", ones.v, 1.0)
    tmpA = P.sb("tmpA", [128, 8, T])
    tq, tsg, tg, tkk, tB, tDm, teD, teDn = [tmpA[:, i, :] for i in range(8)]
    blk = []
    for i in range(3):
        b = Res()
        b.q, b.sg, b.g, b.kk, b.B, b.Dm, b.eD, b.eDn = tq, tsg, tg, tkk, tB, tDm, teD, teDn
        b.qt = P.sb("qt%d" % i, [128, T], BF16)
        b.kt = P.sb("kt%d" % i, [128, T], BF16)
        b.qe = P.sb("qe%d" % i, [128, T], BF16)
        b.ktm = P.sb("ktm%d" % i, [128, NJ, 128], BF16)
        b.prev = P.sb("prev%d" % i, [128, NCH])
        b.d3 = P.sb("d3%d" % i, [128, 3, NCH])
        b.e3 = P.sb("e3%d" % i, [128, 3, NCH])
        blk.append(b)
    ga_sb = P.sb("ga_sb", [16, T])
    tmpkv = [P.sb("tmpkv%d" % i, [128, 128]) for i in range(3)]
    scT = [P.sb("scT%d" % i, [128, 128], BF16) for i in range(6)]
    ML = []
    for h in range(2):
        m = Res()
        m.acc = P.sb("cacc%d" % h, [128, T])
        m.conv = P.sb("conv%d" % h, [128, T], BF16)
        m.ub = P.sb("ub%d" % h, [128, T], BF16)
        m.qT = P.sb("mqT%d" % h, [128, T], BF16)
        m.kT = P.sb("mkT%d" % h, [128, T], BF16)
        m.khat = P.sb("khat%d" % h, [128, NJ, 128], BF16)
        m.vaug = P.sb("vaug%d" % h, [128, NJ, 132], BF16)
        P.memset("pool", m.vaug.v, 1.0)
        m.wp = P.sb("wp%d" % h, [128, NCH])
        ML.append(m)
    sctm = P.sb("sctm", [128, NJ, 256])
    g_sf = P.sb("g_sf", [2, T]); g_lf = P.sb("g_lf", [2, T]); g_B = P.sb("g_B", [2, T])
    g_a = P.sb("g_a", [2, T]); g_wj = P.sb("g_wj", [2, T]); g_thr = P.sb("g_thr", [2, T])
    g_am = P.sb("g_am", [2, NCH]); g_R = P.sb("g_R", [2, NCH]); g_mp = P.sb("g_mp", [2, NCH]); g_wprev = P.sb("g_wprev", [2, NCH])
    gtm = P.sb("gtm", [128, NJ, 2, 2])
    den = P.sb("den", [128, 4])
    st6 = P.sb("st6", [128, NJ, 6]); st6b = P.sb("st6b", [128, NJ, 6]); st6c = P.sb("st6c", [128, NJ, 6])
    msum = P.sb("msum", [128, NJ, 2]); mmean = P.sb("mmean", [128, NJ, 2]); mvar = P.sb("mvar", [128, NJ, 2])
    osq = tmpA.v.rr("p a t -> p (a t)")[:, 0:NJ * 768].rr("p (j c) -> p j c", c=768)
    mixb = scr[4]
    mixT = hT
    ysb = [P.sb("ysb%d" % i, [128, D]) for i in range(2)]
    def c3(v):
        return v.rr("p (c t) -> p c t", t=CH)

    if STOP <= 1:
        return
    for it in range(NT):
        t0 = it * T
        P.dma("sp", xt.v, x_d(t0).rr("(j p) d -> p j d", p=128))
        rms_to_hT(P, R, xt, hT, gmix, NJ, scr)
        if STOP <= 2:
            continue

        def proj_fm(c0, M):
            px = nxt(R, "pX")
            for k in range(8):
                P.mm(px[0:M, :], W[:, k, c0:c0 + M], hT[:, k, :], start=(k == 0), stop=(k == 7))
            return px

        def decay_block(b, B_scale, is_gla):
            P.scan(b.B, ones.v, b.g, 0.0, ALU.mult, ALU.add)
            B3 = c3(b.B)
            P.tt("dve", c3(b.Dm), B3, B3[:, :, 32:33].bc([128, NCH, CH]), ALU.subtract)
            P.act(b.eD, b.Dm, AF.Exp, scale=B_scale)
            P.act(b.eDn, b.Dm, AF.Exp, scale=-B_scale)
            P.memset("pool", b.prev[:, 0:1], 0.0)
            P.copy("pool", b.prev[:, 1:NCH], B3[:, 0:NCH - 1, 63])
            P.tt("pool", b.d3[:, 0, :], B3[:, :, 32], b.prev.v, ALU.subtract)
            P.tt("pool", b.d3[:, 1, :], B3[:, :, 63], b.prev.v, ALU.subtract)
            P.tt("pool", b.d3[:, 2, :], B3[:, :, 63], B3[:, :, 32], ALU.subtract)
            P.act(b.e3.v, b.d3.v, AF.Exp, scale=B_scale)

        for h in range(2):
            b = blk[h]
            px = proj_fm(C_HQ + h * 128, 128)
            P.act(b.q, px.v, AF.Silu)
            px = proj_fm(C_HF + h * 128, 128)
            P.act(b.sg, px.v, AF.Sigmoid)
            P.act(b.g, b.sg, AF.Ln, bias=lb[:, h:h + 1], scale=oml[:, h:h + 1])
            P.ts("dve", b.kk, b.sg, lbm1[:, h:h + 1], ALU.mult, oml[:, h:h + 1], ALU.add)
            decay_block(b, 1.0, False)
            P.tt("pool", b.qt.v, b.q, b.eD, ALU.mult)
            P.tt("pool", b.kt.v, b.kk, b.eDn, ALU.mult)
            P.tt("pool", c3(b.qe.v), c3(b.qt.v), b.e3[:, 0, :].unsq(2).bc([128, NCH, CH]), ALU.mult)
        if STOP <= 2.1:
            continue
        b = blk[2]
        px = proj_fm(C_GA, 16)
        P.copy("act", ga_sb.v, px[0:16, :])
        px = nxt(R, "pX")
        P.mm(px.v, gup.v, ga_sb.v)
        P.act(b.sg, px.v, AF.Sigmoid, bias=gbias[:, 0:1])
        P.act(b.g, b.sg, AF.Ln)
        decay_block(b, 1.0 / 16.0, True)
        px = proj_fm(C_GQ, 128)
        P.stt("dve", b.qt.v, px.v, 0.125, b.eD, ALU.mult, ALU.mult)
        px = proj_fm(C_GK, 128)
        P.tt("dve", b.kt.v, px.v, b.eDn, ALU.mult)
        P.tt("pool", c3(b.qe.v), c3(b.qt.v), b.e3[:, 0, :].unsq(2).bc([128, NCH, CH]), ALU.mult)
        if STOP <= 2.2:
            continue
        for i in range(3):
            b = blk[i]
            pt = nxt(R, "pT")
            for j in range(NJ):
                P.tr(pt[:, j * 128:(j + 1) * 128], b.kt[:, j * 128:(j + 1) * 128], R.identb.v)
            P.copy("act", b.ktm.v.rr("p j d -> p (j d)"), pt.v)
        if STOP <= 2.3:
            continue
        for h in range(2):
            m = ML[h]
            px = proj_fm(C_MU + h * 128, 128)
            P.copy("act", uext[h][:, 3:3 + T], px.v)
            P.ts("dve", m.acc.v, uext[h][:, 0:T], convw[:, h, 0:1], ALU.mult, convb[:, h:h + 1], ALU.add)
            for tap in range(1, 4):
                P.stt("dve", m.acc.v, uext[h][:, tap:tap + T], convw[:, h, tap:tap + 1], m.acc.v, ALU.mult, ALU.add)
            P.act(m.conv.v, m.acc.v, AF.Silu)
            P.copy("pool", m.ub.v, uext[h][:, 3:3 + T])
            P.copy("pool", uext[h][:, 0:3], uext[h][:, T:T + 3])
            px = nxt(R, "pX"); P.mm(px.v, BDq[h].v, m.conv.v)
            P.copy("act", m.qT.v, px.v)
            px = nxt(R, "pX"); P.mm(px.v, BDks[h][:, 0:128], m.conv.v)
            P.copy("act", m.kT.v, px.v)
        if STOP <= 2.4:
            continue
        px = proj_fm(C_MF, 2)
        P.act(g_sf.v, px[0:2, :], AF.Sigmoid, bias=bf[:, 0:1])
        P.act(g_lf.v, g_sf.v, AF.Ln)
        P.scan(g_B.v, ones[0:2, :], g_lf.v, 0.0, ALU.mult, ALU.add)
        px = proj_fm(C_MI, 2)
        P.stt("dve", g_a.v, px[0:2, :], bi[:, 0:1], g_B.v, ALU.add, ALU.subtract)
        P.reduce("dve", g_am.v, c3(g_a.v), ALU.max)
        P.scan(g_R.v, g_am.v, g_am.v, m0[:, 0:1], ALU.max, ALU.max)
        P.copy("dve", g_mp[:, 0:1], m0.v)
        P.copy("dve", g_mp[:, 1:NCH], g_R[:, 0:NCH - 1])
        P.tt("dve", g_wprev.v, g_mp.v, g_R.v, ALU.subtract)
        P.act(g_wprev.v, g_wprev.v, AF.Exp)
        P.tt("dve", c3(g_wj.v), c3(g_a.v), g_R.v.unsq(2).bc([2, NCH, CH]), ALU.subtract)
        P.act(g_wj.v, g_wj.v, AF.Exp)
        P.tt("dve", c3(g_thr.v), c3(g_B.v), g_R.v.unsq(2).bc([2, NCH, CH]), ALU.add)
        P.act(g_thr.v, g_thr.v, AF.Exp, scale=-1.0)
        P.tt("dve", m0.v, g_R[:, NCH - 1:NCH], g_B[:, T - 1:T], ALU.add)
        if STOP <= 2.5:
            continue
        for j in range(NJ):
            P.tr(R.pSm[:, j * 4:j * 4 + 2], g_wj[:, j * 128:(j + 1) * 128], R.ident[0:2, 0:2])
            P.tr(R.pSm[:, j * 4 + 2:j * 4 + 4], g_thr[:, j * 128:(j + 1) * 128], R.ident[0:2, 0:2])
        for h in range(2):
            P.mm(R.pSm[:, 16 + h * NCH:16 + (h + 1) * NCH], R.sel[:, h, :], g_wprev.v)
        P.copy("dve", gtm.v.rr("p j a h -> p (j a h)"), R.pSm[:, 0:16])
        P.ts("dve", gtm[:, :, 0, :], gtm[:, :, 0, :], float(128 ** -0.5), ALU.mult)
        for h in range(2):
            P.copy("dve", ML[h].wp.v, R.pSm[:, 16 + h * NCH:16 + (h + 1) * NCH])
        if STOP <= 2.6:
            continue
        for h in range(2):
            m = ML[h]
            for j2 in range(NJ // 2):
                px = nxt(R, "pX")
                for jj in range(2):
                    j = j2 * 2 + jj
                    P.mm(px[:, jj * 256:(jj + 1) * 256], m.conv[:, j * 128:(j + 1) * 128], BDks[h].v)
                for jj in range(2):
                    j = j2 * 2 + jj
                    P.act(m.khat[:, j, :], px[:, jj * 256:jj * 256 + 128], AF.Copy, scale=gtm[:, j, 0, h:h + 1])
                    P.copy("dve", sctm[:, j, h * 128:(h + 1) * 128], px[:, jj * 256 + 128:(jj + 1) * 256])
            if STOP <= 2.65:
                continue
            px = nxt(R, "pX")
            for j in range(NJ):
                P.mm(px[:, j * 128:(j + 1) * 128], m.ub[:, j * 128:(j + 1) * 128], BDv[h].v)
            P.copy("act", m.vaug[:, :, 0:128], px.v.rr("p (j e) -> p j e", e=128))
        if STOP <= 2.7:
            continue
        for j in range(NJ):
            for gi, (c0, n) in enumerate(((C_TM, 512), (C_TM + 512, 512), (C_TM + 1024, 256))):
                px = nxt(R, "pX")
                for k in range(8):
                    P.mm(px[:, 0:n], hT[:, k, j * 128:(j + 1) * 128], W[:, k, c0:c0 + n], start=(k == 0), stop=(k == 7))
                if gi == 0:
                    P.copy("dve", vtm[:, j, :], px.v)
                elif gi == 1:
                    P.act(ztm[:, j, 0:512], px.v, AF.Silu)
                else:
                    P.act(ztm[:, j, 512:768], px[:, 0:256], AF.Silu)

        if STOP <= 3:
            continue
        dl = [(blk[0], 0, 128, st_f["h0"], st_b["h0"], 0, 0),
              (blk[1], 0, 128, st_f["h1"], st_b["h1"], 128, 128),
              (blk[2], 0, 64, st_f["g"], st_b["g0"], 256, 256),
              (blk[2], 64, 64, st_f["g"], st_b["g1"], 384, 384)]
        for pr in range(NJ):
            tok = slice(pr * 128, (pr + 1) * 128)
            for hi, (b, pb, K, sf, sbb, vc, oc) in enumerate(dl):
                pS = nxt(R, "pS")
                P.mm(pS.v, b.kt[pb:pb + K, tok], b.qt[pb:pb + K, tok])
                P.tt("dve", scT[hi].v, pS.v, R.maskT.v, ALU.mult)
            for h in range(2):
                m = ML[h]
                pS = nxt(R, "pS")
                P.mm(pS.v, m.kT[:, tok], m.qT[:, tok])
                P.stt("dve", scT[4 + h].v, pS.v, gtm[:, pr, 0, h:h + 1], R.maskT.v, ALU.mult, ALU.mult)
            c0, c1 = 2 * pr, 2 * pr + 1
            gc0 = it * NCH + c0
            r0, r1 = slice(0, 64), slice(64, 128)
            t0c, t1c = slice(c0 * CH, (c0 + 1) * CH), slice(c1 * CH, (c1 + 1) * CH)

            def dl_update(hi, c, rows, dst_par):
                b, pb, K, sf, sbb, vc, oc = dl[hi]
                pk = slice(pb, pb + K)
                pKV = nxt(R, "pKV")
                P.mm(pKV[pk, 0:128], b.ktm[rows, pr, pk], vtm[rows, pr, vc:vc + 128])
                tk = tmpkv[hi % 3]
                P.act(tk[pk, :], pKV[pk, 0:128], AF.Copy, scale=b.e3[pk, 2, c:c + 1])
                P.stt("dve", sf[pk, :], sf[pk, :], b.e3[pk, 1, c:c + 1], tk[pk, :], ALU.mult, ALU.add)
                P.copy("pool", sbb[dst_par][pk, :], sf[pk, :])

            for hi in range(4):
                dl_update(hi, c0, r0, 1)
            for h in range(2):
                m = ML[h]
                sf = st_f["m%d" % h]; sbb = st_b["m%d" % h]; sP = mstP if h == 0 else mstP2
                P.ts("dve", sP.v, sf.v, m.wp[:, c0:c0 + 1], ALU.mult)
                P.act(sbb[0].v, sf.v, AF.Copy, scale=m.wp[:, c0:c0 + 1])
                pKV = nxt(R, "pKV")
                P.mm(pKV[:, 0:129], m.khat[r0, pr, :], m.vaug[r0, pr, 0:129])
                P.tt("dve", sf.v, sP.v, pKV[:, 0:129], ALU.add)
                P.ts("dve", sP.v, sf.v, m.wp[:, c1:c1 + 1], ALU.mult)
                P.act(sbb[1].v, sf.v, AF.Copy, scale=m.wp[:, c1:c1 + 1])
            for hi, (b, pb, K, sf, sbb, vc, oc) in enumerate(dl):
                pk = slice(pb, pb + K)
                pO = R.pO[hi]
                P.mm(pO[r0, 0:128], b.qe[pk, t0c], sbb[0][pk, :], start=True, stop=False)
                P.mm(pO[r1, 0:128], b.qe[pk, t1c], sbb[1][pk, :], start=True, stop=False)
                P.mm(pO[:, 0:128], scT[hi].v, vtm[:, pr, vc:vc + 128], start=False, stop=True)
                P.copy("act", osb[:, pr, oc:oc + 128], pO[:, 0:128])
            for h in range(2):
                m = ML[h]
                sbb = st_b["m%d" % h]
                pO = R.pO[4 + h]
                P.mm(pO[r0, 0:129], m.qT[:, t0c], sbb[0].v, start=True, stop=False)
                P.mm(pO[r1, 0:129], m.qT[:, t1c], sbb[1].v, start=True, stop=False)
                P.mm(pO[:, 0:129], scT[4 + h].v, m.vaug[:, pr, 0:129], start=False, stop=True)
                P.act(den[:, 0:1], pO[:, 128:129], AF.Abs)
                P.tt("dve", den[:, 1:2], den[:, 0:1], gtm[:, pr, 1, h:h + 1], ALU.max)
                P.recip(den[:, 2:3], den[:, 1:2])
                P.act(osb[:, pr, 512 + h * 128:512 + (h + 1) * 128], pO[:, 0:128], AF.Copy, scale=den[:, 2:3])
            for hi in range(4):
                dl_update(hi, c1, r1, 0)
            for h in range(2):
                m = ML[h]
                sf = st_f["m%d" % h]; sP = mstP if h == 0 else mstP2
                pKV = nxt(R, "pKV")
                P.mm(pKV[:, 0:129], m.khat[r1, pr, :], m.vaug[r1, pr, 0:129])
                P.tt("dve", sf.v, sP.v, pKV[:, 0:129], ALU.add)

        if STOP <= 4:
            continue
        P.tt("pool", osq, osb[:, :, 0:768], osb[:, :, 0:768], ALU.mult)
        P.reduce("dve", st6.v, osq.rr("p j (h e) -> p j h e", e=128), ALU.add)
        P.reduce("dve", msum.v, osb[:, :, 512:768].rr("p j (h e) -> p j h e", e=128), ALU.add)
        P.ts("dve", mmean.v, msum.v, 1.0 / 128, ALU.mult)
        P.tt("dve", mvar.v, mmean.v, mmean.v, ALU.mult)
        P.ts("dve", st6b.v, st6.v, 1.0 / 128, ALU.mult)
        P.tt("dve", st6b[:, :, 4:6], st6b[:, :, 4:6], mvar.v, ALU.subtract)
        P.ts("dve", st6b.v, st6b.v, EPS, ALU.add)
        P.act(st6c.v, st6b.v, AF.Ln)
        P.act(st6c.v, st6c.v, AF.Exp, scale=-0.5)
        o4 = osb[:, :, 512:768].rr("p j (h e) -> p j h e", e=128)
        P.tt("pool", o4, o4, mmean.v.unsq(3).bc([128, NJ, 2, 128]), ALU.subtract)
        oall = osb[:, :, 0:768].rr("p j (h e) -> p j h e", e=128)
        P.tt("dve", oall, oall, st6c.v.unsq(3).bc([128, NJ, 6, 128]), ALU.mult)
        P.tt("pool", osb[:, :, 0:768], osb[:, :, 0:768], grow.v.unsq(1).bc([128, NJ, 768]), ALU.mult)
        P.tt("dve", osb[:, :, 512:768], osb[:, :, 512:768], sctm.v, ALU.add)
        P.tt("pool", mixb[:, :, 0:768], osb[:, :, 0:768], ztm.v, ALU.mult)
        for cc in range(6):
            pt = nxt(R, "pT")
            for j in range(NJ):
                P.tr(pt[:, j * 128:(j + 1) * 128], mixb[:, j, cc * 128:(cc + 1) * 128], R.identb.v)
            P.copy("act" if cc % 2 == 0 else "dve", mixT[:, cc, :], pt.v)
        for j in range(NJ):
            yb = ysb[j % 2]
            for nh in range(2):
                px = nxt(R, "pX")
                for cc in range(6):
                    P.mm(px.v, mixT[:, cc, j * 128:(j + 1) * 128], Wo[:, cc, nh * 512:(nh + 1) * 512], start=(cc == 0), stop=(cc == 5))
                P.copy("act" if nh == 0 else "dve", yb[:, nh * 512:(nh + 1) * 512], px.v)
            P.dma("sp", y_d[t0 + j * 128:t0 + (j + 1) * 128, :], yb.v)


def consts_np():
    ident = np.eye(128, dtype=np.float32)
    maskT = np.zeros((128, 128), np.float32)
    for a in range(2):
        blk = np.triu(np.ones((CH, CH), np.float32))
        maskT[a * CH:(a + 1) * CH, a * CH:(a + 1) * CH] = blk
    bdmask = np.kron(np.eye(32, dtype=np.float32), np.ones((4, 1), np.float32))
    sel = np.zeros((2, 2, 128), np.float32)
    sel[0, 0, :] = 1.0
    sel[1, 1, :] = 1.0
    return {"ident": ident, "maskT": maskT, "bdmask": bdmask, "sel": sel}


CONST_SHAPES = {"ident": [128, 128], "maskT": [128, 128], "bdmask": [128, 32], "sel": [2, 2, 128]}

MIX_SHAPES = {
    "wc": [D, NCOL], "wo": [768, D], "gmix": [128, 8], "lblog": [128, 2, DEPTH], "hnorm": [128], "gnorm": [128],
    "mnorm": [256], "gup": [16, 128], "gbias": [128, 1], "convw": [128, 2, 4], "convb": [128, 2], "skip": [128, 2],
    "bi": [2, 1], "bf": [2, 1], "wq": [128, 2, 4], "wk": [128, 2, 4], "wv": [128, 2, 4],
}


def mixer_weights_np(inp, l, hh):
    w_in = inp["w_in"][l]
    h2 = slice(hh * 256, (hh + 1) * 256)
    h1 = slice(hh * 128, (hh + 1) * 128)

    def cols(i, sl):
        return w_in[:, OFF[i] + sl.start:OFF[i] + sl.stop]

    wc = np.concatenate([
        cols(0, h2), cols(1, h2), cols(4, h1), cols(5, h1), cols(9, h2),
        w_in[:, OFF[7]:OFF[8]], cols(11, slice(hh * 2, hh * 2 + 2)), cols(12, slice(hh * 2, hh * 2 + 2)),
        cols(2, h2), cols(6, h2), cols(3, h2), cols(8, h2), cols(10, h2)], axis=1)
    assert wc.shape[1] == NCOL
    w_out = inp["w_out"][l]
    wo = np.concatenate([w_out[hh * 256:(hh + 1) * 256], w_out[512 + hh * 256:512 + (hh + 1) * 256],
                         w_out[1024 + hh * 256:1024 + (hh + 1) * 256]], axis=0)
    d = {
        "wc": wc, "wo": wo,
        "gmix": inp["norm_mix"][l].reshape(8, 128).T,
        "lblog": inp["hgrn_lb_logits"][:, h2].reshape(DEPTH, 2, 128).transpose(2, 1, 0),
        "hnorm": inp["hgrn_norm"][l], "gnorm": inp["gla_norm"][l],
        "mnorm": inp["mlstm_norm"][l][h2],
        "gup": inp["gla_gate_up"][l][:, h1],
        "gbias": inp["gla_gate_bias"][l][h1].reshape(128, 1),
        "convw": inp["mlstm_conv_w"][l][:, h2].reshape(4, 2, 128).transpose(2, 1, 0),
        "convb": inp["mlstm_conv_b"][l][h2].reshape(2, 128).T,
        "skip": inp["mlstm_skip"][l][h2].reshape(2, 128).T,
        "bi": inp["mlstm_igate_bias"][l][hh * 2:hh * 2 + 2].reshape(2, 1),
        "bf": inp["mlstm_fgate_bias"][l][hh * 2:hh * 2 + 2].reshape(2, 1),
    }
    for nm in ("wq", "wk", "wv"):
        w = inp["mlstm_" + nm][l]
        d[nm] = w[hh * 64:(hh + 1) * 64].reshape(2, 128, 4).transpose(1, 0, 2)
    return {k: np.ascontiguousarray(v, dtype=np.float32) for k, v in d.items()}


def build_mixer_prog(S, layer):
    nc = bass.Bass("TRN2", target_bir_lowering=False)
    P = Prog(nc)
    cd = {k: P.dram_in("c_" + k, shp) for k, shp in CONST_SHAPES.items()}
    wd = {k: P.dram_in("w_" + k, shp) for k, shp in MIX_SHAPES.items()}
    x_d = P.dram_in("x", [S, D])
    y_d = P.dram_out("y", [S, D])
    R = setup_common(P, cd)
    emit_mixer(P, R, S, lambda t0: x_d[t0:t0 + T, :], y_d, wd, layer)
    P.finish("sp", [y_d])
    P.es.close()
    return nc, P


XA_SHAPES = {"wq": [D, D], "wk": [D, D], "wv": [D, D], "wo": [D, D], "gx": [128, 8], "gm": [128, 8], "gfin": [D]}


def emit_xattn(P, R, S2, x_d, ys_d, mem_d, out_d, wd, final):
    NT = S2 // T
    stage = P.sb("xstage", [128, D])
    Wq = P.sb("Wq", [128, 8, D], BF16)
    Wo = P.sb("Wo2", [128, 8, D], BF16)
    kT = P.sb("kT", [128, 8, MEM], BF16)
    vtm = P.sb("xvtm", [128, 2, D], BF16)
    gx = P.sb("gx", [128, 8]); P.dma("sp", gx.v, wd["gx"].v)
    gm = P.sb("gm", [128, 8]); P.dma("sp", gm.v, wd["gm"].v)
    onesb = P.sb("onesb", [128, 128], BF16); P.memset("pool", onesb.v, 1.0)
    gfin = None
    if final:
        gfin = P.sb("gfin", [128, D]); P.dma("sp", gfin.v, wd["gfin"].v.pbc(128))
    xt = P.sb("x_xt", [128, NJ, D])
    yt = P.sb("x_yt", [128, NJ, D])
    scr = (P.sb("x_ss", [128, NJ]), P.sb("x_sstmp", [128, NJ]), P.sb("x_rs", [128, NJ]),
           P.sb("x_junk", [128, D], BF16), P.sb("x_xn", [128, NJ, D], BF16))
    hT = P.sb("x_hT", [128, 8, T], BF16)
    P.push()
    Wk = P.sb("Wk", [128, 8, D], BF16)
    Wv = P.sb("Wv", [128, 8, D], BF16)
    load_cast_weight(P, Wk, wd["wk"], 8, D, [stage], ["pool", "dve"])
    load_cast_weight(P, Wv, wd["wv"], 8, D, [stage], ["pool", "dve"])
    P.dma("sp", xt[:, 0:2, :], mem_d.v.rr("(j p) d -> p j d", p=128))
    rms_to_hT(P, R, xt, hT, gm, 2, scr)
    for cb in range(8):
        px = nxt(R, "pX")
        for k in range(8):
            P.mm(px[:, 0:MEM], Wk[:, k, cb * 128:(cb + 1) * 128], hT[:, k, 0:MEM], start=(k == 0), stop=(k == 7))
        P.copy("act" if cb % 2 == 0 else "dve", kT[:, cb, :], px[:, 0:MEM])
    for mj in range(2):
        for nh in range(2):
            px = nxt(R, "pX")
            for k in range(8):
                P.mm(px.v, hT[:, k, mj * 128:(mj + 1) * 128], Wv[:, k, nh * 512:(nh + 1) * 512], start=(k == 0), stop=(k == 7))
            P.copy("act" if nh == 0 else "dve", vtm[:, mj, nh * 512:(nh + 1) * 512], px.v)
    P.pop()
    load_cast_weight(P, Wq, wd["wq"], 8, D, [stage], ["pool", "dve"])
    load_cast_weight(P, Wo, wd["wo"], 8, D, [stage], ["pool", "dve"])
    qT = P.sb("x_qT", [128, 8, T], BF16)
    pT = [P.sb("x_pT%d" % i, [128, T], BF16) for i in range(2)]
    rinv = P.sb("x_rinv", [128, T])
    oT = P.sb("x_oT", [128, 8, T], BF16)
    for it in range(NT):
        t0 = it * T
        P.dma("sp", xt.v, x_d(t0).rr("(j p) d -> p j d", p=128))
        for yi, y_d in enumerate(ys_d):
            P.dma("act", yt.v, y_d[t0:t0 + T, :].rr("(j p) d -> p j d", p=128))
            P.tt("dve" if yi == 0 else "pool", xt.v, xt.v, yt.v, ALU.add)
        rms_to_hT(P, R, xt, hT, gx, NJ, scr)
        for cb in range(8):
            px = nxt(R, "pX")
            for k in range(8):
                P.mm(px.v, Wq[:, k, cb * 128:(cb + 1) * 128], hT[:, k, :], start=(k == 0), stop=(k == 7))
            P.copy("act" if cb % 2 == 0 else "dve", qT[:, cb, :], px.v)
        for hd in range(4):
            for mj in range(2):
                px = nxt(R, "pX")
                for i2 in range(2):
                    cb = hd * 2 + i2
                    P.mm(px.v, kT[:, cb, mj * 128:(mj + 1) * 128], qT[:, cb, :], start=(i2 == 0), stop=(i2 == 1))
                P.act(pT[mj].v, px.v, AF.Exp, scale=1.0 / 16.0)
            px = nxt(R, "pX")
            for mj in range(2):
                P.mm(px.v, onesb.v, pT[mj].v, start=(mj == 0), stop=(mj == 1))
            P.recip(rinv.v, px.v)
            for e2 in range(2):
                px = nxt(R, "pX")
                c0 = hd * 256 + e2 * 128
                for mj in range(2):
                    P.mm(px.v, vtm[:, mj, c0:c0 + 128], pT[mj].v, start=(mj == 0), stop=(mj == 1))
                P.tt("dve", oT[:, hd * 2 + e2, :], px.v, rinv.v, ALU.mult)
        for j in range(NJ):
            for nh in range(2):
                px = nxt(R, "pX")
                for cb in range(8):
                    P.mm(px.v, oT[:, cb, j * 128:(j + 1) * 128], Wo[:, cb, nh * 512:(nh + 1) * 512], start=(cb == 0), stop=(cb == 7))
                P.tt("dve", xt[:, j, nh * 512:(nh + 1) * 512], px.v, xt[:, j, nh * 512:(nh + 1) * 512], ALU.add)
        if final:
            ss, tmp, rs, junk, xn = scr
            P.memset("pool", ss.v, 0.0)
            for j in range(NJ):
                P.act(junk.v, xt[:, j, :], AF.Square, accum=ss[:, j:j + 1])
            P.rstd(rs.v, ss.v, 1.0 / D, tmp.v)
            for j in range(NJ):
                P.act(xt[:, j, :], xt[:, j, :], AF.Copy, scale=rs[:, j:j + 1])
                P.tt("pool", xt[:, j, :], xt[:, j, :], gfin.v, ALU.mult)
        P.dma("sp", out_d(t0).rr("(j p) d -> p j d", p=128), xt.v)


def xattn_weights_np(inp, l):
    d = {"wq": inp["xa_wq"][l], "wk": inp["xa_wk"][l], "wv": inp["xa_wv"][l], "wo": inp["xa_wo"][l],
         "gx": inp["norm_xattn"][l].reshape(8, 128).T, "gm": inp["norm_mem"][l].reshape(8, 128).T,
         "gfin": inp["norm_final"]}
    return {k: np.ascontiguousarray(v, dtype=np.float32) for k, v in d.items()}


def build_xattn_prog(S2, final, ny=2):
    nc = bass.Bass("TRN2", target_bir_lowering=False)
    P = Prog(nc)
    cd = {k: P.dram_in("c_" + k, shp) for k, shp in CONST_SHAPES.items()}
    wd = {k: P.dram_in("w_" + k, shp) for k, shp in XA_SHAPES.items()}
    x_d = P.dram_in("x", [S2, D])
    ys_d = [P.dram_in("y%d" % i, [S2, D]) for i in range(ny)]
    mem_d = P.dram_in("mem", [MEM, D])
    out_d = P.dram_out("out", [S2, D])
    R = setup_common(P, cd)
    emit_xattn(P, R, S2, lambda t0: x_d[t0:t0 + T, :], ys_d, mem_d, lambda t0: out_d[t0:t0 + T, :], wd, final)
    P.finish("sp", [out_d])
    P.es.close()
    return nc, P


N_CORES = 8
BATCH = 4
SEQ = 8192
PAIRS = [[0, 1], [2, 3], [4, 5], [6, 7]]


def build_fused(S):
    S2 = S // 2
    nc = bass.Bass("TRN2", target_bir_lowering=False)
    P = Prog(nc)
    cd = {k: P.dram_in("c_" + k, shp) for k, shp in CONST_SHAPES.items()}
    mwd = [{k: P.dram_in("m%d_%s" % (l, k), shp) for k, shp in MIX_SHAPES.items()} for l in range(DEPTH)]
    xwd = [{k: P.dram_in("a%d_%s" % (l, k), shp) for k, shp in XA_SHAPES.items()} for l in range(DEPTH)]
    x_full = P.dram_in("x_full", [S, D])
    x_half = P.dram_in("x_half", [S2, D])
    mem_d = P.dram_in("mem", [MEM, D])
    out_d = P.dram_out("out", [S2, D])
    ypart = P.dram_tmp("ypart", [S, D])
    ysum = P.dram_tmp("ysum", [S2, D])
    NC2 = S2 // T
    xh = [P.dram_tmp("xh%d" % c, [T, D]) for c in range(NC2)]
    xf = [P.dram_tmp("xf%d" % c, [2 * T, D]) for c in range(NC2)]
    R = setup_common(P, cd)

    def full_from_xf(t0):
        s_, u = t0 // S2, t0 % S2
        return xf[u // T][s_ * T:(s_ + 1) * T, :]

    full_src = lambda t0: x_full[t0:t0 + T, :]
    half_src = lambda t0: x_half[t0:t0 + T, :]
    for l in range(DEPTH):
        final = (l == DEPTH - 1)
        P.push()
        emit_mixer(P, R, S, full_src, ypart, mwd[l], l)
        P.barrier()
        P.pop()
        P.collective("ReduceScatter", ALU.add, PAIRS, ypart, ysum)
        P.push()
        dst = (lambda t0: out_d[t0:t0 + T, :]) if final else (lambda t0: xh[t0 // T].v)
        emit_xattn(P, R, S2, half_src, [ysum], mem_d, dst, xwd[l], final)
        P.barrier()
        P.pop()
        if not final:
            for c in range(NC2):
                P.collective("AllGather", ALU.bypass, PAIRS, xh[c], xf[c])
            full_src = full_from_xf
            half_src = lambda t0: xh[t0 // T].v
    for i in range(PADPE):
        P.mm(R.pSm[0:1, 0:1], R.ident[0:1, 0:1], R.ident[0:1, 0:1])
    for i in range(PADV):
        P.memset("dve" if i % 2 == 0 else "act", R.bdmask[0:1, 0:1], 1.0) if i % 2 == 0 else P.act(R.sel[0:1, 0, 0:1], R.sel[0:1, 0, 0:1], AF.Copy)
    if PAD:
        padt = P.sb("padt", [128, 4096])
        P.memset("pool", padt.v, 0.5)
        for i in range(PAD):
            P.act(padt.v, padt.v, AF.Copy)
    P.finish("sp", [out_d])
    P.es.close()
    return nc, P


PAD = 0
PADPE = 0
PADV = 0


def fused_inputs(inp, S):
    S2 = S // 2
    cn = {"c_" + k: v for k, v in consts_np().items()}
    mw = [[mixer_weights_np(inp, l, hh) for hh in range(2)] for l in range(DEPTH)]
    xw = [xattn_weights_np(inp, l) for l in range(DEPTH)]
    maps = []
    for i in range(N_CORES):
        b, hh = i // 2, i % 2
        xb = np.ascontiguousarray(inp["x"][b, :S], dtype=np.float32)
        m = {"x_full": xb, "x_half": np.ascontiguousarray(xb[hh * S2:(hh + 1) * S2]),
             "mem": np.ascontiguousarray(inp["mem"][b], dtype=np.float32)}
        m.update(cn)
        for l in range(DEPTH):
            m.update({"m%d_%s" % (l, k): v for k, v in mw[l][hh].items()})
            m.update({"a%d_%s" % (l, k): v for k, v in xw[l].items()})
        maps.append(m)
    return maps


def kernel(**inp):
    inp = {k: np.asarray(v) for k, v in inp.items()}
    S = inp["x"].shape[1]
    nc, _ = build_fused(S)
    maps = fused_inputs(inp, S)
    res = run_bass_kernel_spmd(nc, maps, core_ids=list(range(N_CORES)))
    outs = [r["out"] for r in res.results]
    full = [np.concatenate([outs[2 * b], outs[2 * b + 1]], axis=0) for b in range(BATCH)]
    return np.stack(full, axis=0).astype(np.float32)
```

```python
import contextlib
import numpy as np
import concourse.bass as bass
import concourse.mybir as mybir
from concourse.bass_utils import run_bass_kernel_spmd

F32 = mybir.dt.float32
BF16 = mybir.dt.bfloat16
AF = mybir.ActivationFunctionType
ALU = mybir.AluOpType
AX = mybir.AxisListType

D = 1024
DEPTH = 2
CH = 64
T = 512
NJ = T // 128
NCH = T // CH
EPS = 1e-6
MEM = 256
IN_SPLITS = [512, 512, 512, 512, 256, 256, 512, 16, 512, 512, 512, 4, 4]
OFF = np.concatenate([[0], np.cumsum(IN_SPLITS)]).tolist()
C_HQ, C_HF, C_GQ, C_GK, C_MU, C_GA, C_MI, C_MF = 0, 256, 512, 640, 768, 1024, 1040, 1042
C_TM = 1044
NCOL = C_TM + 1280


class V:
    __slots__ = ("buf", "ap")

    def __init__(self, buf, ap):
        self.buf = buf
        self.ap = ap

    def __getitem__(self, k):
        return V(self.buf, self.ap[k])

    def rr(self, s, **kw):
        return V(self.buf, self.ap.rearrange(s, **kw))

    def bc(self, shape):
        return V(self.buf, self.ap.to_broadcast(list(shape)))

    def unsq(self, axis):
        return V(self.buf, self.ap.unsqueeze(axis))

    def pbc(self, n):
        return V(self.buf, self.ap.partition_broadcast(n))

    @property
    def v(self):
        return self


class Buf:
    __slots__ = ("name", "t", "w", "r", "dsem", "dcnt", "excl")

    def __init__(self, name, t):
        self.name = name
        self.t = t
        self.excl = False
        self.w = {}
        self.r = {}
        self.dsem = None
        self.dcnt = 0

    def __getitem__(self, k):
        return V(self, self.t[k])

    @property
    def v(self):
        return V(self, self.t[:])


def _u(x):
    return x.ap if isinstance(x, V) else x


class Prog:
    def __init__(self, nc):
        self.nc = nc
        self.es = contextlib.ExitStack()
        self.eng = {"pe": nc.tensor, "act": nc.scalar, "dve": nc.vector, "pool": nc.gpsimd, "sp": nc.sync}
        self.sem = {}
        self.cnt = {}
        self.seen = {}
        for k in self.eng:
            self.sem[k] = self.es.enter_context(nc.semaphore("s_" + k))
            self.cnt[k] = 0
            self.seen[k] = {}
        self.nbuf = 0
        self.ninst = 0
        self.uid = 0
        self.stack = []
        self.root = self.es
        self.dcounts = {}
        self.nwait = {}

    def push(self):
        self.stack.append(self.es)
        self.es = contextlib.ExitStack()

    def pop(self):
        self.es.close()
        self.es = self.stack.pop()

    def sb(self, name, shape, dt=F32):
        self.uid += 1
        t = self.es.enter_context(self.nc.sbuf_tensor("sb%d_%s" % (self.uid, name), list(shape), dt))
        return Buf(name, t)

    def ps(self, name, shape, dt=F32):
        self.uid += 1
        t = self.es.enter_context(self.nc.psum_tensor("ps%d_%s" % (self.uid, name), list(shape), dt))
        b = Buf(name, t)
        b.excl = True
        return b

    def dram_in(self, name, shape, dt=F32):
        return Buf(name, self.nc.dram_tensor(name, list(shape), dt, kind="ExternalInput").ap())

    def dram_out(self, name, shape, dt=F32):
        return Buf(name, self.nc.dram_tensor(name, list(shape), dt, kind="ExternalOutput").ap())

    def dram_tmp(self, name, shape, dt=F32):
        return Buf(name, self.nc.dram_tensor(name, list(shape), dt, kind="Internal").ap())

    def alias(self, name, buf, ap):
        return V(buf, ap)

    def _wait(self, e, deps):
        for k, v in deps.items():
            if self.seen[e].get(k, 0) >= v:
                continue
            if k == e and e == "pe":
                continue
            self.eng[e].wait_ge(self.sem[k], v)
            self.nwait[e] = self.nwait.get(e, 0) + 1
            self.seen[e][k] = v

    @staticmethod
    def _deps(reads, writes, e=None):
        deps = {}
        for b in reads:
            for k, v in b.w.items():
                if deps.get(k, 0) < v:
                    deps[k] = v
            if b.excl:
                for k, v in b.r.items():
                    if k != e and deps.get(k, 0) < v:
                        deps[k] = v
        for b in writes:
            for d in (b.w, b.r):
                for k, v in d.items():
                    if deps.get(k, 0) < v:
                        deps[k] = v
        return deps

    def op(self, e, fn, ins, outs):
        reads = []
        for x in ins:
            if isinstance(x, V) and x.buf not in reads:
                reads.append(x.buf)
        writes = []
        for x in outs:
            if isinstance(x, V) and x.buf not in writes:
                writes.append(x.buf)
        self._wait(e, self._deps(reads, writes, e))
        ins_ = fn(self.eng[e])
        self.cnt[e] += 1
        c = self.cnt[e]
        ins_.then_inc(self.sem[e], 1)
        for b in writes:
            b.w = {e: c}
            b.r = {}
        for b in reads:
            if b not in writes:
                b.r[e] = c
        self.ninst += 1

    def dma(self, e, out, in_):
        reads = [in_.buf]
        writes = [out.buf]
        self._wait(e, self._deps(reads, writes))
        sbuf = out.buf
        if sbuf.dsem is None:
            key = "d%d" % self.nbuf
            self.nbuf += 1
            self.sem[key] = self.root.enter_context(self.nc.semaphore(key))
            sbuf.dsem = key
        self.eng[e].dma_start(out=out.ap, in_=in_.ap).then_inc(self.sem[sbuf.dsem], 16)
        sbuf.dcnt += 16
        self.dcounts[sbuf.dsem] = sbuf.dcnt
        out.buf.w = {sbuf.dsem: sbuf.dcnt}
        out.buf.r = {}
        in_.buf.r[sbuf.dsem] = sbuf.dcnt
        self.ninst += 1

    def barrier(self):
        deps = {k: v for k, v in self.cnt.items() if v > 0}
        deps.update(self.dcounts)
        for e in self.eng:
            self._wait(e, {k: v for k, v in deps.items() if k != e})

    def collective(self, kind, op, groups, in_buf, out_buf):
        e = "pool"
        self._wait(e, self._deps([in_buf], [out_buf], e))
        if out_buf.dsem is None:
            key = "c%d" % self.nbuf
            self.nbuf += 1
            self.sem[key] = self.root.enter_context(self.nc.semaphore(key))
            out_buf.dsem = key
        self.nc.gpsimd.collective_compute(kind, op, replica_groups=groups, ins=[in_buf.t.opt()],
                                          outs=[out_buf.t.opt()]).then_inc(self.sem[out_buf.dsem])
        out_buf.dcnt += 1
        self.dcounts[out_buf.dsem] = out_buf.dcnt
        out_buf.w = {out_buf.dsem: out_buf.dcnt}
        out_buf.r = {}
        in_buf.r[out_buf.dsem] = out_buf.dcnt
        self.ninst += 1

    def finish(self, e, bufs):
        self._wait(e, self._deps((), bufs))

    def act(self, out, in_, func, bias=None, scale=None, accum=None, e="act"):
        kw = {}
        if bias is not None:
            kw["bias"] = _u(bias)
        if scale is not None:
            kw["scale"] = _u(scale)
        if accum is not None:
            kw["accum_out"] = _u(accum)
        outs = [out] + ([accum] if accum is not None else [])
        self.op(e, lambda g: g.activation(out=out.ap, in_=in_.ap, func=func, **kw), [in_, bias, scale], outs)

    def tt(self, e, out, in0, in1, op):
        self.op(e, lambda g: g.tensor_tensor(out=out.ap, in0=in0.ap, in1=in1.ap, op=op), [in0, in1], [out])

    def ts(self, e, out, in0, s1, op0, s2=None, op1=None):
        if op1 is None:
            self.op(e, lambda g: g.tensor_scalar(out=out.ap, in0=in0.ap, scalar1=_u(s1), scalar2=None, op0=op0),
                    [in0, s1], [out])
        else:
            self.op(e, lambda g: g.tensor_scalar(out=out.ap, in0=in0.ap, scalar1=_u(s1), scalar2=_u(s2), op0=op0, op1=op1),
                    [in0, s1, s2], [out])

    def stt(self, e, out, in0, scalar, in1, op0, op1):
        self.op(e, lambda g: g.scalar_tensor_tensor(out=out.ap, in0=in0.ap, scalar=_u(scalar), in1=in1.ap, op0=op0, op1=op1),
                [in0, scalar, in1], [out])

    def copy(self, e, out, in_):
        if e == "act":
            self.act(out, in_, AF.Copy)
        else:
            self.op(e, lambda g: g.tensor_copy(out=out.ap, in_=in_.ap), [in_], [out])

    def memset(self, e, out, val):
        self.op(e, lambda g: g.memset(out.ap, val), [], [out])

    def scan(self, out, d0, d1, init, op0, op1):
        self.op("dve", lambda g: g.tensor_tensor_scan(out=out.ap, data0=d0.ap, data1=d1.ap, initial=_u(init), op0=op0, op1=op1),
                [d0, d1, init], [out])

    def reduce(self, e, out, in_, op, axis=AX.X):
        self.op(e, lambda g: g.tensor_reduce(out=out.ap, in_=in_.ap, axis=axis, op=op), [in_], [out])

    def recip(self, out, in_):
        self.op("dve", lambda g: g.reciprocal(out=out.ap, in_=in_.ap), [in_], [out])

    def mm(self, out, lhsT, rhs, start=True, stop=True):
        self.op("pe", lambda g: g.matmul(out.ap, lhsT=lhsT.ap, rhs=rhs.ap, start=start, stop=stop), [lhsT, rhs], [out])

    def tr(self, out, in_, ident):
        self.op("pe", lambda g: g.transpose(out=out.ap, in_=in_.ap, identity=ident.ap), [in_, ident], [out])

    def rstd(self, out, in_, scale, tmp):
        self.ts("dve", tmp, in_, scale, ALU.mult, EPS, ALU.add)
        self.act(tmp, tmp, AF.Ln)
        self.act(out, tmp, AF.Exp, scale=-0.5)


class Res:
    pass


def setup_common(P, consts_d):
    R = Res()
    R.ident = P.sb("ident", [128, 128])
    R.identb = P.sb("identb", [128, 128], BF16)
    R.maskT = P.sb("maskT", [128, 128])
    R.bdmask = P.sb("bdmask", [128, 32])
    R.sel = P.sb("sel", [2, 2, 128])
    P.dma("sp", R.ident.v, consts_d["ident"].v)
    P.dma("sp", R.maskT.v, consts_d["maskT"].v)
    P.dma("sp", R.bdmask.v, consts_d["bdmask"].v)
    P.dma("sp", R.sel.v, consts_d["sel"].v)
    P.copy("dve", R.identb.v, R.ident.v)
    R.pX = [P.ps("pX%d" % i, [128, 512]) for i in range(2)]
    bT = P.ps("bT", [128, 1024], BF16)
    R.pT = [P.alias("pT%d" % i, bT, bT.t[:, i * 512:(i + 1) * 512]) for i in range(2)]
    bO = [P.ps("bO%d" % i, [128, 512]) for i in range(2)]
    R.pO = [P.alias("pO%d" % i, bO[i % 2], bO[i % 2].t[:, (i // 2) * 160:(i // 2) * 160 + 132]) for i in range(6)]
    bS = P.ps("bS", [128, 512])
    R.pS = [P.alias("pS%d" % i, bS, bS.t[:, i * 128:(i + 1) * 128]) for i in range(4)]
    bKV = P.ps("bKV", [128, 512])
    R.pKV = [P.alias("pKV%d" % i, bKV, bKV.t[:, i * 160:i * 160 + 132]) for i in range(3)]
    bSm = P.ps("bSm", [128, 512])
    R.pSm = P.alias("pSm", bSm, bSm.t[:, 0:64])
    R.pX = R.pX + [bO[0], bO[1], bS, bKV]
    R.px_i = 0
    R.ps_i = 0
    R.pkv_i = 0
    R.pt_i = 0
    return R


def nxt(R, what):
    if what == "pX":
        R.px_i += 1
        return R.pX[R.px_i % len(R.pX)]
    if what == "pS":
        R.ps_i += 1
        return R.pS[R.ps_i % len(R.pS)]
    if what == "pKV":
        R.pkv_i += 1
        return R.pKV[R.pkv_i % len(R.pKV)]
    if what == "pT":
        R.pt_i += 1
        return R.pT[R.pt_i % len(R.pT)]
    raise KeyError(what)


def rms_to_hT(P, R, xt, hT, gcol, ntok_j, scr):
    nj = ntok_j
    ss, tmp, rs, junk, xn = scr
    P.memset("pool", ss.v, 0.0)
    for j in range(nj):
        P.act(junk.v, xt[:, j, :], AF.Square, accum=ss[:, j:j + 1])
    P.rstd(rs[:, 0:nj], ss[:, 0:nj], 1.0 / D, tmp[:, 0:nj])
    for j in range(nj):
        if j % 2 == 0:
            P.act(xn[:, j, :], xt[:, j, :], AF.Copy, scale=rs[:, j:j + 1])
        else:
            P.ts("dve", xn[:, j, :], xt[:, j, :], rs[:, j:j + 1], ALU.mult)
    for k in range(8):
        pt = nxt(R, "pT")
        for j in range(nj):
            P.tr(pt[:, j * 128:(j + 1) * 128], xn[:, j, k * 128:(k + 1) * 128], R.identb.v)
        P.ts("dve", hT[:, k, 0:nj * 128], pt[:, 0:nj * 128], gcol[:, k:k + 1], ALU.mult)


def load_cast_weight(P, w_sb, w_d, nk, ncols, stage, eng_cycle, nsplit=1):
    cw = ncols // nsplit
    i = 0
    for k in range(nk):
        for sp_ in range(nsplit):
            c0 = sp_ * cw
            c1 = ncols if sp_ == nsplit - 1 else c0 + cw
            st = stage[i % len(stage)]
            P.dma("sp" if i % 2 == 0 else "act", st[:, 0:c1 - c0], w_d[k * 128:(k + 1) * 128, c0:c1])
            P.copy(eng_cycle[i % len(eng_cycle)], w_sb[:, k, c0:c1], st[:, 0:c1 - c0])
            i += 1


STOP = 99


def emit_mixer(P, R, S, x_d, y_d, wd, layer):
    NT = S // T
    stage = [P.sb("wstage%d" % i, [128, NCOL // 2]) for i in range(2)]
    W = P.sb("W", [128, 8, NCOL], BF16)
    load_cast_weight(P, W, wd["wc"], 8, NCOL, stage, ["pool", "dve"], nsplit=2)
    Wo = P.sb("Wo", [128, 6, D], BF16)
    load_cast_weight(P, Wo, wd["wo"], 6, D, stage, ["pool", "dve"])
    gmix = P.sb("gmix", [128, 8]); P.dma("sp", gmix.v, wd["gmix"].v)
    lbl = P.sb("lbl", [128, 2, DEPTH]); P.dma("sp", lbl.v, wd["lblog"].v)
    lbe = P.sb("lbe", [128, 2, DEPTH]); P.act(lbe.v, lbl.v, AF.Exp)
    lbtot = P.sb("lbtot", [128, 2]); P.reduce("dve", lbtot.v, lbe.v, ALU.add)
    lbr = P.sb("lbr", [128, 2]); P.recip(lbr.v, lbtot.v)
    lb = P.sb("lb", [128, 2]); P.memset("pool", lb.v, 0.0)
    for l2 in range(1, layer + 1):
        P.tt("dve", lb.v, lb.v, lbe[:, :, l2], ALU.add)
    P.tt("dve", lb.v, lb.v, lbr.v, ALU.mult)
    oml = P.sb("oml", [128, 2]); P.ts("dve", oml.v, lb.v, -1.0, ALU.mult, 1.0, ALU.add)
    lbm1 = P.sb("lbm1", [128, 2]); P.ts("dve", lbm1.v, lb.v, -1.0, ALU.add)
    grow = P.sb("grow", [128, 768])
    for i, nm in enumerate(["hnorm", "hnorm", "gnorm", "gnorm"]):
        P.dma("sp", grow[:, i * 128:(i + 1) * 128], wd[nm].v.pbc(128))
    P.dma("sp", grow[:, 512:768], wd["mnorm"].v.pbc(128))
    gup = P.sb("gup", [16, 128]); P.dma("sp", gup.v, wd["gup"].v)
    gbias = P.sb("gbias", [128, 1]); P.dma("sp", gbias.v, wd["gbias"].v)
    convw = P.sb("convw", [128, 2, 4]); P.dma("sp", convw.v, wd["convw"].v)
    convb = P.sb("convb", [128, 2]); P.dma("sp", convb.v, wd["convb"].v)
    skip = P.sb("skip", [128, 2]); P.dma("sp", skip.v, wd["skip"].v)
    bi = P.sb("bi", [2, 1]); P.dma("sp", bi.v, wd["bi"].v)
    bf = P.sb("bf", [2, 1]); P.dma("sp", bf.v, wd["bf"].v)
    BDq, BDks, BDv = [], [], []
    wsm = {}
    for nm in ("wq", "wk", "wv"):
        wsm[nm] = P.sb("wsm_" + nm, [128, 2, 4]); P.dma("sp", wsm[nm].v, wd[nm].v)
    for h in range(2):
        bq = P.sb("BDq%d" % h, [128, 128], BF16)
        bks = P.sb("BDks%d" % h, [128, 256], BF16)
        bv = P.sb("BDv%d" % h, [128, 128], BF16)
        for dst, nm in ((bq.v, "wq"), (bks[:, 0:128], "wk"), (bv.v, "wv")):
            P.tt("pool", dst.rr("p (g o) -> p g o", o=4),
                 wsm[nm][:, h, :].unsq(1).bc([128, 32, 4]),
                 R.bdmask.v.unsq(2).bc([128, 32, 4]), ALU.mult)
        P.ts("pool", bks[:, 128:256], R.ident.v, skip[:, h:h + 1], ALU.mult)
        BDq.append(bq); BDks.append(bks); BDv.append(bv)

    st_f = {}
    st_b = {}
    for nm, ncol in (("h0", 128), ("h1", 128), ("g", 128), ("m0", 129), ("m1", 129)):
        st_f[nm] = P.sb("stf_" + nm, [128, ncol])
        P.memset("pool", st_f[nm].v, 0.0)
    for nm, ncol in (("h0", 128), ("h1", 128), ("g0", 128), ("g1", 128), ("m0", 129), ("m1", 129)):
        st_b[nm] = [P.sb("stb_%s_%d" % (nm, i), [128, ncol], BF16) for i in range(2)]
        P.memset("pool", st_b[nm][0].v, 0.0)
    mstP = P.sb("mstP", [128, 129])
    mstP2 = P.sb("mstP2", [128, 129])
    uext = [P.sb("uext%d" % h, [128, 3 + T]) for h in range(2)]
    for h in range(2):
        P.memset("pool", uext[h][:, 0:3], 0.0)
    m0 = P.sb("m0", [2, 1]); P.memset("pool", m0.v, 0.0)

    xt = P.sb("xt", [128, NJ, D])
    scr = (P.sb("ss", [128, NJ]), P.sb("sstmp", [128, NJ]), P.sb("rs", [128, NJ]),
           P.sb("junk", [128, D], BF16), P.sb("xn", [128, NJ, D], BF16))
    hT = P.sb("hT", [128, 8, T], BF16)
    vtm = P.sb("vtm", [128, NJ, 512], BF16)
    ztm = P.sb("ztm", [128, NJ, 768])
    osb = xt
    ones = P.sb("ones", [128, T]); P.memset("pool", ones.v, 1.0)
    tmpA = P.sb("tmpA", [128, 8, T])
    tq, tsg, tg, tkk, tB, tDm, teD, teDn = [tmpA[:, i, :] for i in range(8)]
    blk = []
    for i in range(3):
        b = Res()
        b.q, b.sg, b.g, b.kk, b.B, b.Dm, b.eD, b.eDn = tq, tsg, tg, tkk, tB, tDm, teD, teDn
        b.qt = P.sb("qt%d" % i, [128, T], BF16)
        b.kt = P.sb("kt%d" % i, [128, T], BF16)
        b.qe = P.sb("qe%d" % i, [128, T], BF16)
        b.ktm = P.sb("ktm%d" % i, [128, NJ, 128], BF16)
        b.prev = P.sb("prev%d" % i, [128, NCH])
        b.d3 = P.sb("d3%d" % i, [128, 3, NCH])
        b.e3 = P.sb("e3%d" % i, [128, 3, NCH])
        blk.append(b)
    ga_sb = P.sb("ga_sb", [16, T])
    tmpkv = [P.sb("tmpkv%d" % i, [128, 128]) for i in range(3)]
    scT = [P.sb("scT%d" % i, [128, 128], BF16) for i in range(6)]
    ML = []
    for h in range(2):
        m = Res()
        m.acc = P.sb("cacc%d" % h, [128, T])
        m.conv = P.sb("conv%d" % h, [128, T], BF16)
        m.ub = P.sb("ub%d" % h, [128, T], BF16)
        m.qT = P.sb("mqT%d" % h, [128, T], BF16)
        m.kT = P.sb("mkT%d" % h, [128, T], BF16)
        m.khat = P.sb("khat%d" % h, [128, NJ, 128], BF16)
        m.vaug = P.sb("vaug%d" % h, [128, NJ, 132], BF16)
        P.memset("pool", m.vaug.v, 1.0)
        m.wp = P.sb("wp%d" % h, [128, NCH])
        ML.append(m)
    sctm = P.sb("sctm", [128, NJ, 256])
    g_sf = P.sb("g_sf", [2, T]); g_lf = P.sb("g_lf", [2, T]); g_B = P.sb("g_B", [2, T])
    g_a = P.sb("g_a", [2, T]); g_wj = P.sb("g_wj", [2, T]); g_thr = P.sb("g_thr", [2, T])
    g_am = P.sb("g_am", [2, NCH]); g_R = P.sb("g_R", [2, NCH]); g_mp = P.sb("g_mp", [2, NCH]); g_wprev = P.sb("g_wprev", [2, NCH])
    gtm = P.sb("gtm", [128, NJ, 2, 2])
    den = P.sb("den", [128, 4])
    st6 = P.sb("st6", [128, NJ, 6]); st6b = P.sb("st6b", [128, NJ, 6]); st6c = P.sb("st6c", [128, NJ, 6])
    msum = P.sb("msum", [128, NJ, 2]); mmean = P.sb("mmean", [128, NJ, 2]); mvar = P.sb("mvar", [128, NJ, 2])
    osq = tmpA.v.rr("p a t -> p (a t)")[:, 0:NJ * 768].rr("p (j c) -> p j c", c=768)
    mixb = scr[4]
    mixT = hT
    ysb = [P.sb("ysb%d" % i, [128, D]) for i in range(2)]
    def c3(v):
        return v.rr("p (c t) -> p c t", t=CH)

    if STOP <= 1:
        return
    for it in range(NT):
        t0 = it * T
        P.dma("sp", xt.v, x_d(t0).rr("(j p) d -> p j d", p=128))
        rms_to_hT(P, R, xt, hT, gmix, NJ, scr)
        if STOP <= 2:
            continue

        def proj_fm(c0, M):
            px = nxt(R, "pX")
            for k in range(8):
                P.mm(px[0:M, :], W[:, k, c0:c0 + M], hT[:, k, :], start=(k == 0), stop=(k == 7))
            return px

        def decay_block(b, B_scale, is_gla):
            P.scan(b.B, ones.v, b.g, 0.0, ALU.mult, ALU.add)
            B3 = c3(b.B)
            P.tt("dve", c3(b.Dm), B3, B3[:, :, 32:33].bc([128, NCH, CH]), ALU.subtract)
            P.act(b.eD, b.Dm, AF.Exp, scale=B_scale)
            P.act(b.eDn, b.Dm, AF.Exp, scale=-B_scale)
            P.memset("pool", b.prev[:, 0:1], 0.0)
            P.copy("pool", b.prev[:, 1:NCH], B3[:, 0:NCH - 1, 63])
            P.tt("pool", b.d3[:, 0, :], B3[:, :, 32], b.prev.v, ALU.subtract)
            P.tt("pool", b.d3[:, 1, :], B3[:, :, 63], b.prev.v, ALU.subtract)
            P.tt("pool", b.d3[:, 2, :], B3[:, :, 63], B3[:, :, 32], ALU.subtract)
            P.act(b.e3.v, b.d3.v, AF.Exp, scale=B_scale)

        for h in range(2):
            b = blk[h]
            px = proj_fm(C_HQ + h * 128, 128)
            P.act(b.q, px.v, AF.Silu)
            px = proj_fm(C_HF + h * 128, 128)
            P.act(b.sg, px.v, AF.Sigmoid)
            P.act(b.g, b.sg, AF.Ln, bias=lb[:, h:h + 1], scale=oml[:, h:h + 1])
            P.ts("dve", b.kk, b.sg, lbm1[:, h:h + 1], ALU.mult, oml[:, h:h + 1], ALU.add)
            decay_block(b, 1.0, False)
            P.tt("pool", b.qt.v, b.q, b.eD, ALU.mult)
            P.tt("pool", b.kt.v, b.kk, b.eDn, ALU.mult)
            P.tt("pool", c3(b.qe.v), c3(b.qt.v), b.e3[:, 0, :].unsq(2).bc([128, NCH, CH]), ALU.mult)
        if STOP <= 2.1:
            continue
        b = blk[2]
        px = proj_fm(C_GA, 16)
        P.copy("act", ga_sb.v, px[0:16, :])
        px = nxt(R, "pX")
        P.mm(px.v, gup.v, ga_sb.v)
        P.act(b.sg, px.v, AF.Sigmoid, bias=gbias[:, 0:1])
        P.act(b.g, b.sg, AF.Ln)
        decay_block(b, 1.0 / 16.0, True)
        px = proj_fm(C_GQ, 128)
        P.stt("dve", b.qt.v, px.v, 0.125, b.eD, ALU.mult, ALU.mult)
        px = proj_fm(C_GK, 128)
        P.tt("dve", b.kt.v, px.v, b.eDn, ALU.mult)
        P.tt("pool", c3(b.qe.v), c3(b.qt.v), b.e3[:, 0, :].unsq(2).bc([128, NCH, CH]), ALU.mult)
        if STOP <= 2.2:
            continue
        for i in range(3):
            b = blk[i]
            pt = nxt(R, "pT")
            for j in range(NJ):
                P.tr(pt[:, j * 128:(j + 1) * 128], b.kt[:, j * 128:(j + 1) * 128], R.identb.v)
            P.copy("act", b.ktm.v.rr("p j d -> p (j d)"), pt.v)
        if STOP <= 2.3:
            continue
        for h in range(2):
            m = ML[h]
            px = proj_fm(C_MU + h * 128, 128)
            P.copy("act", uext[h][:, 3:3 + T], px.v)
            P.ts("dve", m.acc.v, uext[h][:, 0:T], convw[:, h, 0:1], ALU.mult, convb[:, h:h + 1], ALU.add)
            for tap in range(1, 4):
                P.stt("dve", m.acc.v, uext[h][:, tap:tap + T], convw[:, h, tap:tap + 1], m.acc.v, ALU.mult, ALU.add)
            P.act(m.conv.v, m.acc.v, AF.Silu)
            P.copy("pool", m.ub.v, uext[h][:, 3:3 + T])
            P.copy("pool", uext[h][:, 0:3], uext[h][:, T:T + 3])
            px = nxt(R, "pX"); P.mm(px.v, BDq[h].v, m.conv.v)
            P.copy("act", m.qT.v, px.v)
            px = nxt(R, "pX"); P.mm(px.v, BDks[h][:, 0:128], m.conv.v)
            P.copy("act", m.kT.v, px.v)
        if STOP <= 2.4:
            continue
        px = proj_fm(C_MF, 2)
        P.act(g_sf.v, px[0:2, :], AF.Sigmoid, bias=bf[:, 0:1])
        P.act(g_lf.v, g_sf.v, AF.Ln)
        P.scan(g_B.v, ones[0:2, :], g_lf.v, 0.0, ALU.mult, ALU.add)
        px = proj_fm(C_MI, 2)
        P.stt("dve", g_a.v, px[0:2, :], bi[:, 0:1], g_B.v, ALU.add, ALU.subtract)
        P.reduce("dve", g_am.v, c3(g_a.v), ALU.max)
        P.scan(g_R.v, g_am.v, g_am.v, m0[:, 0:1], ALU.max, ALU.max)
        P.copy("dve", g_mp[:, 0:1], m0.v)
        P.copy("dve", g_mp[:, 1:NCH], g_R[:, 0:NCH - 1])
        P.tt("dve", g_wprev.v, g_mp.v, g_R.v, ALU.subtract)
        P.act(g_wprev.v, g_wprev.v, AF.Exp)
        P.tt("dve", c3(g_wj.v), c3(g_a.v), g_R.v.unsq(2).bc([2, NCH, CH]), ALU.subtract)
        P.act(g_wj.v, g_wj.v, AF.Exp)
        P.tt("dve", c3(g_thr.v), c3(g_B.v), g_R.v.unsq(2).bc([2, NCH, CH]), ALU.add)
        P.act(g_thr.v, g_thr.v, AF.Exp, scale=-1.0)
        P.tt("dve", m0.v, g_R[:, NCH - 1:NCH], g_B[:, T - 1:T], ALU.add)
        if STOP <= 2.5:
            continue
        for j in range(NJ):
            P.tr(R.pSm[:, j * 4:j * 4 + 2], g_wj[:, j * 128:(j + 1) * 128], R.ident[0:2, 0:2])
            P.tr(R.pSm[:, j * 4 + 2:j * 4 + 4], g_thr[:, j * 128:(j + 1) * 128], R.ident[0:2, 0:2])
        for h in range(2):
            P.mm(R.pSm[:, 16 + h * NCH:16 + (h + 1) * NCH], R.sel[:, h, :], g_wprev.v)
        P.copy("dve", gtm.v.rr("p j a h -> p (j a h)"), R.pSm[:, 0:16])
        P.ts("dve", gtm[:, :, 0, :], gtm[:, :, 0, :], float(128 ** -0.5), ALU.mult)
        for h in range(2):
            P.copy("dve", ML[h].wp.v, R.pSm[:, 16 + h * NCH:16 + (h + 1) * NCH])
        if STOP <= 2.6:
            continue
        for h in range(2):
            m = ML[h]
            for j2 in range(NJ // 2):
                px = nxt(R, "pX")
                for jj in range(2):
                    j = j2 * 2 + jj
                    P.mm(px[:, jj * 256:(jj + 1) * 256], m.conv[:, j * 128:(j + 1) * 128], BDks[h].v)
                for jj in range(2):
                    j = j2 * 2 + jj
                    P.act(m.khat[:, j, :], px[:, jj * 256:jj * 256 + 128], AF.Copy, scale=gtm[:, j, 0, h:h + 1])
                    P.copy("dve", sctm[:, j, h * 128:(h + 1) * 128], px[:, jj * 256 + 128:(jj + 1) * 256])
            if STOP <= 2.65:
                continue
            px = nxt(R, "pX")
            for j in range(NJ):
                P.mm(px[:, j * 128:(j + 1) * 128], m.ub[:, j * 128:(j + 1) * 128], BDv[h].v)
            P.copy("act", m.vaug[:, :, 0:128], px.v.rr("p (j e) -> p j e", e=128))
        if STOP <= 2.7:
            continue
        for j in range(NJ):
            for gi, (c0, n) in enumerate(((C_TM, 512), (C_TM + 512, 512), (C_TM + 1024, 256))):
                px = nxt(R, "pX")
                for k in range(8):
                    P.mm(px[:, 0:n], hT[:, k, j * 128:(j + 1) * 128], W[:, k, c0:c0 + n], start=(k == 0), stop=(k == 7))
                if gi == 0:
                    P.copy("dve", vtm[:, j, :], px.v)
                elif gi == 1:
                    P.act(ztm[:, j, 0:512], px.v, AF.Silu)
                else:
                    P.act(ztm[:, j, 512:768], px[:, 0:256], AF.Silu)

        if STOP <= 3:
            continue
        dl = [(blk[0], 0, 128, st_f["h0"], st_b["h0"], 0, 0),
              (blk[1], 0, 128, st_f["h1"], st_b["h1"], 128, 128),
              (blk[2], 0, 64, st_f["g"], st_b["g0"], 256, 256),
              (blk[2], 64, 64, st_f["g"], st_b["g1"], 384, 384)]
        for pr in range(NJ):
            tok = slice(pr * 128, (pr + 1) * 128)
            for hi, (b, pb, K, sf, sbb, vc, oc) in enumerate(dl):
                pS = nxt(R, "pS")
                P.mm(pS.v, b.kt[pb:pb + K, tok], b.qt[pb:pb + K, tok])
                P.tt("dve", scT[hi].v, pS.v, R.maskT.v, ALU.mult)
            for h in range(2):
                m = ML[h]
                pS = nxt(R, "pS")
                P.mm(pS.v, m.kT[:, tok], m.qT[:, tok])
                P.stt("dve", scT[4 + h].v, pS.v, gtm[:, pr, 0, h:h + 1], R.maskT.v, ALU.mult, ALU.mult)
            c0, c1 = 2 * pr, 2 * pr + 1
            gc0 = it * NCH + c0
            r0, r1 = slice(0, 64), slice(64, 128)
            t0c, t1c = slice(c0 * CH, (c0 + 1) * CH), slice(c1 * CH, (c1 + 1) * CH)

            def dl_update(hi, c, rows, dst_par):
                b, pb, K, sf, sbb, vc, oc = dl[hi]
                pk = slice(pb, pb + K)
                pKV = nxt(R, "pKV")
                P.mm(pKV[pk, 0:128], b.ktm[rows, pr, pk], vtm[rows, pr, vc:vc + 128])
                tk = tmpkv[hi % 3]
                P.act(tk[pk, :], pKV[pk, 0:128], AF.Copy, scale=b.e3[pk, 2, c:c + 1])
                P.stt("dve", sf[pk, :], sf[pk, :], b.e3[pk, 1, c:c + 1], tk[pk, :], ALU.mult, ALU.add)
                P.copy("pool", sbb[dst_par][pk, :], sf[pk, :])

            for hi in range(4):
                dl_update(hi, c0, r0, 1)
            for h in range(2):
                m = ML[h]
                sf = st_f["m%d" % h]; sbb = st_b["m%d" % h]; sP = mstP if h == 0 else mstP2
                P.ts("dve", sP.v, sf.v, m.wp[:, c0:c0 + 1], ALU.mult)
                P.act(sbb[0].v, sf.v, AF.Copy, scale=m.wp[:, c0:c0 + 1])
                pKV = nxt(R, "pKV")
                P.mm(pKV[:, 0:129], m.khat[r0, pr, :], m.vaug[r0, pr, 0:129])
                P.tt("dve", sf.v, sP.v, pKV[:, 0:129], ALU.add)
                P.ts("dve", sP.v, sf.v, m.wp[:, c1:c1 + 1], ALU.mult)
                P.act(sbb[1].v, sf.v, AF.Copy, scale=m.wp[:, c1:c1 + 1])
            for hi, (b, pb, K, sf, sbb, vc, oc) in enumerate(dl):
                pk = slice(pb, pb + K)
                pO = R.pO[hi]
                P.mm(pO[r0, 0:128], b.qe[pk, t0c], sbb[0][pk, :], start=True, stop=False)
                P.mm(pO[r1, 0:128], b.qe[pk, t1c], sbb[1][pk, :], start=True, stop=False)
                P.mm(pO[:, 0:128], scT[hi].v, vtm[:, pr, vc:vc + 128], start=False, stop=True)
                P.copy("act", osb[:, pr, oc:oc + 128], pO[:, 0:128])
            for h in range(2):
                m = ML[h]
                sbb = st_b["m%d" % h]
                pO = R.pO[4 + h]
                P.mm(pO[r0, 0:129], m.qT[:, t0c], sbb[0].v, start=True, stop=False)
                P.mm(pO[r1, 0:129], m.qT[:, t1c], sbb[1].v, start=True, stop=False)
                P.mm(pO[:, 0:129], scT[4 + h].v, m.vaug[:, pr, 0:129], start=False, stop=True)
                P.act(den[:, 0:1], pO[:, 128:129], AF.Abs)
                P.tt("dve", den[:, 1:2], den[:, 0:1], gtm[:, pr, 1, h:h + 1], ALU.max)
                P.recip(den[:, 2:3], den[:, 1:2])
                P.act(osb[:, pr, 512 + h * 128:512 + (h + 1) * 128], pO[:, 0:128], AF.Copy, scale=den[:, 2:3])
            for hi in range(4):
                dl_update(hi, c1, r1, 0)
            for h in range(2):
                m = ML[h]
                sf = st_f["m%d" % h]; sP = mstP if h == 0 else mstP2
                pKV = nxt(R, "pKV")
                P.mm(pKV[:, 0:129], m.khat[r1, pr, :], m.vaug[r1, pr, 0:129])
                P.tt("dve", sf.v, sP.v, pKV[:, 0:129], ALU.add)

        if STOP <= 4:
            continue
        P.tt("pool", osq, osb[:, :, 0:768], osb[:, :, 0:768], ALU.mult)
        P.reduce("dve", st6.v, osq.rr("p j (h e) -> p j h e", e=128), ALU.add)
        P.reduce("dve", msum.v, osb[:, :, 512:768].rr("p j (h e) -> p j h e", e=128), ALU.add)
        P.ts("dve", mmean.v, msum.v, 1.0 / 128, ALU.mult)
        P.tt("dve", mvar.v, mmean.v, mmean.v, ALU.mult)
        P.ts("dve", st6b.v, st6.v, 1.0 / 128, ALU.mult)
        P.tt("dve", st6b[:, :, 4:6], st6b[:, :, 4:6], mvar.v, ALU.subtract)
        P.ts("dve", st6b.v, st6b.v, EPS, ALU.add)
        P.act(st6c.v, st6b.v, AF.Ln)
        P.act(st6c.v, st6c.v, AF.Exp, scale=-0.5)
        o4 = osb[:, :, 512:768].rr("p j (h e) -> p j h e", e=128)
        P.tt("pool", o4, o4, mmean.v.unsq(3).bc([128, NJ, 2, 128]), ALU.subtract)
        oall = osb[:, :, 0:768].rr("p j (h e) -> p j h e", e=128)
        P.tt("dve", oall, oall, st6c.v.unsq(3).bc([128, NJ, 6, 128]), ALU.mult)
        P.tt("pool", osb[:, :, 0:768], osb[:, :, 0:768], grow.v.unsq(1).bc([128, NJ, 768]), ALU.mult)
        P.tt("dve", osb[:, :, 512:768], osb[:, :, 512:768], sctm.v, ALU.add)
        P.tt("pool", mixb[:, :, 0:768], osb[:, :, 0:768], ztm.v, ALU.mult)
        for cc in range(6):
            pt = nxt(R, "pT")
            for j in range(NJ):
                P.tr(pt[:, j * 128:(j + 1) * 128], mixb[:, j, cc * 128:(cc + 1) * 128], R.identb.v)
            P.copy("act" if cc % 2 == 0 else "dve", mixT[:, cc, :], pt.v)
        for j in range(NJ):
            yb = ysb[j % 2]
            for nh in range(2):
                px = nxt(R, "pX")
                for cc in range(6):
                    P.mm(px.v, mixT[:, cc, j * 128:(j + 1) * 128], Wo[:, cc, nh * 512:(nh + 1) * 512], start=(cc == 0), stop=(cc == 5))
                P.copy("act" if nh == 0 else "dve", yb[:, nh * 512:(nh + 1) * 512], px.v)
            P.dma("sp", y_d[t0 + j * 128:t0 + (j + 1) * 128, :], yb.v)


def consts_np():
    ident = np.eye(128, dtype=np.float32)
    maskT = np.zeros((128, 128), np.float32)
    for a in range(2):
        blk = np.triu(np.ones((CH, CH), np.float32))
        maskT[a * CH:(a + 1) * CH, a * CH:(a + 1) * CH] = blk
    bdmask = np.kron(np.eye(32, dtype=np.float32), np.ones((4, 1), np.float32))
    sel = np.zeros((2, 2, 128), np.float32)
    sel[0, 0, :] = 1.0
    sel[1, 1, :] = 1.0
    return {"ident": ident, "maskT": maskT, "bdmask": bdmask, "sel": sel}


CONST_SHAPES = {"ident": [128, 128], "maskT": [128, 128], "bdmask": [128, 32], "sel": [2, 2, 128]}

MIX_SHAPES = {
    "wc": [D, NCOL], "wo": [768, D], "gmix": [128, 8], "lblog": [128, 2, DEPTH], "hnorm": [128], "gnorm": [128],
    "mnorm": [256], "gup": [16, 128], "gbias": [128, 1], "convw": [128, 2, 4], "convb": [128, 2], "skip": [128, 2],
    "bi": [2, 1], "bf": [2, 1], "wq": [128, 2, 4], "wk": [128, 2, 4], "wv": [128, 2, 4],
}


def mixer_weights_np(inp, l, hh):
    w_in = inp["w_in"][l]
    h2 = slice(hh * 256, (hh + 1) * 256)
    h1 = slice(hh * 128, (hh + 1) * 128)

    def cols(i, sl):
        return w_in[:, OFF[i] + sl.start:OFF[i] + sl.stop]

    wc = np.concatenate([
        cols(0, h2), cols(1, h2), cols(4, h1), cols(5, h1), cols(9, h2),
        w_in[:, OFF[7]:OFF[8]], cols(11, slice(hh * 2, hh * 2 + 2)), cols(12, slice(hh * 2, hh * 2 + 2)),
        cols(2, h2), cols(6, h2), cols(3, h2), cols(8, h2), cols(10, h2)], axis=1)
    assert wc.shape[1] == NCOL
    w_out = inp["w_out"][l]
    wo = np.concatenate([w_out[hh * 256:(hh + 1) * 256], w_out[512 + hh * 256:512 + (hh + 1) * 256],
                         w_out[1024 + hh * 256:1024 + (hh + 1) * 256]], axis=0)
    d = {
        "wc": wc, "wo": wo,
        "gmix": inp["norm_mix"][l].reshape(8, 128).T,
        "lblog": inp["hgrn_lb_logits"][:, h2].reshape(DEPTH, 2, 128).transpose(2, 1, 0),
        "hnorm": inp["hgrn_norm"][l], "gnorm": inp["gla_norm"][l],
        "mnorm": inp["mlstm_norm"][l][h2],
        "gup": inp["gla_gate_up"][l][:, h1],
        "gbias": inp["gla_gate_bias"][l][h1].reshape(128, 1),
        "convw": inp["mlstm_conv_w"][l][:, h2].reshape(4, 2, 128).transpose(2, 1, 0),
        "convb": inp["mlstm_conv_b"][l][h2].reshape(2, 128).T,
        "skip": inp["mlstm_skip"][l][h2].reshape(2, 128).T,
        "bi": inp["mlstm_igate_bias"][l][hh * 2:hh * 2 + 2].reshape(2, 1),
        "bf": inp["mlstm_fgate_bias"][l][hh * 2:hh * 2 + 2].reshape(2, 1),
    }
    for nm in ("wq", "wk", "wv"):
        w = inp["mlstm_" + nm][l]
        d[nm] = w[hh * 64:(hh + 1) * 64].reshape(2, 128, 4).transpose(1, 0, 2)
    return {k: np.ascontiguousarray(v, dtype=np.float32) for k, v in d.items()}


def build_mixer_prog(S, layer):
    nc = bass.Bass("TRN2", target_bir_lowering=False)
    P = Prog(nc)
    cd = {k: P.dram_in("c_" + k, shp) for k, shp in CONST_SHAPES.items()}
    wd = {k: P.dram_in("w_" + k, shp) for k, shp in MIX_SHAPES.items()}
    x_d = P.dram_in("x", [S, D])
    y_d = P.dram_out("y", [S, D])
    R = setup_common(P, cd)
    emit_mixer(P, R, S, lambda t0: x_d[t0:t0 + T, :], y_d, wd, layer)
    P.finish("sp", [y_d])
    P.es.close()
    return nc, P


XA_SHAPES = {"wq": [D, D], "wk": [D, D], "wv": [D, D], "wo": [D, D], "gx": [128, 8], "gm": [128, 8], "gfin": [D]}


def emit_xattn(P, R, S2, x_d, ys_d, mem_d, out_d, wd, final):
    NT = S2 // T
    stages = [P.sb("xstage%d" % i, [128, D]) for i in range(4)]
    Wq = P.sb("Wq", [128, 8, D], BF16)
    Wo = P.sb("Wo2", [128, 8, D], BF16)
    kT = P.sb("kT", [128, 8, MEM], BF16)
    vtm = P.sb("xvtm", [128, 2, D], BF16)
    gx = P.sb("gx", [128, 8]); P.dma("sp", gx.v, wd["gx"].v)
    gm = P.sb("gm", [128, 8]); P.dma("sp", gm.v, wd["gm"].v)
    onesb = P.sb("onesb", [128, 128], BF16); P.memset("pool", onesb.v, 1.0)
    gfin = None
    if final:
        gfin = P.sb("gfin", [128, D]); P.dma("sp", gfin.v, wd["gfin"].v.pbc(128))
    xt = P.sb("x_xt", [128, NJ, D])
    yt = P.sb("x_yt", [128, NJ, D])
    scr = (P.sb("x_ss", [128, NJ]), P.sb("x_sstmp", [128, NJ]), P.sb("x_rs", [128, NJ]),
           P.sb("x_junk", [128, D], BF16), P.sb("x_xn", [128, NJ, D], BF16))
    hT = P.sb("x_hT", [128, 8, T], BF16)
    P.push()
    Wk = P.sb("Wk", [128, 8, D], BF16)
    Wv = P.sb("Wv", [128, 8, D], BF16)
    load_cast_weight(P, Wk, wd["wk"], 8, D, stages, ["pool", "dve"])
    load_cast_weight(P, Wv, wd["wv"], 8, D, stages, ["pool", "dve"])
    P.dma("sp", xt[:, 0:2, :], mem_d.v.rr("(j p) d -> p j d", p=128))
    rms_to_hT(P, R, xt, hT, gm, 2, scr)
    for cb in range(8):
        px = nxt(R, "pX")
        for k in range(8):
            P.mm(px[:, 0:MEM], Wk[:, k, cb * 128:(cb + 1) * 128], hT[:, k, 0:MEM], start=(k == 0), stop=(k == 7))
        P.copy("act" if cb % 2 == 0 else "dve", kT[:, cb, :], px[:, 0:MEM])
    for mj in range(2):
        for nh in range(2):
            px = nxt(R, "pX")
            for k in range(8):
                P.mm(px.v, hT[:, k, mj * 128:(mj + 1) * 128], Wv[:, k, nh * 512:(nh + 1) * 512], start=(k == 0), stop=(k == 7))
            P.copy("act" if nh == 0 else "dve", vtm[:, mj, nh * 512:(nh + 1) * 512], px.v)
    P.pop()
    load_cast_weight(P, Wq, wd["wq"], 8, D, stages, ["pool", "dve"])
    load_cast_weight(P, Wo, wd["wo"], 8, D, stages, ["pool", "dve"])
    qT = P.sb("x_qT", [128, 8, T], BF16)
    pT = [P.sb("x_pT%d" % i, [128, T], BF16) for i in range(2)]
    rinv = P.sb("x_rinv", [128, T])
    oT = P.sb("x_oT", [128, 8, T], BF16)
    for it in range(NT):
        t0 = it * T
        P.dma("sp", xt.v, x_d(t0).rr("(j p) d -> p j d", p=128))
        for yi, y_d in enumerate(ys_d):
            P.dma("act", yt.v, y_d[t0:t0 + T, :].rr("(j p) d -> p j d", p=128))
            P.tt("dve" if yi == 0 else "pool", xt.v, xt.v, yt.v, ALU.add)
        rms_to_hT(P, R, xt, hT, gx, NJ, scr)
        for cb in range(8):
            px = nxt(R, "pX")
            for k in range(8):
                P.mm(px.v, Wq[:, k, cb * 128:(cb + 1) * 128], hT[:, k, :], start=(k == 0), stop=(k == 7))
            P.copy("act" if cb % 2 == 0 else "dve", qT[:, cb, :], px.v)
        for hd in range(4):
            for mj in range(2):
                px = nxt(R, "pX")
                for i2 in range(2):
                    cb = hd * 2 + i2
                    P.mm(px.v, kT[:, cb, mj * 128:(mj + 1) * 128], qT[:, cb, :], start=(i2 == 0), stop=(i2 == 1))
                P.act(pT[mj].v, px.v, AF.Exp, scale=1.0 / 16.0)
            px = nxt(R, "pX")
            for mj in range(2):
                P.mm(px.v, onesb.v, pT[mj].v, start=(mj == 0), stop=(mj == 1))
            P.recip(rinv.v, px.v)
            for e2 in range(2):
                px = nxt(R, "pX")
                c0 = hd * 256 + e2 * 128
                for mj in range(2):
                    P.mm(px.v, vtm[:, mj, c0:c0 + 128], pT[mj].v, start=(mj == 0), stop=(mj == 1))
                P.tt("dve", oT[:, hd * 2 + e2, :], px.v, rinv.v, ALU.mult)
        for j in range(NJ):
            for nh in range(2):
                px = nxt(R, "pX")
                for cb in range(8):
                    P.mm(px.v, oT[:, cb, j * 128:(j + 1) * 128], Wo[:, cb, nh * 512:(nh + 1) * 512], start=(cb == 0), stop=(cb == 7))
                P.tt("dve", xt[:, j, nh * 512:(nh + 1) * 512], px.v, xt[:, j, nh * 512:(nh + 1) * 512], ALU.add)
        if final:
            ss, tmp, rs, junk, xn = scr
            P.memset("pool", ss.v, 0.0)
            for j in range(NJ):
                P.act(junk.v, xt[:, j, :], AF.Square, accum=ss[:, j:j + 1])
            P.rstd(rs.v, ss.v, 1.0 / D, tmp.v)
            for j in range(NJ):
                P.act(xt[:, j, :], xt[:, j, :], AF.Copy, scale=rs[:, j:j + 1])
                P.tt("pool", xt[:, j, :], xt[:, j, :], gfin.v, ALU.mult)
        P.dma("sp", out_d(t0).rr("(j p) d -> p j d", p=128), xt.v)


def xattn_weights_np(inp, l):
    d = {"wq": inp["xa_wq"][l], "wk": inp["xa_wk"][l], "wv": inp["xa_wv"][l], "wo": inp["xa_wo"][l],
         "gx": inp["norm_xattn"][l].reshape(8, 128).T, "gm": inp["norm_mem"][l].reshape(8, 128).T,
         "gfin": inp["norm_final"]}
    return {k: np.ascontiguousarray(v, dtype=np.float32) for k, v in d.items()}


def build_xattn_prog(S2, final, ny=2):
    nc = bass.Bass("TRN2", target_bir_lowering=False)
    P = Prog(nc)
    cd = {k: P.dram_in("c_" + k, shp) for k, shp in CONST_SHAPES.items()}
    wd = {k: P.dram_in("w_" + k, shp) for k, shp in XA_SHAPES.items()}
    x_d = P.dram_in("x", [S2, D])
    ys_d = [P.dram_in("y%d" % i, [S2, D]) for i in range(ny)]
    mem_d = P.dram_in("mem", [MEM, D])
    out_d = P.dram_out("out", [S2, D])
    R = setup_common(P, cd)
    emit_xattn(P, R, S2, lambda t0: x_d[t0:t0 + T, :], ys_d, mem_d, lambda t0: out_d[t0:t0 + T, :], wd, final)
    P.finish("sp", [out_d])
    P.es.close()
    return nc, P


N_CORES = 8
BATCH = 4
SEQ = 8192
PAIRS = [[0, 1], [2, 3], [4, 5], [6, 7]]


def build_fused(S):
    S2 = S // 2
    nc = bass.Bass("TRN2", target_bir_lowering=False)
    P = Prog(nc)
    cd = {k: P.dram_in("c_" + k, shp) for k, shp in CONST_SHAPES.items()}
    mwd = [{k: P.dram_in("m%d_%s" % (l, k), shp) for k, shp in MIX_SHAPES.items()} for l in range(DEPTH)]
    xwd = [{k: P.dram_in("a%d_%s" % (l, k), shp) for k, shp in XA_SHAPES.items()} for l in range(DEPTH)]
    x_full = P.dram_in("x_full", [S, D])
    x_half = P.dram_in("x_half", [S2, D])
    mem_d = P.dram_in("mem", [MEM, D])
    out_d = P.dram_out("out", [S2, D])
    ypart = P.dram_tmp("ypart", [S, D])
    ysum = P.dram_tmp("ysum", [S2, D])
    NC2 = S2 // T
    xh = [P.dram_tmp("xh%d" % c, [T, D]) for c in range(NC2)]
    xf = [P.dram_tmp("xf%d" % c, [2 * T, D]) for c in range(NC2)]
    R = setup_common(P, cd)

    def full_from_xf(t0):
        s_, u = t0 // S2, t0 % S2
        return xf[u // T][s_ * T:(s_ + 1) * T, :]

    full_src = lambda t0: x_full[t0:t0 + T, :]
    half_src = lambda t0: x_half[t0:t0 + T, :]
    for l in range(DEPTH):
        final = (l == DEPTH - 1)
        P.push()
        emit_mixer(P, R, S, full_src, ypart, mwd[l], l)
        P.barrier()
        P.pop()
        P.collective("ReduceScatter", ALU.add, PAIRS, ypart, ysum)
        P.push()
        dst = (lambda t0: out_d[t0:t0 + T, :]) if final else (lambda t0: xh[t0 // T].v)
        emit_xattn(P, R, S2, half_src, [ysum], mem_d, dst, xwd[l], final)
        P.barrier()
        P.pop()
        if not final:
            for c in range(NC2):
                P.collective("AllGather", ALU.bypass, PAIRS, xh[c], xf[c])
            full_src = full_from_xf
            half_src = lambda t0: xh[t0 // T].v
    for i in range(PADPE):
        P.mm(R.pSm[0:1, 0:1], R.ident[0:1, 0:1], R.ident[0:1, 0:1])
    for i in range(PADV):
        P.memset("dve" if i % 2 == 0 else "act", R.bdmask[0:1, 0:1], 1.0) if i % 2 == 0 else P.act(R.sel[0:1, 0, 0:1], R.sel[0:1, 0, 0:1], AF.Copy)
    if PAD:
        padt = P.sb("padt", [128, 4096])
        P.memset("pool", padt.v, 0.5)
        for i in range(PAD):
            P.act(padt.v, padt.v, AF.Copy)
    P.finish("sp", [out_d])
    P.es.close()
    return nc, P


PAD = 0
PADPE = 0
PADV = 0


def fused_inputs(inp, S):
    S2 = S // 2
    cn = {"c_" + k: v for k, v in consts_np().items()}
    mw = [[mixer_weights_np(inp, l, hh) for hh in range(2)] for l in range(DEPTH)]
    xw = [xattn_weights_np(inp, l) for l in range(DEPTH)]
    maps = []
    for i in range(N_CORES):
        b, hh = i // 2, i % 2
        xb = np.ascontiguousarray(inp["x"][b, :S], dtype=np.float32)
        m = {"x_full": xb, "x_half": np.ascontiguousarray(xb[hh * S2:(hh + 1) * S2]),
             "mem": np.ascontiguousarray(inp["mem"][b], dtype=np.float32)}
        m.update(cn)
        for l in range(DEPTH):
            m.update({"m%d_%s" % (l, k): v for k, v in mw[l][hh].items()})
            m.update({"a%d_%s" % (l, k): v for k, v in xw[l].items()})
        maps.append(m)
    return maps


def kernel(**inp):
    inp = {k: np.asarray(v) for k, v in inp.items()}
    S = inp["x"].shape[1]
    nc, _ = build_fused(S)
    maps = fused_inputs(inp, S)
    res = run_bass_kernel_spmd(nc, maps, core_ids=list(range(N_CORES)))
    outs = [r["out"] for r in res.results]
    full = [np.concatenate([outs[2 * b], outs[2 * b + 1]], axis=0) for b in range(BATCH)]
    return np.stack(full, axis=0).astype(np.float32)
```
